# Optimizing a Trainium2 kernel written in Bass

```python
import math
import jax
import jax.numpy as jnp
from jax import lax
import numpy as np

D_MODEL = 1024
BATCH = 16
SEQ = 4096
DEPTH = 4

GRID_W = 64
CTX_LEN = 256
D_MIX = D_MODEL
GROUP = D_MIX // 4

MLA_HEADS = 4
MLA_NOPE = 64
MLA_ROPE = 32
MLA_V = 64
MLA_Q_RANK = 192
MLA_KV_RANK = 128
ROPE_BASE = 10000.0
Q_BLOCK = 128

LRU_BLOCKS = 4
LRU_CONV = 4
LRU_CONV_LEFT = 2
LRU_C = 8.0

RWKV_HEADS = 4
RWKV_HEAD = GROUP // RWKV_HEADS
RWKV_DECAY_LORA = 32
RWKV_AAA_LORA = 32
RWKV_GATE_LORA = 64
RWKV_GN_EPS = 64e-5

HY_ORDER = 2
HY_SHORT = 3
HY_BANDS = 16
HY_EMB = 1 + 2 * HY_BANDS
HY_HIDDEN = 64
HY_SIN_FREQ = 1.0
HY_DECAY_MIN = math.log(1e-2) / 1.5
HY_DECAY_MAX = math.log(1e-2) / 0.3
HY_SHIFT = 0.05

FF_DENSE = 2816
N_EXPERTS = 8
TOP_K = 2
FF_EXPERT = 3584
MOE_BLOCK = 512

ALPHA = (2.0 * DEPTH) ** 0.25
BETA = (8.0 * DEPTH) ** -0.25

A_CQ = 0
A_CKV = A_CQ + MLA_Q_RANK
A_KR = A_CKV + MLA_KV_RANK
B_X = A_KR + MLA_ROPE
B_GATE = B_X + GROUP
C_OFF = B_GATE + GROUP
C_R = 0
C_K = GROUP
C_V = 2 * GROUP
C_WD = 3 * GROUP
C_AD = C_WD + 2 * RWKV_DECAY_LORA
C_GD = C_AD + 2 * RWKV_AAA_LORA
C_COLS = C_GD + RWKV_GATE_LORA
D_OFF = C_OFF + C_COLS
D_COLS = (HY_ORDER + 1) * GROUP
N_IN = D_OFF + D_COLS

kernel_name = 'hybrid_mla_rglru_rwkv7_hyena_moe_dit'


def modulate(x, shift, scale):
    return x * (1.0 + scale) + shift


def layer_norm(x, g, b, eps=1e-5):
    xf = x.astype(jnp.float32)
    mu = jnp.mean(xf, -1, keepdims=True)
    var = jnp.mean(jnp.square(xf - mu), -1, keepdims=True)
    return ((xf - mu) * lax.rsqrt(var + eps) * g + b).astype(x.dtype)


def rms_norm(x, g, eps=1e-6):
    xf = x.astype(jnp.float32)
    return (xf * lax.rsqrt(jnp.mean(jnp.square(xf), -1, keepdims=True) + eps) * g).astype(x.dtype)


def dwconv(x, w, b, left):
    K = w.shape[0]
    L = x.shape[1]
    xp = jnp.pad(x, ((0, 0), (left, K - 1 - left), (0, 0)))
    return sum(xp[:, j:j + L] * w[j] for j in range(K)) + b


def token_shift(z, mu_prev, mu_next):
    zp = jnp.pad(z, ((0, 0), (1, 1), (0, 0)))
    return z + mu_prev * (zp[:, :-2] - z) + mu_next * (zp[:, 2:] - z)


def axial_rope(rows):
    f32 = jnp.float32
    r, col = jnp.meshgrid(jnp.arange(rows, dtype=f32), jnp.arange(GRID_W, dtype=f32), indexing='ij')
    half = MLA_ROPE // 2
    inv = 1.0 / (ROPE_BASE ** (jnp.arange(0, half, 2, dtype=f32) / half))
    ang = jnp.concatenate([r.reshape(-1, 1) * inv, col.reshape(-1, 1) * inv], -1)
    return jnp.cos(ang), jnp.sin(ang)


def apply_rope(x, cos, sin):
    xf = x.astype(jnp.float32)
    h = xf.shape[-1] // 2
    x1, x2 = xf[..., :h], xf[..., h:]
    return jnp.concatenate([x1 * cos - x2 * sin, x1 * sin + x2 * cos], -1).astype(x.dtype)


def block_attention(q, k, v):
    B, Lq, H, Dh = q.shape
    nb = Lq // Q_BLOCK
    qb = jnp.moveaxis(q.reshape(B, nb, Q_BLOCK, H, Dh), 1, 0)
    scale = Dh ** -0.5

    def one(qblk):
        s = jnp.einsum('bqhd,bkhd->bhqk', qblk, k, preferred_element_type=jnp.float32) * scale
        p = jax.nn.softmax(s, axis=-1).astype(v.dtype)
        return jnp.einsum('bhqk,bkhd->bqhd', p, v)

    o = lax.map(one, qb)
    return jnp.moveaxis(o, 0, 1).reshape(B, Lq, H, v.shape[-1])


def mla_queries(P, q_norm, w_uq, cos, sin):
    B, L, _ = P.shape
    cq = rms_norm(P[..., A_CQ:A_CQ + MLA_Q_RANK], q_norm)
    q = (cq @ w_uq).reshape(B, L, MLA_HEADS, MLA_NOPE + MLA_ROPE)
    if cos is None:
        return q
    q_rope = apply_rope(q[..., MLA_NOPE:], cos[None, :, None, :], sin[None, :, None, :])
    return jnp.concatenate([q[..., :MLA_NOPE], q_rope], -1)


def mla_keys(P, kv_norm, w_ukv, cos, sin):
    B, L, _ = P.shape
    ckv = rms_norm(P[..., A_CKV:A_CKV + MLA_KV_RANK], kv_norm)
    kv = (ckv @ w_ukv).reshape(B, L, MLA_HEADS, MLA_NOPE + MLA_V)
    k_nope, v = kv[..., :MLA_NOPE], kv[..., MLA_NOPE:]
    k_rope = P[..., A_KR:A_KR + MLA_ROPE]
    if cos is not None:
        k_rope = apply_rope(k_rope, cos[None], sin[None])
    k_rope = jnp.broadcast_to(k_rope[:, :, None, :], (B, L, MLA_HEADS, MLA_ROPE))
    return jnp.concatenate([k_nope, k_rope], -1), v


def mla_mixer(Pl, Pc, q_norm, kv_norm, w_uq, w_ukv, out_norm, cos, sin, ctx_out):
    B, L, _ = Pl.shape
    kl, vl = mla_keys(Pl, kv_norm, w_ukv, cos, sin)
    kc, vc = mla_keys(Pc, kv_norm, w_ukv, None, None)
    ql = mla_queries(Pl, q_norm, w_uq, cos, sin)
    k_all = jnp.concatenate([kc, kl], 1)
    v_all = jnp.concatenate([vc, vl], 1)
    yl = rms_norm(block_attention(ql, k_all, v_all).reshape(B, L, GROUP), out_norm)
    yc = None
    if ctx_out:
        qc = mla_queries(Pc, q_norm, w_uq, None, None)
        yc = rms_norm(block_attention(qc, kc, vc).reshape(B, Pc.shape[1], GROUP), out_norm)
    return yl, yc


def block_diag(x, w, b):
    B, L, _ = x.shape
    xb = x.reshape(B, L, LRU_BLOCKS, -1)
    return jnp.einsum('blnc,ncd->blnd', xb, w).reshape(B, L, -1) + b


def linear_scan(a, b, h0, reverse):
    if reverse:
        a, b = jnp.flip(a, 1), jnp.flip(b, 1)
    b = b.at[:, 0].add(a[:, 0] * h0)
    _, h = lax.associative_scan(lambda l, r: (l[0] * r[0], r[0] * l[1] + r[1]), (a, b), axis=1)
    final = h[:, -1]
    if reverse:
        h = jnp.flip(h, 1)
    return h, final


def rglru_direction(u, w_r, b_r, w_i, b_i, lam, h0, reverse):
    r = jax.nn.sigmoid(block_diag(u, w_r, b_r))
    i = jax.nn.sigmoid(block_diag(u, w_i, b_i))
    log_a = -LRU_C * r * jax.nn.softplus(-lam)
    a = jnp.exp(log_a)
    b = jnp.sqrt(-jnp.expm1(2.0 * log_a)) * (i * u)
    return linear_scan(a, b, h0, reverse)


def rglru_mixer(Pl, Pc, conv_w, conv_b, w_r, b_r, w_i, b_i, lam, out_norm, ctx_out):
    f32 = jnp.float32
    ul = dwconv(Pl[..., B_X:B_X + GROUP], conv_w, conv_b, LRU_CONV_LEFT).astype(f32)
    uc = dwconv(Pc[..., B_X:B_X + GROUP], conv_w, conv_b, LRU_CONV_LEFT).astype(f32)
    h0 = jnp.zeros((Pc.shape[0], GROUP), f32)
    hl = 0.0
    hc = 0.0
    for d in range(2):
        rev = d == 1
        h_c, s_c = rglru_direction(uc, w_r[d], b_r[d], w_i[d], b_i[d], lam[d], h0, rev)
        h_l, _ = rglru_direction(ul, w_r[d], b_r[d], w_i[d], b_i[d], lam[d], s_c, rev)
        hl = hl + h_l
        hc = hc + h_c
    gl = jax.nn.gelu(Pl[..., B_GATE:B_GATE + GROUP].astype(f32))
    yl = rms_norm(hl * gl, out_norm).astype(Pl.dtype)
    yc = None
    if ctx_out:
        gc = jax.nn.gelu(Pc[..., B_GATE:B_GATE + GROUP].astype(f32))
        yc = rms_norm(hc * gc, out_norm).astype(Pc.dtype)
    return yl, yc


def rwkv_prepare(P, mu_prev, mu_next, w0, w_up, a0, a_up, k_k, k_a):
    z = token_shift(P[..., C_OFF:C_OFF + C_COLS], mu_prev, mu_next).astype(jnp.float32)
    B, L, _ = z.shape

    def heads(t):
        return t.reshape(B, L, RWKV_HEADS, RWKV_HEAD)

    r = heads(z[..., C_R:C_R + GROUP])
    k = heads(z[..., C_K:C_K + GROUP])
    v = heads(z[..., C_V:C_V + GROUP])
    kk = k * k_k.reshape(RWKV_HEADS, RWKV_HEAD)
    kk = kk * lax.rsqrt(jnp.maximum(jnp.sum(jnp.square(kk), -1, keepdims=True), 1e-24))
    dirs = []
    for d in range(2):
        wd = z[..., C_WD + d * RWKV_DECAY_LORA:C_WD + (d + 1) * RWKV_DECAY_LORA]
        ad = z[..., C_AD + d * RWKV_AAA_LORA:C_AD + (d + 1) * RWKV_AAA_LORA]
        log_w = -jnp.exp(-jax.nn.softplus(-(w0[d] + jnp.tanh(wd) @ w_up[d])) - 0.5)
        a = heads(jax.nn.sigmoid(a0[d] + ad @ a_up[d]))
        k_d = k * (1.0 + (a - 1.0) * k_a.reshape(RWKV_HEADS, RWKV_HEAD))
        dirs.append((heads(jnp.exp(log_w)), kk * a, k_d))
    gd = z[..., C_GD:C_GD + RWKV_GATE_LORA]
    return r, v, kk, dirs, gd


def rwkv_scan(r, decay, kk, kka, v, k, s0, reverse, emit):
    xs = tuple(jnp.moveaxis(t, 1, 0) for t in (r, decay, kk, kka, v, k))

    def step(S, inp):
        r_t, w_t, kk_t, kka_t, v_t, k_t = inp
        sa = jnp.einsum('bhvk,bhk->bhv', S, kk_t)
        S = S * w_t[:, :, None, :] - sa[..., None] * kka_t[:, :, None, :] + v_t[..., None] * k_t[:, :, None, :]
        return S, (jnp.einsum('bhvk,bhk->bhv', S, r_t) if emit else None)

    S, ys = lax.scan(step, s0, xs, reverse=reverse)
    return (jnp.moveaxis(ys, 0, 1) if emit else None), S


def rwkv_finish(y, r, v, dirs, gd, g_up, r_k, ln_g, ln_b):
    B, L = y.shape[:2]
    mu = jnp.mean(y, -1, keepdims=True)
    var = jnp.mean(jnp.square(y - mu), -1, keepdims=True)
    yn = (y - mu) * lax.rsqrt(var + RWKV_GN_EPS) * ln_g.reshape(RWKV_HEADS, RWKV_HEAD) + ln_b.reshape(RWKV_HEADS, RWKV_HEAD)
    bonus = sum(jnp.sum(r * kd * r_k, -1, keepdims=True) for (_, _, kd) in dirs) * v
    g = jax.nn.sigmoid(gd) @ g_up
    return (yn + bonus).reshape(B, L, GROUP) * g


def rwkv_mixer(Pl, Pc, mu_prev, mu_next, w0, w_up, a0, a_up, g_up, k_k, k_a, r_k, ln_g, ln_b, ctx_out):
    rl, vl, kkl, dl, gdl = rwkv_prepare(Pl, mu_prev, mu_next, w0, w_up, a0, a_up, k_k, k_a)
    rc, vc, kkc, dc, gdc = rwkv_prepare(Pc, mu_prev, mu_next, w0, w_up, a0, a_up, k_k, k_a)
    s0 = jnp.zeros((Pl.shape[0], RWKV_HEADS, RWKV_HEAD, RWKV_HEAD), jnp.float32)
    yl = 0.0
    yc = 0.0
    for d in range(2):
        rev = d == 1
        y_c, s_c = rwkv_scan(rc, dc[d][0], kkc, dc[d][1], vc, dc[d][2], s0, rev, ctx_out)
        y_l, _ = rwkv_scan(rl, dl[d][0], kkl, dl[d][1], vl, dl[d][2], s_c, rev, True)
        yl = yl + y_l
        if ctx_out:
            yc = yc + y_c
    out_l = rwkv_finish(yl, rl, vl, dl, gdl, g_up, r_k, ln_g, ln_b).astype(Pl.dtype)
    out_c = None
    if ctx_out:
        out_c = rwkv_finish(yc, rc, vc, dc, gdc, g_up, r_k, ln_g, ln_b).astype(Pc.dtype)
    return out_l, out_c


def hyena_filters(L, w1, b1, w2, b2, w3):
    f32 = jnp.float32
    t01 = jnp.linspace(0.0, 1.0, L, dtype=f32)[:, None]
    bands = jnp.linspace(1e-4, HY_BANDS - 1, HY_BANDS, dtype=f32)[None, :]
    wpos = (2.0 * math.pi / L) * jnp.arange(L, dtype=f32)[:, None]
    z = jnp.concatenate([t01, jnp.cos(bands * wpos), -jnp.sin(bands * wpos)], -1)
    h = jnp.sin(HY_SIN_FREQ * (z @ w1 + b1))
    h = jnp.sin(HY_SIN_FREQ * (h @ w2 + b2))
    h = (h @ w3).astype(f32).reshape(L, HY_ORDER, 2, GROUP)
    deltas = jnp.abs(jnp.linspace(HY_DECAY_MIN, HY_DECAY_MAX, GROUP, dtype=f32))
    window = jnp.exp(-t01 * deltas) + HY_SHIFT
    return h * window[:, None, None, :]


def two_sided_spectrum(h_fwd, h_bwd):
    L, C = h_fwd.shape
    k = jnp.concatenate([h_fwd, jnp.zeros((1, C), h_fwd.dtype), jnp.flip(h_bwd[1:], 0)], 0)
    return jnp.fft.rfft(k, axis=0)


def long_conv(u, spec, d_skip):
    L = u.shape[1]
    U = jnp.fft.rfft(u, n=2 * L, axis=1)
    y = jnp.fft.irfft(U * spec[None], n=2 * L, axis=1)[:, :L]
    return y + u * d_skip


def hyena_sequence(P, conv_w, conv_b, w1, b1, w2, b2, w3, d_skip):
    L = P.shape[1]
    z = dwconv(P[..., D_OFF:D_OFF + D_COLS], conv_w, conv_b, 1).astype(jnp.float32)
    v, x1, x2 = z[..., :GROUP], z[..., GROUP:2 * GROUP], z[..., 2 * GROUP:]
    h = hyena_filters(L, w1, b1, w2, b2, w3)
    u = x1 * long_conv(v, two_sided_spectrum(h[:, 0, 0], h[:, 0, 1]), d_skip[0])
    return x2 * long_conv(u, two_sided_spectrum(h[:, 1, 0], h[:, 1, 1]), d_skip[1])


def swiglu(x, wg, wu, wd):
    return (jax.nn.silu(x @ wg) * (x @ wu)) @ wd


def moe_swiglu(x, router, wg, wu, wd):
    N, D = x.shape
    logits = (x @ router).astype(jnp.float32)
    top_v, top_i = lax.top_k(logits, TOP_K)
    gates = jax.nn.softmax(top_v, axis=-1)
    A = N * TOP_K
    e_flat = top_i.reshape(-1)
    tok_flat = jnp.arange(A, dtype=jnp.int32) // TOP_K
    g_flat = gates.reshape(-1)
    order = jnp.argsort(e_flat)
    e_sorted = e_flat[order]
    counts = jnp.bincount(e_flat, length=N_EXPERTS)
    starts = jnp.cumsum(counts) - counts
    padded = (counts + MOE_BLOCK - 1) // MOE_BLOCK * MOE_BLOCK
    pends = jnp.cumsum(padded)
    pstarts = pends - padded
    dest = pstarts[e_sorted] + jnp.arange(A, dtype=jnp.int32) - starts[e_sorted]
    n_blocks = -(-A // MOE_BLOCK) + N_EXPERTS
    n_slots = n_blocks * MOE_BLOCK
    slot_tok = jnp.full((n_slots,), N, jnp.int32).at[dest].set(tok_flat[order])
    slot_gate = jnp.zeros((n_slots,), jnp.float32).at[dest].set(g_flat[order])
    block_expert = jnp.clip(jnp.searchsorted(pends, jnp.arange(n_blocks) * MOE_BLOCK, side='right'), 0, N_EXPERTS - 1)
    xp = jnp.concatenate([x, jnp.zeros((1, D), x.dtype)], 0)
    xs = xp[slot_tok].reshape(n_blocks, MOE_BLOCK, D)

    def expert_block(args):
        xb, e = args
        return swiglu(xb, wg[e], wu[e], wd[e])

    ys = lax.map(expert_block, (xs, block_expert)).reshape(n_slots, D)
    out = jnp.zeros((N + 1, D), jnp.float32).at[slot_tok].add(ys.astype(jnp.float32) * slot_gate[:, None])
    return out[:N].astype(x.dtype)


def setup_inputs(seed: int = 0) -> dict:
    key = jax.random.key(seed)
    keys = iter(jax.random.split(key, 80))
    f32 = jnp.float32

    def nrm(shape, std):
        return std * jax.random.normal(next(keys), shape, f32)

    def gain(shape):
        return 1.0 + nrm(shape, 0.02)

    def unif(shape, lo, hi):
        return jax.random.uniform(next(keys), shape, f32, lo, hi)

    n_dense = (DEPTH + 1) // 2
    n_moe = DEPTH // 2
    lru_s = unif((DEPTH, 2, GROUP), 0.9, 0.999) ** (1.0 / LRU_C)
    decay_base = jnp.linspace(-6.5, -1.5, GROUP, dtype=f32)
    return {
        'x': nrm((BATCH, SEQ, D_MODEL), 1.0),
        'c': nrm((BATCH, D_MODEL), 1.0),
        'ctx': nrm((BATCH, CTX_LEN, D_MODEL), 1.0),
        'c_ctx': nrm((D_MODEL,), 1.0),
        'ada_w': nrm((DEPTH, D_MODEL, 6 * D_MODEL), 0.5 * D_MODEL ** -0.5),
        'ada_b': nrm((DEPTH, 6 * D_MODEL), 0.02),
        'w_in': nrm((DEPTH, D_MODEL, N_IN), D_MODEL ** -0.5),
        'mla_q_norm': gain((DEPTH, MLA_Q_RANK)),
        'mla_kv_norm': gain((DEPTH, MLA_KV_RANK)),
        'mla_w_uq': nrm((DEPTH, MLA_Q_RANK, MLA_HEADS * (MLA_NOPE + MLA_ROPE)), MLA_Q_RANK ** -0.5),
        'mla_w_ukv': nrm((DEPTH, MLA_KV_RANK, MLA_HEADS * (MLA_NOPE + MLA_V)), MLA_KV_RANK ** -0.5),
        'mla_out_norm': gain((DEPTH, GROUP)),
        'lru_conv_w': nrm((DEPTH, LRU_CONV, GROUP), LRU_CONV ** -0.5),
        'lru_conv_b': nrm((DEPTH, GROUP), 0.02),
        'lru_w_r': nrm((DEPTH, 2, LRU_BLOCKS, GROUP // LRU_BLOCKS, GROUP // LRU_BLOCKS), (GROUP // LRU_BLOCKS) ** -0.5),
        'lru_b_r': nrm((DEPTH, 2, GROUP), 0.02),
        'lru_w_i': nrm((DEPTH, 2, LRU_BLOCKS, GROUP // LRU_BLOCKS, GROUP // LRU_BLOCKS), (GROUP // LRU_BLOCKS) ** -0.5),
        'lru_b_i': nrm((DEPTH, 2, GROUP), 0.02),
        'lru_lambda': jnp.log(lru_s) - jnp.log1p(-lru_s),
        'lru_out_norm': gain((DEPTH, GROUP)),
        'rwkv_mu_prev': unif((DEPTH, C_COLS), 0.0, 0.5),
        'rwkv_mu_next': unif((DEPTH, C_COLS), 0.0, 0.5),
        'rwkv_w0': decay_base + nrm((DEPTH, 2, GROUP), 0.1),
        'rwkv_w_up': nrm((DEPTH, 2, RWKV_DECAY_LORA, GROUP), 0.5 * RWKV_DECAY_LORA ** -0.5),
        'rwkv_a0': nrm((DEPTH, 2, GROUP), 0.1),
        'rwkv_a_up': nrm((DEPTH, 2, RWKV_AAA_LORA, GROUP), 0.5 * RWKV_AAA_LORA ** -0.5),
        'rwkv_g_up': nrm((DEPTH, RWKV_GATE_LORA, GROUP), RWKV_GATE_LORA ** -0.5),
        'rwkv_k_k': 0.85 + nrm((DEPTH, GROUP), 0.02),
        'rwkv_k_a': gain((DEPTH, GROUP)),
        'rwkv_r_k': -0.04 + nrm((DEPTH, RWKV_HEADS, RWKV_HEAD), 0.02),
        'rwkv_ln_g': gain((DEPTH, GROUP)),
        'rwkv_ln_b': nrm((DEPTH, GROUP), 0.02),
        'hy_conv_w': nrm((DEPTH, HY_SHORT, D_COLS), HY_SHORT ** -0.5),
        'hy_conv_b': nrm((DEPTH, D_COLS), 0.02),
        'hy_f_w1': nrm((DEPTH, HY_EMB, HY_HIDDEN), HY_EMB ** -0.5),
        'hy_f_b1': nrm((DEPTH, HY_HIDDEN), 0.02),
        'hy_f_w2': nrm((DEPTH, HY_HIDDEN, HY_HIDDEN), HY_HIDDEN ** -0.5),
        'hy_f_b2': nrm((DEPTH, HY_HIDDEN), 0.02),
        'hy_f_w3': nrm((DEPTH, HY_HIDDEN, HY_ORDER * 2 * GROUP), 0.02),
        'hy_d': nrm((DEPTH, HY_ORDER, GROUP), 0.1),
        'hy_out_norm': gain((DEPTH, GROUP)),
        'w_out': nrm((DEPTH, D_MIX, D_MODEL), BETA * D_MIX ** -0.5),
        'ln1_g': gain((DEPTH, D_MODEL)),
        'ln1_b': nrm((DEPTH, D_MODEL), 0.02),
        'ln2_g': gain((DEPTH, D_MODEL)),
        'ln2_b': nrm((DEPTH, D_MODEL), 0.02),
        'ffn_w_gate': nrm((n_dense, D_MODEL, FF_DENSE), D_MODEL ** -0.5),
        'ffn_w_up': nrm((n_dense, D_MODEL, FF_DENSE), D_MODEL ** -0.5),
        'ffn_w_down': nrm((n_dense, FF_DENSE, D_MODEL), BETA * FF_DENSE ** -0.5),
        'moe_router': nrm((n_moe, D_MODEL, N_EXPERTS), D_MODEL ** -0.5),
        'moe_w_gate': nrm((n_moe, N_EXPERTS, D_MODEL, FF_EXPERT), D_MODEL ** -0.5),
        'moe_w_up': nrm((n_moe, N_EXPERTS, D_MODEL, FF_EXPERT), D_MODEL ** -0.5),
        'moe_w_down': nrm((n_moe, N_EXPERTS, FF_EXPERT, D_MODEL), BETA * FF_EXPERT ** -0.5),
    }


def reference(x, c, ctx, c_ctx, ada_w, ada_b, w_in, mla_q_norm, mla_kv_norm, mla_w_uq, mla_w_ukv, mla_out_norm,
              lru_conv_w, lru_conv_b, lru_w_r, lru_b_r, lru_w_i, lru_b_i, lru_lambda, lru_out_norm,
              rwkv_mu_prev, rwkv_mu_next, rwkv_w0, rwkv_w_up, rwkv_a0, rwkv_a_up, rwkv_g_up, rwkv_k_k, rwkv_k_a,
              rwkv_r_k, rwkv_ln_g, rwkv_ln_b, hy_conv_w, hy_conv_b, hy_f_w1, hy_f_b1, hy_f_w2, hy_f_b2, hy_f_w3,
              hy_d, hy_out_norm, w_out, ln1_g, ln1_b, ln2_g, ln2_b, ffn_w_gate, ffn_w_up, ffn_w_down,
              moe_router, moe_w_gate, moe_w_up, moe_w_down):
    B, L, D = x.shape
    Lc = ctx.shape[1]
    rows = L // GRID_W
    cos, sin = axial_rope(rows)
    s_lat = jax.nn.silu(c)
    s_ctx = jax.nn.silu(c_ctx)
    xl, xc = x, ctx
    for li in range(DEPTH):
        ctx_out = li < DEPTH - 1
        mod_l = (s_lat @ ada_w[li] + ada_b[li]).reshape(B, 6, 1, D)
        mod_c = (s_ctx @ ada_w[li] + ada_b[li]).reshape(6, 1, D)

        hl = modulate(xl, mod_l[:, 0], mod_l[:, 1])
        hc = modulate(xc, mod_c[0], mod_c[1])
        w_in_l = w_in[li]
        Pl = hl @ w_in_l
        Pc = hc @ (w_in_l if ctx_out else w_in_l[:, :D_OFF])
        a_l, a_c = mla_mixer(Pl, Pc, mla_q_norm[li], mla_kv_norm[li], mla_w_uq[li], mla_w_ukv[li],
                             mla_out_norm[li], cos, sin, ctx_out)
        b_l, b_c = rglru_mixer(Pl, Pc, lru_conv_w[li], lru_conv_b[li], lru_w_r[li], lru_b_r[li], lru_w_i[li],
                               lru_b_i[li], lru_lambda[li], lru_out_norm[li], ctx_out)
        c_l, c_c = rwkv_mixer(Pl, Pc, rwkv_mu_prev[li], rwkv_mu_next[li], rwkv_w0[li], rwkv_w_up[li], rwkv_a0[li],
                              rwkv_a_up[li], rwkv_g_up[li], rwkv_k_k[li], rwkv_k_a[li], rwkv_r_k[li],
                              rwkv_ln_g[li], rwkv_ln_b[li], ctx_out)
        d_l = rms_norm(hyena_sequence(Pl, hy_conv_w[li], hy_conv_b[li], hy_f_w1[li], hy_f_b1[li], hy_f_w2[li],
                                      hy_f_b2[li], hy_f_w3[li], hy_d[li]), hy_out_norm[li]).astype(Pl.dtype)
        yl = jnp.concatenate([a_l, b_l, c_l, d_l], -1) @ w_out[li]
        xl = layer_norm(ALPHA * xl + mod_l[:, 2] * yl, ln1_g[li], ln1_b[li])
        if ctx_out:
            d_c = rms_norm(hyena_sequence(Pc, hy_conv_w[li], hy_conv_b[li], hy_f_w1[li], hy_f_b1[li], hy_f_w2[li],
                                          hy_f_b2[li], hy_f_w3[li], hy_d[li]), hy_out_norm[li]).astype(Pc.dtype)
            yc = jnp.concatenate([a_c, b_c, c_c, d_c], -1) @ w_out[li]
            xc = layer_norm(ALPHA * xc + mod_c[2] * yc, ln1_g[li], ln1_b[li])

        fl = modulate(xl, mod_l[:, 3], mod_l[:, 4]).reshape(B * L, D)
        if ctx_out:
            fc = modulate(xc, mod_c[3], mod_c[4]).reshape(B * Lc, D)
            tokens = jnp.concatenate([fl, fc], 0)
        else:
            tokens = fl
        j = li // 2
        if li % 2 == 0:
            out = swiglu(tokens, ffn_w_gate[j], ffn_w_up[j], ffn_w_down[j])
        else:
            out = moe_swiglu(tokens, moe_router[j], moe_w_gate[j], moe_w_up[j], moe_w_down[j])
        xl = layer_norm(ALPHA * xl + mod_l[:, 5] * out[:B * L].reshape(B, L, D), ln2_g[li], ln2_b[li])
        if ctx_out:
            xc = layer_norm(ALPHA * xc + mod_c[5] * out[B * L:].reshape(B, Lc, D), ln2_g[li], ln2_b[li])
    return xl
```

```python
import math
import numpy as np
import ml_dtypes
from contextlib import ExitStack
import concourse.bass as bass
import concourse.mybir as mybir
from concourse.bass_utils import run_bass_kernel_spmd

F32 = mybir.dt.float32
BF16 = mybir.dt.bfloat16
AF = mybir.ActivationFunctionType
ALU = mybir.AluOpType
AX = mybir.AxisListType

NCORES = 8
D = 1024
DEPTH = 4
LC = 256
LL = 4096
TT = LC + LL
NB = 2
NTOK = NB * TT
GROUP = 256
ALPHA = (2.0 * DEPTH) ** 0.25
FF_DENSE = 2816
FF_EXPERT = 3584
NEXP = 8


class Res:
    __slots__ = ("w", "rd", "name")

    def __init__(self, name=""):
        self.w = None
        self.rd = {}
        self.name = name


class T:
    def __init__(self, t, name=""):
        self.t = t
        self.res = Res(name)

    def __getitem__(self, idx):
        return self.t[idx]


class K:
    ENGS = ("pe", "dve", "act", "pool", "sp")

    def __init__(self, nc, n_dma_sems=16):
        self.nc = nc
        self.es = ExitStack()
        self.ops = {e: [] for e in self.ENGS}
        self.sem = {e: nc.alloc_semaphore(name=f"sem_{e}") for e in ("pe", "dve", "act", "pool")}
        self.cnt = {e: 0 for e in self.sem}
        self.seen = {e: {} for e in self.ENGS}
        self.dsem, self.dcnt, self.dnext = {}, {}, {}
        for q in ("sp", "pool", "act"):
            n = n_dma_sems if q == "sp" else 8
            self.dsem[q] = [nc.alloc_semaphore(name=f"dsem_{q}{i}") for i in range(n)]
            self.dcnt[q] = [0] * n
            self.dnext[q] = 0
        self.n_inst = 0
        self.scopes = []

    def sb(self, name, shape, dt=F32):
        st = self.scopes[-1] if self.scopes else self.es
        self.uid = getattr(self, "uid", 0) + 1
        name = f"{name}_{self.uid}"
        t = st.enter_context(self.nc.sbuf_tensor(name, list(shape), dt))
        return T(t, name)

    def csb(self, name, shape, dt=F32):
        c = self.__dict__.setdefault("_cache", {})
        if name not in c:
            c[name] = self.sb(name, shape, dt)
        return c[name]

    def ps(self, name, shape, dt=F32):
        t = self.es.enter_context(self.nc.psum_tensor(name, list(shape), dt))
        return T(t, name)

    def dram(self, name, shape, dt=F32, kind="Internal"):
        t = self.nc.dram_tensor(name, list(shape), dt, kind=kind)
        return T(t, name)

    def _collect(self, eng, reads, writes, skip_self_w=False):
        waits = {}

        def add(tok, is_w=False):
            if tok is None:
                return
            s, v, e = tok
            if skip_self_w and is_w and e == eng:
                return
            key = id(s)
            if self.seen[eng].get(key, 0) >= v:
                return
            if key not in waits or waits[key][1] < v:
                waits[key] = (s, v)

        for r in reads:
            r = r.res if isinstance(r, T) else r
            add(r.w)
        for w in writes:
            w = w.res if isinstance(w, T) else w
            add(w.w, True)
            for t in w.rd.values():
                add(t)
        out = list(waits.values())
        for s, v in out:
            self.seen[eng][id(s)] = v
        return out

    def _update(self, tok, reads, writes):
        for r in reads:
            r = r.res if isinstance(r, T) else r
            r.rd[id(tok[0])] = tok
        for w in writes:
            w = w.res if isinstance(w, T) else w
            w.w = tok
            w.rd = {}

    def op(self, eng, fn, reads=(), writes=(), acc=False):
        waits = self._collect(eng, reads, writes, skip_self_w=acc)
        self.cnt[eng] += 1
        sem = self.sem[eng]
        tok = (sem, self.cnt[eng], eng)

        def thunk(e):
            for s, v in waits:
                e.wait_ge(s, v)
            fn(e).then_inc(sem, 1)

        self.ops[eng].append(thunk)
        self._update(tok, reads, writes)
        self.n_inst += 1

    def do(self, eng, method, outs, ins, acc=False, **kw):
        aps = {n: v[1] for n, v in outs.items()}
        aps.update({n: v[1] for n, v in ins.items()})
        aps.update(kw)
        self.op(eng, lambda e: getattr(e, method)(**aps), reads=[v[0] for v in ins.values()],
                writes=[v[0] for v in outs.values()], acc=acc)

    def dma(self, q, out, in_, reads=(), writes=(), **kw):
        i = self.dnext[q]
        self.dnext[q] = (i + 1) % len(self.dsem[q])
        sem = self.dsem[q][i]
        waits = self._collect(q, reads, writes)
        prev = self.dcnt[q][i]
        if prev > 0 and self.seen[q].get(id(sem), 0) < prev:
            waits.append((sem, prev))
            self.seen[q][id(sem)] = prev
        self.dcnt[q][i] = prev + 16
        tok = (sem, prev + 16, "dma_" + q)

        def thunk(e):
            for s, v in waits:
                e.wait_ge(s, v)
            e.dma_start(out=out, in_=in_, **kw).then_inc(sem, 16)

        self.ops[q].append(thunk)
        self._update(tok, reads, writes)
        self.n_inst += 1

    def barrier(self):
        targets = [(self.sem[e], self.cnt[e]) for e in self.sem if self.cnt[e] > 0]
        for q in self.dsem:
            for s, c in zip(self.dsem[q], self.dcnt[q]):
                if c > 0:
                    targets.append((s, c))
        for eng in self.ENGS:
            ws = []
            for s, v in targets:
                if self.seen[eng].get(id(s), 0) < v:
                    ws.append((s, v))
                    self.seen[eng][id(s)] = v

            def thunk(e, ws=ws):
                for s, v in ws:
                    e.wait_ge(s, v)

            self.ops[eng].append(thunk)

    def phase(self):
        k = self

        class _P:
            def __enter__(self_):
                st = ExitStack()
                k.scopes.append(st)
                return st

            def __exit__(self_, *a):
                k.barrier()
                k.__dict__["_cache"] = {}
                st = k.scopes.pop()
                st.close()
                return False

        return _P()

    def finish(self):
        self.barrier()
        nc = self.nc
        with nc.Block() as block:
            @block.sync
            def _(e):
                for th in self.ops["sp"]:
                    th(e)

            @block.tensor
            def _(e):
                for th in self.ops["pe"]:
                    th(e)

            @block.vector
            def _(e):
                for th in self.ops["dve"]:
                    th(e)

            @block.scalar
            def _(e):
                for th in self.ops["act"]:
                    th(e)

            @block.gpsimd
            def _(e):
                for th in self.ops["pool"]:
                    th(e)
        self.es.close()


A_CQ, A_CKV, A_KR, B_X, B_GATE, C_OFF, D_OFF, N_IN = 0, 192, 320, 352, 608, 864, 1824, 2592
NPC = 23
PC_CQ0, PC_CQ1, PC_CKV, PC_KR, PC_KRS, PC_LX, PC_LG, PC_R, PC_KK, PC_V, PC_LORA, PC_GD, PC_HY = \
    0, 1, 2, 3, 4, 5, 7, 9, 11, 13, 15, 16, 17


def _pcols():
    cols = -np.ones(NPC * 128, np.int64)

    def put(chunk, off, idx):
        idx = np.asarray(idx)
        cols[chunk * 128 + off: chunk * 128 + off + len(idx)] = idx

    put(0, 0, np.arange(0, 128))
    put(1, 0, np.arange(128, 192))
    put(2, 0, np.arange(192, 320))
    kr = np.arange(320, 352)
    put(3, 64, kr)
    put(4, 64, np.concatenate([kr[16:], kr[:16]]))
    put(5, 0, np.arange(352, 608))
    put(7, 0, np.arange(608, 864))
    put(9, 0, np.arange(864, 1632))
    put(15, 0, np.arange(1632, 1760))
    put(16, 0, np.arange(1760, 1824))
    put(17, 0, np.arange(1824, 2592))
    return cols


def _gather_cols(w, cols):
    out = np.zeros(w.shape[:-1] + (len(cols),), w.dtype)
    m = cols >= 0
    out[..., m] = w[..., cols[m]]
    return out


def _chunks(v, n=None):
    v = np.asarray(v, np.float32).reshape(-1)
    c = (len(v) + 127) // 128
    o = np.zeros(c * 128, np.float32)
    o[:len(v)] = v
    return o.reshape(c, 128).T


SEGS = [(0, LC, 2), (LC, TT, 0), (TT, TT + LC, 2), (TT + LC, 2 * TT, 1)]


def segs_in(t0, t1):
    for a, b, col in SEGS:
        lo, hi = max(a, t0), min(b, t1)
        if lo < hi:
            yield lo - t0, hi - t0, col


class G:
    pass


def ev(k, i, out, in_):
    if i % 2 == 0:
        k.do("act", "copy", dict(out=out), dict(in_=in_))
    else:
        k.do("dve", "tensor_copy", dict(out=out), dict(in_=in_))


def load_w_bf16(k, dst, dst_ap_fn, src_T, src_ap_fn, n, shape, name, qs=("sp",)):
    stg = [k.sb(f"{name}_stg{j}", shape, F32) for j in range(2)]
    for i in range(n):
        s = stg[i % 2]
        k.dma(qs[i % len(qs)], s[:], src_ap_fn(i), reads=[src_T], writes=[s])
        if i % 2 == 0:
            k.do("pool", "tensor_copy", dict(out=(dst, dst_ap_fn(i))), dict(in_=(s, s[:])))
        else:
            k.do("dve", "tensor_copy", dict(out=(dst, dst_ap_fn(i))), dict(in_=(s, s[:])))


def phase_mod(g, li):
    k = g.k
    with k.phase():
        wst = [k.sb(f"adaw{j}", [128, 8, 1024], F32) for j in range(2)]
        for grp in range(6):
            w = wst[grp % 2]
            k.dma("sp" if grp % 2 == 0 else "act", w[:],
                  g.ada_w[li, :, grp * 1024:(grp + 1) * 1024].rearrange("(c p) n -> p c n", p=128),
                  reads=[g.ada_w], writes=[w])
            for jj in range(8):
                j = grp * 8 + jj
                ps = g.PS[j % 4]
                for kc in range(8):
                    k.do("pe", "matmul", dict(out=(ps, ps[:, 0:3])),
                         dict(lhsT=(w, w[:, kc, jj * 128:(jj + 1) * 128]), rhs=(g.sT, g.sT[:, kc, :])),
                         acc=(kc > 0), start=(kc == 0), stop=(kc == 7))
                k.do("act", "activation", dict(out=(g.MOD, g.MOD[:, j, :])),
                     dict(in_=(ps, ps[:, 0:3]), bias=(g.PV, g.PV[:, g.pv["ada_b"] + j:g.pv["ada_b"] + j + 1])),
                     func=AF.Identity, scale=1.0)
        for grp in (1, 4):
            k.do("dve", "tensor_scalar_add", dict(out=(g.MOD1, g.MOD1[:, grp * 8:(grp + 1) * 8, :])),
                 dict(in0=(g.MOD, g.MOD[:, grp * 8:(grp + 1) * 8, :])), scalar1=1.0)


def modulate_block(g, xt, hb, t0, t1, gs, gb):
    k = g.k
    i = 0
    for a, b, col in segs_in(t0, t1):
        for c in range(8):
            sc = (g.MOD1, g.MOD1[:, gs * 8 + c, col:col + 1])
            bi = (g.MOD, g.MOD[:, gb * 8 + c, col:col + 1])
            if i % 2 == 0:
                k.do("act", "activation", dict(out=(hb, hb[:, c, a:b])),
                     dict(in_=(xt, xt[:, c, a:b]), scale=sc, bias=bi), func=AF.Identity)
            else:
                k.do("dve", "tensor_scalar", dict(out=(hb, hb[:, c, a:b])),
                     dict(in0=(xt, xt[:, c, a:b]), scalar1=sc, scalar2=bi), op0=ALU.mult, op1=ALU.add)
            i += 1


def phase_p(g, li):
    k = g.k
    with k.phase():
        win = k.sb("win", [128, 8, NPC * 128], BF16)
        load_w_bf16(k, win, lambda i: win[:, i, :], g.w_in_p,
                    lambda i: g.w_in_p[li, i * 128:(i + 1) * 128, :], 8, [128, NPC * 128], "win", qs=("sp", "act"))
        xts = [k.sb(f"xt{j}", [128, 8, 512], F32) for j in range(2)]
        hbs = [k.sb(f"hb{j}", [128, 8, 512], BF16) for j in range(2)]
        stg = [k.sb(f"pstg{j}", [128, 512], F32) for j in range(4)]
        XTv = g.XT.t.rearrange("(c p) t -> p c t", p=128)
        n = 0
        for tb in range(NTOK // 512):
            t0, t1 = tb * 512, (tb + 1) * 512
            xt, hb = xts[tb % 2], hbs[tb % 2]
            k.dma("sp", xt[:], XTv[:, :, t0:t1], reads=[g.XT], writes=[xt])
            modulate_block(g, xt, hb, t0, t1, 1, 0)
            for pc in range(NPC):
                ps = g.PS[n % 8]
                for kc in range(8):
                    k.do("pe", "matmul", dict(out=(ps, ps[:])),
                         dict(lhsT=(win, win[:, kc, pc * 128:(pc + 1) * 128]), rhs=(hb, hb[:, kc, :])),
                         acc=(kc > 0), start=(kc == 0), stop=(kc == 7))
                s = stg[n % 4]
                ev(k, n, (s, s[:]), (ps, ps[:]))
                k.dma("sp" if n % 2 == 0 else "pool", g.PT[pc, :, t0:t1], s[:], reads=[s], writes=[g.PTr[pc]])
                n += 1


def pv_spec():
    spec = [("ada_b", 48), ("ln1_g", 8), ("ln1_b", 8), ("ln2_g", 8), ("ln2_b", 8),
            ("q_norm", 2), ("kv_norm", 1), ("mla_on", 4),
            ("lru_cw", 8), ("lru_cb", 2), ("lru_br", 4), ("lru_bi", 4), ("lru_lam", 4), ("lru_on", 2),
            ("mu_prev", 8), ("mu_next", 8), ("rw_lng", 2), ("rw_lnb", 2), ("rw_rk", 2), ("rw_ka", 2),
            ("rw_a0", 4), ("rw_w0", 4), ("rw_kk", 2),
            ("hy_cw", 18), ("hy_cb", 6), ("hy_b1", 1), ("hy_b2", 1), ("hy_on", 2), ("hy_d", 4)]
    pv, off = {}, 0
    for n, c in spec:
        pv[n] = off
        off += c
    return pv, off


def make_pv(inp, li):
    pv, n = pv_spec()
    out = np.zeros((128, n), np.float32)

    def put(name, arr):
        arr = np.asarray(arr, np.float32)
        out[:, pv[name]:pv[name] + arr.shape[1]] = arr

    put("ada_b", _chunks(inp["ada_b"][li]))
    for nm in ("ln1_g", "ln1_b", "ln2_g", "ln2_b"):
        put(nm, _chunks(inp[nm][li]))
    put("q_norm", _chunks(inp["mla_q_norm"][li]))
    put("kv_norm", _chunks(inp["mla_kv_norm"][li]))
    on = np.zeros((128, 4), np.float32)
    on[:64, :] = inp["mla_out_norm"][li].reshape(4, 64).T
    put("mla_on", on)
    put("lru_cw", np.concatenate([_chunks(inp["lru_conv_w"][li][j]) for j in range(4)], 1))
    put("lru_cb", _chunks(inp["lru_conv_b"][li]))
    put("lru_br", np.concatenate([_chunks(inp["lru_b_r"][li][d]) for d in range(2)], 1))
    put("lru_bi", np.concatenate([_chunks(inp["lru_b_i"][li][d]) for d in range(2)], 1))
    put("lru_lam", np.concatenate([_chunks(inp["lru_lambda"][li][d]) for d in range(2)], 1))
    put("lru_on", _chunks(inp["lru_out_norm"][li]))
    mp = np.zeros(8 * 128, np.float32); mn = np.zeros(8 * 128, np.float32)
    mp[:960] = inp["rwkv_mu_prev"][li]; mn[:960] = inp["rwkv_mu_next"][li]
    put("mu_prev", mp.reshape(8, 128).T); put("mu_next", mn.reshape(8, 128).T)
    put("rw_lng", _chunks(inp["rwkv_ln_g"][li])); put("rw_lnb", _chunks(inp["rwkv_ln_b"][li]))
    put("rw_rk", _chunks(inp["rwkv_r_k"][li].reshape(-1))); put("rw_ka", _chunks(inp["rwkv_k_a"][li]))
    put("rw_a0", np.concatenate([_chunks(inp["rwkv_a0"][li][d]) for d in range(2)], 1))
    put("rw_w0", np.concatenate([_chunks(inp["rwkv_w0"][li][d]) for d in range(2)], 1))
    put("rw_kk", _chunks(inp["rwkv_k_k"][li]))
    put("hy_cw", np.concatenate([_chunks(inp["hy_conv_w"][li][j]) for j in range(3)], 1))
    put("hy_cb", _chunks(inp["hy_conv_b"][li]))
    b1 = np.zeros((128, 1), np.float32); b1[:64, 0] = inp["hy_f_b1"][li]
    b2 = np.zeros((128, 1), np.float32); b2[:64, 0] = inp["hy_f_b2"][li]
    put("hy_b1", b1); put("hy_b2", b2)
    put("hy_on", _chunks(inp["hy_out_norm"][li]))
    put("hy_d", np.concatenate([_chunks(inp["hy_d"][li][o]) for o in range(2)], 1))
    return out


def build(layers, dbg=(), stages=("mod", "p", "mix", "lru", "mla", "hy", "rwkv", "wout", "ffn"), ym_in=False):
    nc = bass.Bass("TRN2", target_bir_lowering=False)
    k = K(nc)
    g = G()
    g.k, g.nc = k, nc
    g.pv, npv = pv_spec()

    def inp(name, shape, dt=F32):
        return k.dram(name, shape, dt, kind="ExternalInput")

    g.XT0 = inp("xT0", [D, NTOK])
    g.cT = inp("cT", [128, 8, 3])
    g.ada_w = inp("ada_w", [DEPTH, D, 6 * D])
    g.w_in_p = inp("w_in_p", [DEPTH, D, NPC * 128])
    g.PVd = inp("pvd", [DEPTH, 128, npv])
    g.w_out = inp("w_out", [DEPTH, D, D])
    g.ffn_wg = inp("ffn_wg_t", [2, FF_DENSE // 128, 128, D])
    g.ffn_wu = inp("ffn_wu_t", [2, FF_DENSE // 128, 128, D])
    g.ffn_wd = inp("ffn_w_down", [2, FF_DENSE, D])
    g.moe_router = inp("moe_router", [2, D, NEXP])
    g.moe_wg = inp("moe_wg_t", [2, NEXP, FF_EXPERT // 128, 128, D])
    g.moe_wu = inp("moe_wu_t", [2, NEXP, FF_EXPERT // 128, 128, D])
    g.moe_wd = inp("moe_w_down", [2, NEXP, FF_EXPERT, D])
    g.cst = inp("cst", [128, 128 * 3 + NEXP * 128])
    declare_mixer_inputs(g, inp)
    g.OUT = k.dram("outT", [D, NTOK], F32, kind="ExternalOutput")
    g.XT = g.OUT
    g.PT = k.dram("PT", [NPC, 128, NTOK], F32)
    g.PTr = [Res(f"PT{i}") for i in range(NPC)]
    g.YM = k.dram("YM", [D, NTOK], F32)
    g.PS = [k.ps(f"ps{i}", [128, 512], F32) for i in range(8)]
    g.MOD = k.sb("MOD", [128, 48, 3], F32)
    g.MOD1 = k.sb("MOD1", [128, 48, 3], F32)
    g.sT = k.sb("sT", [128, 8, 3], F32)
    g.PV = k.sb("PV", [128, npv], F32)
    g.CST = k.sb("CST", [128, 128 * 3 + NEXP * 128], F32)
    g.ONESD = T(g.CST.t[:, 0:128]); g.ONESD.res = g.CST.res
    g.IDENT = T(g.CST.t[:, 128:256]); g.IDENT.res = g.CST.res
    g.SEL = T(g.CST.t[:, 256:256 + NEXP * 128]); g.SEL.res = g.CST.res
    g.ONES1 = T(g.CST.t[:, 256 + NEXP * 128:384 + NEXP * 128]); g.ONES1.res = g.CST.res

    k.dma("sp", g.CST[:], g.cst[:], reads=[g.cst], writes=[g.CST])
    k.dma("sp", g.XT[:, :], g.XT0[:, :], reads=[g.XT0], writes=[g.XT])
    k.dma("sp", g.sT[:], g.cT[:], reads=[g.cT], writes=[g.sT])
    k.do("act", "activation", dict(out=(g.sT, g.sT[:])), dict(in_=(g.sT, g.sT[:])), func=AF.Silu)
    if ym_in:
        ymi = inp("dbg_YMin", [D, NTOK])
        k.dma("sp", g.YM[:, :], ymi[:, :], reads=[ymi], writes=[g.YM])
    for li in layers:
        k.dma("sp", g.PV[:], g.PVd[li], reads=[g.PVd], writes=[g.PV])
        if "mod" in stages:
            phase_mod(g, li)
        if "p" in stages:
            phase_p(g, li)
        if "mix" in stages:
            phase_mixers(g, li, stages)
        if "wout" in stages:
            phase_wout(g, li)
        if "ffn" in stages:
            phase_ffn(g, li)
    if "PT" in dbg:
        o = k.dram("dbg_PT", [NPC, 128, NTOK], F32, kind="ExternalOutput")
        k.dma("sp", o[:, :, :], g.PT[:, :, :], reads=list(g.PTr), writes=[o])
    if "YM" in dbg:
        o = k.dram("dbg_YM", [D, NTOK], F32, kind="ExternalOutput")
        k.dma("sp", o[:, :], g.YM[:, :], reads=[g.YM], writes=[o])
    for name, (t, shape) in g.dbgd.items():
        if name in dbg:
            o = k.dram("dbg_" + name, shape, F32, kind="ExternalOutput")
            k.dma("sp", o[:], t[:], reads=[t], writes=[o])
    k.finish()
    print("instructions:", k.n_inst)
    return nc


def declare_mixer_inputs(g, inp):
    g.dbgd = {}
    g.lru_w_r = inp("lru_w_r", [DEPTH, 2, 4, 64, 64])
    g.lru_w_i = inp("lru_w_i", [DEPTH, 2, 4, 64, 64])
    g.w_uq = inp("mla_w_uq", [DEPTH, 192, 384])
    g.w_uq_sw = inp("w_uq_sw", [DEPTH, 192, 384])
    g.w_uk_p = inp("w_uk_p", [DEPTH, 128, 384])
    g.w_uv = inp("w_uv", [DEPTH, 128, 256])
    g.rope = inp("rope", [128, 2, TT])
    g.rw_th = inp("rw_th", [128, 64 * 128], BF16)
    g.rw_f32c = inp("rw_f32c", [128, 512])
    g.rw_rep = inp("rw_rep", [DEPTH, 128, 6, 256])
    g.rw_lora = inp("rw_lora", [DEPTH, 2, 128, 256])
    g.rw_gup = inp("rwkv_g_up", [DEPTH, 64, 256])
    g.ZF = g.k.dram("ZF", [8, 128, NTOK], F32)
    g.OPD = g.k.dram("OPD", [4, NCH, 128, 512], BF16)
    g.GAM = g.k.dram("GAM", [NCH, 128, 512], F32)
    g.YS = g.k.dram("YS", [2, NB, 256, TT], F32)
    g.dbgd.update({"ZF": (g.ZF, [8, 128, NTOK]), "YS": (g.YS, [2, NB, 256, TT]), "GAM": (g.GAM, [NCH, 128, 512])})
    g.hy_zT = inp("hy_zT", [33, TT])
    g.hy_win = inp("hy_win", [TT, 256])
    g.dftC = inp("dftC", [4, LC // 128, 128, LC], BF16)
    g.dftL = inp("dftL", [4, LL // 128, 128, LL], BF16)
    g.hy_w1 = inp("hy_f_w1", [DEPTH, 33, 64])
    g.hy_w2 = inp("hy_f_w2", [DEPTH, 64, 64])
    g.hy_w3 = inp("hy_f_w3", [DEPTH, 64, 1024])
    g.hy_drep = inp("hy_drep", [DEPTH, 128, 2, 512])
    g.ZTOK = g.k.dram("ZTOK", [4, TT, NB, 256], F32)
    g.SPEC = g.k.dram("SPEC", [2, LL // 128, 128, 512], F32)
    g.k.HPI = g.k.sb("HPI", [128, 1], F32)
    g.k.do("pool", "memset", dict(ap=(g.k.HPI, g.k.HPI[:])), {}, constant=math.pi / 2)


def phase_mixers(g, li, stages):
    if "lru" in stages:
        phase_lru(g, li)
    if "mla" in stages:
        phase_mla(g, li)
    if "hy" in stages:
        phase_hyena(g, li)
    if "rwkv" in stages:
        phase_rwkv(g, li)


def make_cst():
    c = np.zeros((128, 384 + NEXP * 128), np.float32)
    c[:, 256 + NEXP * 128:] = 1.0
    c[:, 0:128] = 1.0 / D
    c[:, 128:256] = np.eye(128, dtype=np.float32)
    for e in range(NEXP):
        c[e, 256 + e * 128: 256 + (e + 1) * 128] = 1.0
    return c


def host_prep(inp):
    pc = _pcols()
    shared = {
        "ada_w": np.ascontiguousarray(inp["ada_w"], np.float32),
        "w_in_p": _gather_cols(np.asarray(inp["w_in"], np.float32), pc),
        "pvd": np.stack([make_pv(inp, li) for li in range(DEPTH)]),
        "cst": make_cst(),
    }
    wuq = np.asarray(inp["mla_w_uq"], np.float32)
    wukv = np.asarray(inp["mla_w_ukv"], np.float32)
    sw = np.zeros_like(wuq)
    ukp = np.zeros((DEPTH, 128, 384), np.float32)
    uv = np.zeros((DEPTH, 128, 256), np.float32)
    for h in range(4):
        sw[:, :, h * 96 + 64:h * 96 + 80] = wuq[:, :, h * 96 + 80:h * 96 + 96]
        sw[:, :, h * 96 + 80:h * 96 + 96] = wuq[:, :, h * 96 + 64:h * 96 + 80]
        ukp[:, :, h * 96:h * 96 + 64] = wukv[:, :, h * 128:h * 128 + 64]
        uv[:, :, h * 64:(h + 1) * 64] = wukv[:, :, h * 128 + 64:(h + 1) * 128]
    shared.update({"mla_w_uq": wuq, "w_uq_sw": sw, "w_uk_p": ukp, "w_uv": uv, "rope": rope_tables()})
    zT, win, dC, dL = hyena_consts()
    hd = np.asarray(inp["hy_d"], np.float32)
    drep = np.ascontiguousarray(np.broadcast_to(np.tile(hd, (1, 1, 2))[:, None], (DEPTH, 128, 2, 512)))
    shared.update({"hy_zT": zT, "hy_win": win, "dftC": dC, "dftL": dL, "hy_drep": drep})
    for nm in ("hy_f_w1", "hy_f_w2", "hy_f_w3"):
        shared[nm] = np.ascontiguousarray(inp[nm], np.float32)
    th, f32c = rwkv_consts()
    rep = np.zeros((DEPTH, 128, 6, 256), np.float32)
    rep[:, :, 0, :] = np.asarray(inp["rwkv_k_k"], np.float32)[:, None, :]
    rep[:, :, 1, :] = np.asarray(inp["rwkv_k_a"], np.float32)[:, None, :]
    for d_ in range(2):
        rep[:, :, 2 + d_, :] = np.asarray(inp["rwkv_w0"], np.float32)[:, d_, None, :]
        rep[:, :, 4 + d_, :] = np.asarray(inp["rwkv_a0"], np.float32)[:, d_, None, :]
    lora = np.zeros((DEPTH, 2, 128, 256), np.float32)
    for d_ in range(2):
        lora[:, d_, 32 * d_:32 * d_ + 32, :] = np.asarray(inp["rwkv_w_up"], np.float32)[:, d_]
        lora[:, d_, 64 + 32 * d_:96 + 32 * d_, :] = np.asarray(inp["rwkv_a_up"], np.float32)[:, d_]
    shared.update({"rw_th": th, "rw_f32c": f32c, "rw_rep": rep, "rw_lora": np.ascontiguousarray(lora),
                   "rwkv_g_up": np.ascontiguousarray(inp["rwkv_g_up"], np.float32)})
    for nm in ("lru_w_r", "lru_w_i"):
        shared[nm] = np.ascontiguousarray(inp[nm], np.float32)
    for nm in ("w_out", "ffn_w_down", "moe_router", "moe_w_down"):
        shared[nm] = np.ascontiguousarray(inp[nm], np.float32)

    def retile(w):
        w = np.asarray(w, np.float32)
        lead = w.shape[:-2]
        F = w.shape[-1]
        w = w.reshape(lead + (8, 128, F // 128, 128))
        nl = len(lead)
        w = w.transpose(tuple(range(nl)) + (nl + 2, nl + 1, nl + 0, nl + 3))
        return np.ascontiguousarray(w).reshape(lead + (F // 128, 128, D))

    shared["ffn_wg_t"] = retile(inp["ffn_w_gate"]); shared["ffn_wu_t"] = retile(inp["ffn_w_up"])
    shared["moe_wg_t"] = retile(inp["moe_w_gate"]); shared["moe_wu_t"] = retile(inp["moe_w_up"])
    maps = []
    x, ctx, c, c_ctx = (np.asarray(inp[n], np.float32) for n in ("x", "ctx", "c", "c_ctx"))
    for core in range(NCORES):
        m = dict(shared)
        cols = []
        for b in range(NB):
            bb = core * NB + b
            cols.append(ctx[bb].T)
            cols.append(x[bb].T)
        m["xT0"] = np.ascontiguousarray(np.concatenate(cols, 1))
        cc = np.stack([c[core * NB], c[core * NB + 1], c_ctx], 1)
        m["cT"] = np.ascontiguousarray(cc.reshape(8, 128, 3).transpose(1, 0, 2))
        maps.append(m)
    return maps


def kernel(**inputs):
    maps = host_prep(inputs)
    nc = build(list(range(DEPTH)))
    res = run_bass_kernel_spmd(nc, maps, core_ids=list(range(NCORES)))
    out = np.zeros((NCORES * NB, LL, D), np.float32)
    for core in range(NCORES):
        oT = res.results[core]["outT"]
        for b in range(NB):
            out[core * NB + b] = oT[:, b * TT + LC:(b + 1) * TT].T
    return out


def ln_block(g, u, sq, out, gcol, bcol, tmp, W=512):
    k = g.k
    pm, pe = g.PS[0], g.PS[1]
    for c in range(8):
        if c % 2 == 0:
            k.do("act", "activation", dict(out=(sq, sq[:, c, :W])), dict(in_=(u, u[:, c, :W])), func=AF.Square)
        else:
            k.do("pool", "tensor_tensor", dict(out=(sq, sq[:, c, :W])), dict(in0=(u, u[:, c, :W]), in1=(u, u[:, c, :W])), op=ALU.mult)
    for c in range(8):
        k.do("pe", "matmul", dict(out=(pm, pm[:, :W])), dict(lhsT=(g.ONESD, g.ONESD[:]), rhs=(u, u[:, c, :W])),
             acc=(c > 0), start=(c == 0), stop=(c == 7))
    for c in range(8):
        k.do("pe", "matmul", dict(out=(pe, pe[:, :W])), dict(lhsT=(g.ONESD, g.ONESD[:]), rhs=(sq, sq[:, c, :W])),
             acc=(c > 0), start=(c == 0), stop=(c == 7))
    mean, var, rstd = tmp
    k.do("act", "copy", dict(out=(mean, mean[:, :W])), dict(in_=(pm, pm[:, :W])))
    k.do("pool", "tensor_tensor", dict(out=(var, var[:, :W])), dict(in0=(mean, mean[:, :W]), in1=(mean, mean[:, :W])), op=ALU.mult)
    k.do("dve", "tensor_tensor", dict(out=(var, var[:, :W])), dict(in0=(pe, pe[:, :W]), in1=(var, var[:, :W])), op=ALU.subtract)
    k.do("dve", "tensor_scalar", dict(out=(var, var[:, :W])), dict(in0=(var, var[:, :W])), scalar1=0.0, scalar2=1e-5, op0=ALU.max, op1=ALU.add)
    k.do("act", "activation", dict(out=(rstd, rstd[:, :W])), dict(in_=(var, var[:, :W])), func=AF.Sqrt)
    k.do("dve", "reciprocal", dict(out=(rstd, rstd[:, :W])), dict(in_=(rstd, rstd[:, :W])))
    for c in range(8):
        e1 = "dve" if c % 2 == 0 else "pool"
        k.do(e1, "tensor_tensor", dict(out=(sq, sq[:, c, :W])), dict(in0=(u, u[:, c, :W]), in1=(mean, mean[:, :W])), op=ALU.subtract)
        k.do("dve", "tensor_tensor", dict(out=(sq, sq[:, c, :W])), dict(in0=(sq, sq[:, c, :W]), in1=(rstd, rstd[:, :W])), op=ALU.mult)
        k.do("act", "activation", dict(out=(out, out[:, c, :W])),
             dict(in_=(sq, sq[:, c, :W]), scale=(g.PV, g.PV[:, gcol + c:gcol + c + 1]), bias=(g.PV, g.PV[:, bcol + c:bcol + c + 1])),
             func=AF.Identity)


def phase_wout(g, li):
    k = g.k
    with k.phase():
        wo = k.sb("wo", [128, 8, D], BF16)
        load_w_bf16(k, wo, lambda i: wo[:, i, :], g.w_out, lambda i: g.w_out[li, i * 128:(i + 1) * 128, :], 8, [128, D], "wo", qs=("sp", "act"))
        xt = k.sb("xt", [128, 8, 512], F32)
        ym = k.sb("ym", [128, 8, 512], F32)
        yb = k.sb("yb", [128, 8, 512], BF16)
        u = k.sb("u", [128, 8, 512], F32)
        tmp = [k.sb(f"lnt{j}", [128, 512], F32) for j in range(3)]
        XTv = g.XT.t.rearrange("(c p) t -> p c t", p=128)
        YMv = g.YM.t.rearrange("(c p) t -> p c t", p=128)
        for tb in range(NTOK // 512):
            t0, t1 = tb * 512, (tb + 1) * 512
            k.dma("sp", xt[:], XTv[:, :, t0:t1], reads=[g.XT], writes=[xt])
            k.dma("act", ym[:], YMv[:, :, t0:t1], reads=[g.YM], writes=[ym])
            k.do("pool", "tensor_copy", dict(out=(yb, yb[:, 0:4, :])), dict(in_=(ym, ym[:, 0:4, :])))
            k.do("dve", "tensor_copy", dict(out=(yb, yb[:, 4:8, :])), dict(in_=(ym, ym[:, 4:8, :])))
            k.do("act", "mul", dict(out=(xt, xt[:])), dict(in_=(xt, xt[:])), mul=ALPHA)
            for n in range(8):
                ps = g.PS[2 + n % 6]
                for kc in range(8):
                    k.do("pe", "matmul", dict(out=(ps, ps[:])), dict(lhsT=(wo, wo[:, kc, n * 128:(n + 1) * 128]), rhs=(yb, yb[:, kc, :])),
                         acc=(kc > 0), start=(kc == 0), stop=(kc == 7))
                for a, b, col in segs_in(t0, t1):
                    k.do("dve", "scalar_tensor_tensor", dict(out=(u, u[:, n, a:b])),
                         dict(in0=(ps, ps[:, a:b]), scalar=(g.MOD, g.MOD[:, 2 * 8 + n, col:col + 1]), in1=(xt, xt[:, n, a:b])),
                         op0=ALU.mult, op1=ALU.add)
            ln_block(g, u, ym, xt, g.pv["ln1_g"], g.pv["ln1_b"], tmp)
            k.dma("sp", XTv[:, :, t0:t1], xt[:], reads=[xt], writes=[g.XT])


def phase_ffn(g, li):
    k = g.k
    moe = (li % 2 == 1)
    lj = li // 2
    if moe:
        NE, F, wg_d, wu_d, wd_d = NEXP, FF_EXPERT, g.moe_wg, g.moe_wu, g.moe_wd
    else:
        NE, F, wg_d, wu_d, wd_d = 1, FF_DENSE, g.ffn_wg, g.ffn_wu, g.ffn_wd
    NF = F // 128

    def wsl(wt, e):
        return wt[lj, e] if moe else wt[lj]

    with k.phase():
        xt = k.sb("xt", [128, 8, 512], F32)
        hf = k.sb("hf", [128, 8, 512], F32)
        hb = k.sb("hb", [128, 8, 512], BF16)
        acc = k.sb("acc", [128, 8, 512], F32)
        hid = k.sb("hid", [128, NF, 512], BF16)
        wgs = [k.sb(f"wgs{j}", [128, 8, 128], F32) for j in range(2)]
        wus = [k.sb(f"wus{j}", [128, 8, 128], F32) for j in range(2)]
        wgb = [k.sb(f"wgb{j}", [128, 8, 128], BF16) for j in range(2)]
        wub = [k.sb(f"wub{j}", [128, 8, 128], BF16) for j in range(2)]
        wds = [k.sb(f"wds{j}", [128, D], F32) for j in range(2)]
        wdb = [k.sb(f"wdb{j}", [128, D], BF16) for j in range(2)]
        sg = [k.sb(f"sg{j}", [128, 512], F32) for j in range(2)]
        tmp = [k.sb(f"lnt{j}", [128, 512], F32) for j in range(3)]
        if moe:
            rt = k.sb("rt", [128, 8, NEXP], F32)
            k.dma("sp", rt[:], g.moe_router[lj].rearrange("(c p) e -> p c e", p=128), reads=[g.moe_router], writes=[rt])
            lg = k.sb("lg", [128, NEXP], F32)
            eq1 = k.sb("eq1", [128, NEXP], F32)
            eq2 = k.sb("eq2", [128, NEXP], F32)
            l2 = k.sb("l2", [128, NEXP], F32)
            sm = k.sb("sm", [128, 8], F32)
            gts = k.sb("gts", [128, NEXP], F32)
            gT = k.sb("gT", [NEXP, 512], F32)
            GB = k.sb("GB", [128, NEXP, 512], F32)
        XTv = g.XT.t.rearrange("(c p) t -> p c t", p=128)
        cnt = 0
        for tb in range(NTOK // 512):
            t0, t1 = tb * 512, (tb + 1) * 512
            k.dma("sp", xt[:], XTv[:, :, t0:t1], reads=[g.XT], writes=[xt])
            modulate_block(g, xt, hf, t0, t1, 4, 3)
            k.do("pool", "tensor_copy", dict(out=(hb, hb[:, 0:4, :])), dict(in_=(hf, hf[:, 0:4, :])))
            k.do("dve", "tensor_copy", dict(out=(hb, hb[:, 4:8, :])), dict(in_=(hf, hf[:, 4:8, :])))
            if moe:
                for tt_ in range(4):
                    ps = g.PS[tt_]
                    for kc in range(8):
                        k.do("pe", "matmul", dict(out=(ps, ps[:, 0:NEXP])),
                             dict(lhsT=(hf, hf[:, kc, tt_ * 128:(tt_ + 1) * 128]), rhs=(rt, rt[:, kc, :])),
                             acc=(kc > 0), start=(kc == 0), stop=(kc == 7))
                    k.do("act", "copy", dict(out=(lg, lg[:])), dict(in_=(ps, ps[:, 0:NEXP])))
                    k.do("dve", "reduce_max", dict(out=(sm, sm[:, 0:1])), dict(in_=(lg, lg[:])), axis=AX.X)
                    k.do("dve", "tensor_scalar", dict(out=(eq1, eq1[:])), dict(in0=(lg, lg[:]), scalar1=(sm, sm[:, 0:1])), scalar2=None, op0=ALU.is_equal)
                    k.do("dve", "scalar_tensor_tensor", dict(out=(l2, l2[:])), dict(in0=(eq1, eq1[:]), in1=(lg, lg[:])), scalar=-1e30, op0=ALU.mult, op1=ALU.add)
                    k.do("dve", "reduce_max", dict(out=(sm, sm[:, 1:2])), dict(in_=(l2, l2[:])), axis=AX.X)
                    k.do("dve", "tensor_scalar", dict(out=(eq2, eq2[:])), dict(in0=(l2, l2[:]), scalar1=(sm, sm[:, 1:2])), scalar2=None, op0=ALU.is_equal)
                    k.do("dve", "tensor_tensor", dict(out=(sm, sm[:, 2:3])), dict(in0=(sm, sm[:, 1:2]), in1=(sm, sm[:, 0:1])), op=ALU.subtract)
                    k.do("act", "activation", dict(out=(sm, sm[:, 3:4])), dict(in_=(sm, sm[:, 2:3])), func=AF.Exp)
                    k.do("dve", "tensor_scalar_add", dict(out=(sm, sm[:, 4:5])), dict(in0=(sm, sm[:, 3:4])), scalar1=1.0)
                    k.do("dve", "reciprocal", dict(out=(sm, sm[:, 5:6])), dict(in_=(sm, sm[:, 4:5])))
                    k.do("dve", "tensor_tensor", dict(out=(sm, sm[:, 6:7])), dict(in0=(sm, sm[:, 3:4]), in1=(sm, sm[:, 5:6])), op=ALU.mult)
                    k.do("dve", "tensor_scalar", dict(out=(gts, gts[:])), dict(in0=(eq1, eq1[:]), scalar1=(sm, sm[:, 5:6])), scalar2=None, op0=ALU.mult)
                    k.do("dve", "scalar_tensor_tensor", dict(out=(gts, gts[:])), dict(in0=(eq2, eq2[:]), scalar=(sm, sm[:, 6:7]), in1=(gts, gts[:])), op0=ALU.mult, op1=ALU.add)
                    pt = g.PS[4 + tt_ % 2]
                    k.do("pe", "transpose", dict(out=(pt, pt[0:NEXP, 0:128])), dict(in_=(gts, gts[:]), identity=(g.IDENT, g.IDENT[:])))
                    k.do("act", "copy", dict(out=(gT, gT[:, tt_ * 128:(tt_ + 1) * 128])), dict(in_=(pt, pt[0:NEXP, 0:128])))
                for e in range(NEXP):
                    ps = g.PS[e % 4]
                    k.do("pe", "matmul", dict(out=(ps, ps[:])), dict(lhsT=(g.SEL, g.SEL[0:NEXP, e * 128:(e + 1) * 128]), rhs=(gT, gT[:])), start=True, stop=True)
                    ev(k, e, (GB, GB[:, e, :]), (ps, ps[:]))
            for e in range(NE):
                for j in range(NF):
                    a, b = wgs[cnt % 2], wus[cnt % 2]
                    ab, bb = wgb[cnt % 2], wub[cnt % 2]
                    k.dma("sp", a[:].rearrange("p c f -> p (c f)"), wsl(wg_d, e)[j], reads=[wg_d], writes=[a])
                    k.dma("act", b[:].rearrange("p c f -> p (c f)"), wsl(wu_d, e)[j], reads=[wu_d], writes=[b])
                    k.do("pool", "tensor_copy", dict(out=(ab, ab[:])), dict(in_=(a, a[:])))
                    k.do("pool", "tensor_copy", dict(out=(bb, bb[:])), dict(in_=(b, b[:])))
                    pg, pu = g.PS[(cnt % 2) * 2], g.PS[(cnt % 2) * 2 + 1]
                    for kc in range(8):
                        k.do("pe", "matmul", dict(out=(pg, pg[:])), dict(lhsT=(ab, ab[:, kc, :]), rhs=(hb, hb[:, kc, :])),
                             acc=(kc > 0), start=(kc == 0), stop=(kc == 7))
                    for kc in range(8):
                        k.do("pe", "matmul", dict(out=(pu, pu[:])), dict(lhsT=(bb, bb[:, kc, :]), rhs=(hb, hb[:, kc, :])),
                             acc=(kc > 0), start=(kc == 0), stop=(kc == 7))
                    s = sg[cnt % 2]
                    k.do("act", "activation", dict(out=(s, s[:])), dict(in_=(pg, pg[:])), func=AF.Silu)
                    if moe:
                        k.do("dve", "tensor_tensor", dict(out=(s, s[:])), dict(in0=(s, s[:]), in1=(pu, pu[:])), op=ALU.mult)
                        k.do("dve", "tensor_tensor", dict(out=(hid, hid[:, j, :])), dict(in0=(s, s[:]), in1=(GB, GB[:, e, :])), op=ALU.mult)
                    else:
                        k.do("dve", "tensor_tensor", dict(out=(hid, hid[:, j, :])), dict(in0=(s, s[:]), in1=(pu, pu[:])), op=ALU.mult)
                    cnt += 1
                for j in range(NF):
                    ws_, wb_ = wds[j % 2], wdb[j % 2]
                    k.dma("sp" if j % 2 == 0 else "act", ws_[:], wsl(wd_d, e)[j * 128:(j + 1) * 128, :], reads=[wd_d], writes=[ws_])
                    k.do("pool", "tensor_copy", dict(out=(wb_, wb_[:])), dict(in_=(ws_, ws_[:])))
                    for n in range(8):
                        ps = g.PS[n]
                        k.do("pe", "matmul", dict(out=(ps, ps[:])), dict(lhsT=(wb_, wb_[:, n * 128:(n + 1) * 128]), rhs=(hid, hid[:, j, :])),
                             acc=(j > 0), start=(j == 0), stop=(j == NF - 1))
                for n in range(8):
                    ps = g.PS[n]
                    if e == 0:
                        ev(k, n, (acc, acc[:, n, :]), (ps, ps[:]))
                    else:
                        k.do("dve", "tensor_tensor", dict(out=(acc, acc[:, n, :])), dict(in0=(acc, acc[:, n, :]), in1=(ps, ps[:])), op=ALU.add)
            k.do("act", "mul", dict(out=(xt, xt[:])), dict(in_=(xt, xt[:])), mul=ALPHA)
            for n in range(8):
                for a_, b_, col in segs_in(t0, t1):
                    k.do("dve", "scalar_tensor_tensor", dict(out=(hf, hf[:, n, a_:b_])),
                         dict(in0=(acc, acc[:, n, a_:b_]), scalar=(g.MOD, g.MOD[:, 5 * 8 + n, col:col + 1]), in1=(xt, xt[:, n, a_:b_])),
                         op0=ALU.mult, op1=ALU.add)
            ln_block(g, hf, acc, xt, g.pv["ln2_g"], g.pv["ln2_b"], tmp)
            k.dma("sp", XTv[:, :, t0:t1], xt[:], reads=[xt], writes=[g.XT])


BLK = [(i * 512, min((i + 1) * 512, TT)) for i in range((TT + 511) // 512)]
SEG1 = [(0, LC), (LC, TT)]


def rms_rows(g, ys, gain_col, dst_row0, b, eps=1e-6, nparts=128):
    k = g.k
    nch = len(ys) * nparts
    sq = k.csb("rms_sq", [128, 512], F32)
    rs = k.csb("rms_rs", [128, 512], F32)
    ot = [k.csb(f"rms_o{j}", [128, 512], F32) for j in range(2)]
    n = 0
    for (t0, t1) in BLK:
        W = t1 - t0
        ps = g.PS[n % 2]
        for ci, y in enumerate(ys):
            k.do("act", "activation", dict(out=(sq, sq[:nparts, :W])), dict(in_=(y, y[:nparts, t0:t1])), func=AF.Square)
            k.do("pe", "matmul", dict(out=(ps, ps[:nparts, :W])), dict(lhsT=(g.ONESD, g.ONESD[:nparts, :nparts]), rhs=(sq, sq[:nparts, :W])),
                 acc=(ci > 0), start=(ci == 0), stop=(ci == len(ys) - 1))
        k.do("dve", "tensor_scalar", dict(out=(rs, rs[:nparts, :W])), dict(in0=(ps, ps[:nparts, :W])), scalar1=float(D) / nch, scalar2=eps, op0=ALU.mult, op1=ALU.add)
        k.do("act", "activation", dict(out=(rs, rs[:nparts, :W])), dict(in_=(rs, rs[:nparts, :W])), func=AF.Sqrt)
        k.do("dve", "reciprocal", dict(out=(rs, rs[:nparts, :W])), dict(in_=(rs, rs[:nparts, :W])))
        for ci, y in enumerate(ys):
            o = ot[n % 2]
            k.do("dve", "scalar_tensor_tensor", dict(out=(o, o[:nparts, :W])),
                 dict(in0=(y, y[:nparts, t0:t1]), scalar=(g.PV, g.PV[:nparts, gain_col + ci:gain_col + ci + 1]), in1=(rs, rs[:nparts, :W])),
                 op0=ALU.mult, op1=ALU.mult)
            r0 = dst_row0 + ci * nparts
            k.dma("sp", g.YM[r0:r0 + nparts, b * TT + t0:b * TT + t1], o[:nparts, :W], reads=[o], writes=[g.YM])
            n += 1


def phase_lru(g, li):
    k = g.k
    pv = g.pv
    with k.phase():
        wst = k.sb("lw_st", [128, 128], F32)
        W = {}
        for gi, wd_ in enumerate((g.lru_w_r, g.lru_w_i)):
            for d in range(2):
                for cc in range(2):
                    k.do("pool", "memset", dict(ap=(wst, wst[:])), {}, constant=0.0)
                    for blk in range(2):
                        k.dma("sp", wst[blk * 64:(blk + 1) * 64, blk * 64:(blk + 1) * 64], wd_[li, d, cc * 2 + blk], reads=[wd_], writes=[wst])
                    wb = k.sb(f"lw_{gi}{d}{cc}", [128, 128], BF16)
                    k.do("dve", "tensor_copy", dict(out=(wb, wb[:])), dict(in_=(wst, wst[:])))
                    W[(gi, d, cc)] = wb
        cn = k.sb("cneg", [128, 4], F32)
        cn2 = k.sb("cneg2", [128, 4], F32)
        k.do("act", "activation", dict(out=(cn, cn[:])), dict(in_=(g.PV, g.PV[:, pv["lru_lam"]:pv["lru_lam"] + 4])), func=AF.Exp, scale=-1.0)
        k.do("act", "activation", dict(out=(cn, cn[:])), dict(in_=(cn, cn[:])), func=AF.Ln, bias=1.0, scale=1.0)
        k.do("dve", "tensor_scalar", dict(out=(cn2, cn2[:])), dict(in0=(cn, cn[:])), scalar1=-16.0, scalar2=None, op0=ALU.mult)
        k.do("dve", "tensor_scalar", dict(out=(cn, cn[:])), dict(in0=(cn, cn[:])), scalar1=-8.0, scalar2=None, op0=ALU.mult)
        XA = k.sb("l_xa", [128, TT], F32)
        U = k.sb("l_u", [128, TT], F32)
        UB = k.sb("l_ub", [128, TT], BF16)
        BV = k.sb("l_bv", [128, TT], F32)
        HF = k.sb("l_hf", [128, TT], F32)
        HB = k.sb("l_hb", [128, TT], F32)
        Y = [k.sb(f"l_y{j}", [128, TT], F32) for j in range(2)]
        tr = [k.sb(f"l_tr{j}", [128, 512], F32) for j in range(2)]
        ti = [k.sb(f"l_ti{j}", [128, 512], F32) for j in range(2)]
        for b in range(NB):
            for cc in range(2):
                k.dma("sp", XA[:], g.PT[PC_LX + cc, :, b * TT:(b + 1) * TT], reads=[g.PTr[PC_LX + cc]], writes=[XA])
                cw = lambda j: (g.PV, g.PV[:, pv["lru_cw"] + j * 2 + cc: pv["lru_cw"] + j * 2 + cc + 1])
                k.do("act", "activation", dict(out=(U, U[:])), dict(in_=(XA, XA[:]), scale=cw(2), bias=(g.PV, g.PV[:, pv["lru_cb"] + cc:pv["lru_cb"] + cc + 1])), func=AF.Identity)
                for j in (0, 1, 3):
                    dlt = j - 2
                    for (s0, s1) in SEG1:
                        lo, hi = max(s0, s0 - dlt), min(s1, s1 - dlt)
                        k.do("dve", "scalar_tensor_tensor", dict(out=(U, U[:, lo:hi])),
                             dict(in0=(XA, XA[:, lo + dlt:hi + dlt]), scalar=cw(j), in1=(U, U[:, lo:hi])), op0=ALU.mult, op1=ALU.add)
                k.do("pool", "tensor_copy", dict(out=(UB, UB[:])), dict(in_=(U, U[:])))
                for d in range(2):
                    H = HF if d == 0 else HB
                    col = d * 2 + cc
                    for n, (t0, t1) in enumerate(BLK):
                        Wd = t1 - t0
                        pr, pi = g.PS[(n % 2) * 2], g.PS[(n % 2) * 2 + 1]
                        k.do("pe", "matmul", dict(out=(pr, pr[:, :Wd])), dict(lhsT=(W[(0, d, cc)], W[(0, d, cc)][:]), rhs=(UB, UB[:, t0:t1])), start=True, stop=True)
                        k.do("pe", "matmul", dict(out=(pi, pi[:, :Wd])), dict(lhsT=(W[(1, d, cc)], W[(1, d, cc)][:]), rhs=(UB, UB[:, t0:t1])), start=True, stop=True)
                        r_, i_ = tr[n % 2], ti[n % 2]
                        k.do("act", "activation", dict(out=(r_, r_[:, :Wd])), dict(in_=(pr, pr[:, :Wd]), bias=(g.PV, g.PV[:, pv["lru_br"] + col:pv["lru_br"] + col + 1])), func=AF.Sigmoid)
                        k.do("act", "activation", dict(out=(i_, i_[:, :Wd])), dict(in_=(pi, pi[:, :Wd]), bias=(g.PV, g.PV[:, pv["lru_bi"] + col:pv["lru_bi"] + col + 1])), func=AF.Sigmoid)
                        k.do("act", "activation", dict(out=(XA, XA[:, t0:t1])), dict(in_=(r_, r_[:, :Wd]), scale=(cn, cn[:, col:col + 1])), func=AF.Exp)
                        k.do("act", "activation", dict(out=(r_, r_[:, :Wd])), dict(in_=(r_, r_[:, :Wd]), scale=(cn2, cn2[:, col:col + 1])), func=AF.Exp)
                        k.do("act", "activation", dict(out=(r_, r_[:, :Wd])), dict(in_=(r_, r_[:, :Wd])), func=AF.Sqrt, scale=-1.0, bias=1.0)
                        k.do("dve", "tensor_tensor", dict(out=(i_, i_[:, :Wd])), dict(in0=(i_, i_[:, :Wd]), in1=(U, U[:, t0:t1])), op=ALU.mult)
                        k.do("dve", "tensor_tensor", dict(out=(BV, BV[:, t0:t1])), dict(in0=(i_, i_[:, :Wd]), in1=(r_, r_[:, :Wd])), op=ALU.mult)
                    if d == 0:
                        k.do("dve", "tensor_tensor_scan", dict(out=(H, H[:, 0:LC])), dict(data0=(XA, XA[:, 0:LC]), data1=(BV, BV[:, 0:LC])), initial=0.0, op0=ALU.mult, op1=ALU.add)
                        k.do("dve", "tensor_tensor_scan", dict(out=(H, H[:, LC:TT])), dict(data0=(XA, XA[:, LC:TT]), data1=(BV, BV[:, LC:TT]), initial=(H, H[:, LC - 1:LC])), op0=ALU.mult, op1=ALU.add)
                    else:
                        k.do("dve", "tensor_tensor_scan", dict(out=(H, H[:, LC - 1::-1])), dict(data0=(XA, XA[:, LC - 1::-1]), data1=(BV, BV[:, LC - 1::-1])), initial=0.0, op0=ALU.mult, op1=ALU.add)
                        k.do("dve", "tensor_tensor_scan", dict(out=(H, H[:, TT - 1:LC - 1:-1])), dict(data0=(XA, XA[:, TT - 1:LC - 1:-1]), data1=(BV, BV[:, TT - 1:LC - 1:-1]), initial=(H, H[:, 0:1])), op0=ALU.mult, op1=ALU.add)
                k.do("pool", "tensor_tensor", dict(out=(HF, HF[:])), dict(in0=(HF, HF[:]), in1=(HB, HB[:])), op=ALU.add)
                k.dma("sp", XA[:], g.PT[PC_LG + cc, :, b * TT:(b + 1) * TT], reads=[g.PTr[PC_LG + cc]], writes=[XA])
                k.do("act", "activation", dict(out=(BV, BV[:])), dict(in_=(XA, XA[:])), func=AF.Square)
                k.do("dve", "tensor_scalar", dict(out=(BV, BV[:])), dict(in0=(BV, BV[:])), scalar1=0.044715, scalar2=1.0, op0=ALU.mult, op1=ALU.add)
                k.do("dve", "tensor_tensor", dict(out=(BV, BV[:])), dict(in0=(BV, BV[:]), in1=(XA, XA[:])), op=ALU.mult)
                k.do("act", "activation", dict(out=(BV, BV[:])), dict(in_=(BV, BV[:])), func=AF.Sigmoid, scale=1.5957691216)
                k.do("dve", "tensor_tensor", dict(out=(BV, BV[:])), dict(in0=(BV, BV[:]), in1=(XA, XA[:])), op=ALU.mult)
                k.do("dve", "tensor_tensor", dict(out=(Y[cc], Y[cc][:])), dict(in0=(HF, HF[:]), in1=(BV, BV[:])), op=ALU.mult)
            rms_rows(g, Y, pv["lru_on"], 256, b)


def rope_tables():
    tab = np.zeros((128, 2, TT), np.float32)
    t = np.arange(LL)
    r, c = (t // 64).astype(np.float32), (t % 64).astype(np.float32)
    half = 16
    inv = (1.0 / (10000.0 ** (np.arange(0, half, 2, dtype=np.float32) / half))).astype(np.float32)
    ang = np.concatenate([r[:, None] * inv, c[:, None] * inv], -1).astype(np.float32)
    cos, sin = np.cos(ang).T, np.sin(ang).T
    tab[64:80, 0, LC:] = cos; tab[80:96, 0, LC:] = cos
    tab[64:80, 1, LC:] = -sin; tab[80:96, 1, LC:] = sin
    tab[64:96, 0, :LC] = 1.0
    return tab


def phase_mla(g, li):
    k = g.k
    pv = g.pv
    scale = 96.0 ** -0.5
    with k.phase():
        wq0 = k.sb("wq0", [128, 384], BF16); wq1 = k.sb("wq1", [64, 384], BF16)
        wqs0 = k.sb("wqs0", [128, 384], BF16); wqs1 = k.sb("wqs1", [64, 384], BF16)
        wuk = k.sb("wuk", [128, 384], BF16); wuv = k.sb("wuv", [128, 256], BF16)
        st = k.sb("mw_st", [128, 384], F32)
        for dst, src, rows, cols in ((wq0, g.w_uq[li, 0:128, :], 128, 384), (wq1, g.w_uq[li, 128:192, :], 64, 384),
                                     (wqs0, g.w_uq_sw[li, 0:128, :], 128, 384), (wqs1, g.w_uq_sw[li, 128:192, :], 64, 384),
                                     (wuk, g.w_uk_p[li], 128, 384), (wuv, g.w_uv[li], 128, 256)):
            k.dma("sp", st[:rows, :cols], src, reads=[g.w_uq, g.w_uq_sw, g.w_uk_p, g.w_uv], writes=[st])
            k.do("dve", "tensor_copy", dict(out=(dst, dst[:rows, :cols])), dict(in_=(st, st[:rows, :cols])))
        TAB = k.sb("ropetab", [128, 2, TT], F32)
        k.dma("act", TAB[:], g.rope[:], reads=[g.rope], writes=[TAB])
        CQN0 = k.sb("cqn0", [128, TT], BF16); CQN1 = k.sb("cqn1", [64, TT], BF16); CKVN = k.sb("ckvn", [128, TT], BF16)
        KR = k.sb("kr", [128, TT], BF16)
        QT = k.sb("qt", [96, TT], BF16); KT = k.sb("kt", [96, TT], BF16)
        VA = k.sb("va", [128, TT // 128, 4, 65], BF16)
        AO = [k.sb(f"ao{h}", [64, TT], BF16) for h in range(4)]
        xin = [k.sb(f"m_x{j}", [128, 512], F32) for j in range(3)]
        sq = k.sb("m_sq", [128, 512], F32); rs = k.sb("m_rs", [128, 512], F32)
        t1 = k.sb("m_t1", [128, 512], F32); t2 = k.sb("m_t2", [128, 512], F32)
        E = [k.sb(f"m_e{j}", [128, 512], BF16) for j in range(3)]
        rd = k.sb("m_rd", [128, 512], F32); rb = k.sb("m_rb", [64, 512], F32)
        k.do("pool", "memset", dict(ap=(VA, VA[:])), {}, constant=1.0)
        for b in range(NB):
            for n, (t0, t1_) in enumerate(BLK):
                W = t1_ - t0
                c0, c1, ckv = xin
                k.dma("sp", c0[:, :W], g.PT[PC_CQ0, :, b * TT + t0:b * TT + t1_], reads=[g.PTr[PC_CQ0]], writes=[c0])
                k.dma("act", c1[:64, :W], g.PT[PC_CQ1, 0:64, b * TT + t0:b * TT + t1_], reads=[g.PTr[PC_CQ1]], writes=[c1])
                k.dma("sp", ckv[:, :W], g.PT[PC_CKV, :, b * TT + t0:b * TT + t1_], reads=[g.PTr[PC_CKV]], writes=[ckv])
                for grp in (0, 1):
                    ps = g.PS[grp]
                    srcs = ((c0, 128), (c1, 64)) if grp == 0 else ((ckv, 128),)
                    nch = 192 if grp == 0 else 128
                    for ci, (s_, np_) in enumerate(srcs):
                        k.do("act", "activation", dict(out=(sq, sq[:np_, :W])), dict(in_=(s_, s_[:np_, :W])), func=AF.Square)
                        k.do("pe", "matmul", dict(out=(ps, ps[:, :W])), dict(lhsT=(g.ONESD, g.ONESD[:np_, :]), rhs=(sq, sq[:np_, :W])),
                             acc=(ci > 0), start=(ci == 0), stop=(ci == len(srcs) - 1))
                    k.do("dve", "tensor_scalar", dict(out=(rs, rs[:, :W])), dict(in0=(ps, ps[:, :W])), scalar1=float(D) / nch, scalar2=1e-6, op0=ALU.mult, op1=ALU.add)
                    k.do("act", "activation", dict(out=(rs, rs[:, :W])), dict(in_=(rs, rs[:, :W])), func=AF.Sqrt)
                    k.do("dve", "reciprocal", dict(out=(rs, rs[:, :W])), dict(in_=(rs, rs[:, :W])))
                    if grp == 0:
                        k.do("dve", "scalar_tensor_tensor", dict(out=(CQN0, CQN0[:, t0:t1_])), dict(in0=(c0, c0[:, :W]), scalar=(g.PV, g.PV[:, pv["q_norm"]:pv["q_norm"] + 1]), in1=(rs, rs[:, :W])), op0=ALU.mult, op1=ALU.mult)
                        k.do("dve", "scalar_tensor_tensor", dict(out=(CQN1, CQN1[:, t0:t1_])), dict(in0=(c1, c1[:64, :W]), scalar=(g.PV, g.PV[:64, pv["q_norm"] + 1:pv["q_norm"] + 2]), in1=(rs, rs[:64, :W])), op0=ALU.mult, op1=ALU.mult)
                    else:
                        k.do("dve", "scalar_tensor_tensor", dict(out=(CKVN, CKVN[:, t0:t1_])), dict(in0=(ckv, ckv[:, :W]), scalar=(g.PV, g.PV[:, pv["kv_norm"]:pv["kv_norm"] + 1]), in1=(rs, rs[:, :W])), op0=ALU.mult, op1=ALU.mult)
                k.dma("sp", c0[64:96, :W], g.PT[PC_KR, 64:96, b * TT + t0:b * TT + t1_], reads=[g.PTr[PC_KR]], writes=[c0])
                k.dma("act", c1[64:96, :W], g.PT[PC_KRS, 64:96, b * TT + t0:b * TT + t1_], reads=[g.PTr[PC_KRS]], writes=[c1])
                k.do("dve", "tensor_tensor", dict(out=(t1, t1[64:96, :W])), dict(in0=(c0, c0[64:96, :W]), in1=(TAB, TAB[64:96, 0, t0:t1_])), op=ALU.mult)
                k.do("pool", "tensor_tensor", dict(out=(t2, t2[64:96, :W])), dict(in0=(c1, c1[64:96, :W]), in1=(TAB, TAB[64:96, 1, t0:t1_])), op=ALU.mult)
                k.do("dve", "tensor_tensor", dict(out=(KR, KR[64:96, t0:t1_])), dict(in0=(t1, t1[64:96, :W]), in1=(t2, t2[64:96, :W])), op=ALU.add)
            for kt in range(TT // 128):
                ps = g.PS[kt % 2]
                k.do("pe", "matmul", dict(out=(ps, ps[:, 0:256])), dict(lhsT=(CKVN, CKVN[:, kt * 128:(kt + 1) * 128]), rhs=(wuv, wuv[:])), start=True, stop=True)
                ev(k, kt, (VA, VA[:, kt, :, 0:64]), (ps, ps[:, 0:256].rearrange("p (h v) -> p h v", h=4)))
            for h in range(4):
                hs = slice(h * 96, (h + 1) * 96)
                for n, (t0, t1_) in enumerate(BLK):
                    W = t1_ - t0
                    pq, pqs, pk = g.PS[0], g.PS[1], g.PS[2]
                    k.do("pe", "matmul", dict(out=(pq, pq[:96, :W])), dict(lhsT=(wq0, wq0[:, hs]), rhs=(CQN0, CQN0[:, t0:t1_])), start=True, stop=False)
                    k.do("pe", "matmul", dict(out=(pq, pq[:96, :W])), dict(lhsT=(wq1, wq1[:, hs]), rhs=(CQN1, CQN1[:, t0:t1_])), acc=True, start=False, stop=True)
                    k.do("pe", "matmul", dict(out=(pqs, pqs[:96, :W])), dict(lhsT=(wqs0, wqs0[:, hs]), rhs=(CQN0, CQN0[:, t0:t1_])), start=True, stop=False)
                    k.do("pe", "matmul", dict(out=(pqs, pqs[:96, :W])), dict(lhsT=(wqs1, wqs1[:, hs]), rhs=(CQN1, CQN1[:, t0:t1_])), acc=True, start=False, stop=True)
                    k.do("pe", "matmul", dict(out=(pk, pk[:96, :W])), dict(lhsT=(wuk, wuk[:, hs]), rhs=(CKVN, CKVN[:, t0:t1_])), start=True, stop=True)
                    k.do("act", "copy", dict(out=(QT, QT[0:64, t0:t1_])), dict(in_=(pq, pq[0:64, :W])))
                    k.do("act", "copy", dict(out=(KT, KT[0:64, t0:t1_])), dict(in_=(pk, pk[0:64, :W])))
                    k.do("dve", "tensor_tensor", dict(out=(t1, t1[64:96, :W])), dict(in0=(pq, pq[64:96, :W]), in1=(TAB, TAB[64:96, 0, t0:t1_])), op=ALU.mult)
                    k.do("dve", "tensor_tensor", dict(out=(t2, t2[64:96, :W])), dict(in0=(pqs, pqs[64:96, :W]), in1=(TAB, TAB[64:96, 1, t0:t1_])), op=ALU.mult)
                    k.do("dve", "tensor_tensor", dict(out=(QT, QT[64:96, t0:t1_])), dict(in0=(t1, t1[64:96, :W]), in1=(t2, t2[64:96, :W])), op=ALU.add)
                k.do("pool", "tensor_copy", dict(out=(KT, KT[64:96, :])), dict(in_=(KR, KR[64:96, :])))
                qblocks = [(0, LC, 2)] + [(LC + i * 512, LC + (i + 1) * 512, TT // 128) for i in range(LL // 512)]
                for qi, (q0, q1, nkt) in enumerate(qblocks):
                    W = q1 - q0
                    po = g.PS[4 + qi % 2]
                    for kt in range(nkt):
                        ps = g.PS[kt % 4]
                        k.do("pe", "matmul", dict(out=(ps, ps[:, :W])), dict(lhsT=(KT, KT[:, kt * 128:(kt + 1) * 128]), rhs=(QT, QT[:, q0:q1])), start=True, stop=True)
                        e_ = E[kt % 3]
                        k.do("act", "activation", dict(out=(e_, e_[:, :W])), dict(in_=(ps, ps[:, :W])), func=AF.Exp, scale=scale)
                        k.do("pe", "matmul", dict(out=(po, po[:65, :W])), dict(lhsT=(VA, VA[:, kt, h, :]), rhs=(e_, e_[:, :W])),
                             acc=(kt > 0), start=(kt == 0), stop=(kt == nkt - 1))
                    k.do("dve", "reciprocal", dict(out=(rd, rd[64:65, :W])), dict(in_=(po, po[64:65, :W])))
                    pb = g.PS[6]
                    k.do("pe", "matmul", dict(out=(pb, pb[:64, :W])), dict(lhsT=(g.ONES1, g.ONES1[64:65, 0:64]), rhs=(rd, rd[64:65, :W])), start=True, stop=True)
                    k.do("act", "copy", dict(out=(rb, rb[:, :W])), dict(in_=(pb, pb[:64, :W])))
                    k.do("dve", "tensor_tensor", dict(out=(AO[h], AO[h][:, q0:q1])), dict(in0=(po, po[0:64, :W]), in1=(rb, rb[:, :W])), op=ALU.mult)
            rms_rows(g, AO, pv["mla_on"], 0, b, nparts=64)


HY_BANDS = 16


def hyena_consts():
    def feats(L):
        t01 = np.linspace(0.0, 1.0, L, dtype=np.float32)[:, None]
        bands = np.linspace(1e-4, HY_BANDS - 1, HY_BANDS, dtype=np.float32)[None, :]
        wpos = ((2.0 * math.pi / L) * np.arange(L, dtype=np.float32)[:, None]).astype(np.float32)
        z = np.concatenate([t01, np.cos(bands * wpos), -np.sin(bands * wpos)], -1).astype(np.float32)
        dmin, dmax = math.log(1e-2) / 1.5, math.log(1e-2) / 0.3
        deltas = np.abs(np.linspace(dmin, dmax, GROUP, dtype=np.float32))
        win = (np.exp(-t01 * deltas) + 0.05).astype(np.float32)
        return z, win

    zc, wc = feats(LC)
    zl, wl = feats(LL)
    zT = np.ascontiguousarray(np.concatenate([zc, zl], 0).T)
    win = np.ascontiguousarray(np.concatenate([wc, wl], 0))

    def dft(L):
        n = L // 128
        s = np.arange(L, dtype=np.float64)[:, None]
        f = np.arange(L, dtype=np.float64)[None, :]
        ang = math.pi * (2 * f + 1) * s / (2 * L)
        out = []
        for M in (np.cos(ang), np.sin(ang)):
            M = M.astype(np.float32)
            F = M.reshape(n, 128, n, 128).transpose(2, 1, 0, 3)
            I = M.reshape(n, 128, n, 128).transpose(0, 3, 2, 1)
            out.append((np.ascontiguousarray(F).reshape(n, 128, n * 128).astype(ml_dtypes.bfloat16),
                        np.ascontiguousarray(I).reshape(n, 128, n * 128).astype(ml_dtypes.bfloat16)))
        return np.stack([out[0][0], out[1][0], out[0][1], out[1][1]])

    return zT, win, dft(LC), dft(LL)


def sin_exact(k, out, x, tmp):
    s1, c1 = tmp
    n = x[1].shape[0]
    W = x[1].shape[1]
    k.do("act", "activation", dict(out=(s1, s1[:n, :W])), dict(in_=x), func=AF.Sin, scale=0.25)
    k.do("act", "activation", dict(out=(c1, c1[:n, :W])), dict(in_=x), func=AF.Abs)
    k.do("act", "activation", dict(out=(c1, c1[:n, :W])), dict(in_=(c1, c1[:n, :W]), bias=(k.HPI, k.HPI[:n, 0:1])), func=AF.Sin, scale=-0.25)
    k.do("dve", "tensor_tensor", dict(out=(c1, c1[:n, :W])), dict(in0=(c1, c1[:n, :W]), in1=(s1, s1[:n, :W])), op=ALU.mult)
    k.do("dve", "tensor_tensor", dict(out=(s1, s1[:n, :W])), dict(in0=(s1, s1[:n, :W]), in1=(s1, s1[:n, :W])), op=ALU.mult)
    k.do("dve", "tensor_scalar", dict(out=(s1, s1[:n, :W])), dict(in0=(s1, s1[:n, :W])), scalar1=-8.0, scalar2=4.0, op0=ALU.mult, op1=ALU.add)
    k.do("dve", "tensor_tensor", dict(out=out), dict(in0=(c1, c1[:n, :W]), in1=(s1, s1[:n, :W])), op=ALU.mult)


def phase_hy_prep(g, li):
    k = g.k
    pv = g.pv
    with k.phase():
        X = k.sb("hp_x", [128, TT], F32)
        Z = k.sb("hp_z", [128, TT], F32)
        ZS = k.sb("hp_zs", [128, TT // 128, 128], F32)
        for b in range(NB):
            for j in range(6):
                k.dma("sp", X[:], g.PT[PC_HY + j, :, b * TT:(b + 1) * TT], reads=[g.PTr[PC_HY + j]], writes=[X])
                cw = lambda tap: (g.PV, g.PV[:, pv["hy_cw"] + tap * 6 + j: pv["hy_cw"] + tap * 6 + j + 1])
                k.do("act", "activation", dict(out=(Z, Z[:])), dict(in_=(X, X[:]), scale=cw(1), bias=(g.PV, g.PV[:, pv["hy_cb"] + j:pv["hy_cb"] + j + 1])), func=AF.Identity)
                for tap in (0, 2):
                    dlt = tap - 1
                    for (s0, s1) in SEG1:
                        lo, hi = max(s0, s0 - dlt), min(s1, s1 - dlt)
                        k.do("dve", "scalar_tensor_tensor", dict(out=(Z, Z[:, lo:hi])),
                             dict(in0=(X, X[:, lo + dlt:hi + dlt]), scalar=cw(tap), in1=(Z, Z[:, lo:hi])), op0=ALU.mult, op1=ALU.add)
                for tt_ in range(TT // 128):
                    ps = g.PS[tt_ % 4]
                    k.do("pe", "transpose", dict(out=(ps, ps[:, 0:128])), dict(in_=(Z, Z[:, tt_ * 128:(tt_ + 1) * 128]), identity=(g.IDENT, g.IDENT[:])))
                    ev(k, tt_, (ZS, ZS[:, tt_, :]), (ps, ps[:, 0:128]))
                dst = g.ZTOK[j // 2].rearrange("(n p) b c -> p n b c", p=128)[:, :, b, (j % 2) * 128:(j % 2 + 1) * 128]
                k.dma("sp", dst, ZS[:], reads=[ZS], writes=[g.ZTOK])


def phase_hy_main(g, li, L, tok0, dft, dres):
    k = g.k
    pv = g.pv
    n = L // 128
    with k.phase():
        zT = k.sb("hy_zT", [33, L], F32)
        k.dma("sp", zT[:], g.hy_zT[:, tok0:tok0 + L], reads=[g.hy_zT], writes=[zT])
        w1 = k.sb("hy_w1", [33, 64], F32); w2 = k.sb("hy_w2", [64, 64], F32); w3 = k.sb("hy_w3", [64, 1024], F32)
        k.dma("sp", w1[:], g.hy_w1[li], reads=[g.hy_w1], writes=[w1])
        k.dma("sp", w2[:], g.hy_w2[li], reads=[g.hy_w2], writes=[w2])
        k.dma("sp", w3[:], g.hy_w3[li], reads=[g.hy_w3], writes=[w3])
        h1 = k.sb("hy_h1", [64, L], F32); h2 = k.sb("hy_h2", [64, L], F32)
        xb = k.sb("hy_xb", [64, 512], F32)
        stmp = [k.sb(f"hy_st{j}", [64, 512], F32) for j in range(2)]
        for layer, (wt, src, dstt, bcol) in enumerate(((w1, zT, h1, pv["hy_b1"]), (w2, h1, h2, pv["hy_b2"]))):
            kk_ = 33 if layer == 0 else 64
            for i in range((L + 511) // 512):
                t0, t1 = i * 512, min((i + 1) * 512, L)
                W = t1 - t0
                ps = g.PS[i % 2]
                k.do("pe", "matmul", dict(out=(ps, ps[:64, :W])), dict(lhsT=(wt, wt[:kk_, :]), rhs=(src, src[:kk_, t0:t1])), start=True, stop=True)
                k.do("act", "activation", dict(out=(xb, xb[:, :W])), dict(in_=(ps, ps[:64, :W]), bias=(g.PV, g.PV[:64, bcol:bcol + 1])), func=AF.Identity)
                sin_exact(k, (dstt, dstt[:, t0:t1]), (xb, xb[:, :W]), stmp)
        HS = k.sb("hy_hs", [128, n, 512], BF16); HD = k.sb("hy_hd", [128, n, 512], BF16)
        hw = k.sb("hy_hw", [128, 1024], F32); wn = k.sb("hy_wn", [128, 256], F32)
        for lt in range(n):
            k.dma("sp", wn[:], g.hy_win[tok0 + lt * 128: tok0 + (lt + 1) * 128, :], reads=[g.hy_win], writes=[wn])
            for half in range(2):
                ps = g.PS[2 + half]
                k.do("pe", "matmul", dict(out=(ps, ps[:])), dict(lhsT=(h2, h2[:, lt * 128:(lt + 1) * 128]), rhs=(w3, w3[:, half * 512:(half + 1) * 512])), start=True, stop=True)
                for q in range(2):
                    k.do("dve", "tensor_tensor", dict(out=(hw, hw[:, half * 512 + q * 256: half * 512 + (q + 1) * 256])),
                         dict(in0=(ps, ps[:, q * 256:(q + 1) * 256]), in1=(wn, wn[:])), op=ALU.mult)
            for o in range(2):
                fw, bw = hw[:, o * 512:o * 512 + 256], hw[:, o * 512 + 256:o * 512 + 512]
                k.do("dve", "tensor_tensor", dict(out=(HD, HD[:, lt, o * 256:(o + 1) * 256])), dict(in0=(hw, fw), in1=(hw, bw)), op=ALU.subtract)
                if lt == 0:
                    k.do("pool", "memset", dict(ap=(hw, hw[0:1, o * 512 + 256:o * 512 + 512])), {}, constant=0.0)
                k.do("dve", "tensor_tensor", dict(out=(HS, HS[:, lt, o * 256:(o + 1) * 256])), dict(in0=(hw, fw), in1=(hw, bw)), op=ALU.add)
        cf = [k.sb(f"hy_cf{j}", [128, n * 128], BF16) for j in range(2)]
        sf = [k.sb(f"hy_sf{j}", [128, n * 128], BF16) for j in range(2)]
        so = [k.sb(f"hy_so{j}", [128, 512], F32) for j in range(4)]
        cnt = 0
        for fc in range(n):
            c_, s_ = cf[fc % 2], sf[fc % 2]
            k.dma("sp", c_[:], dft[0, fc], reads=[dres], writes=[c_])
            k.dma("act", s_[:], dft[1, fc], reads=[dres], writes=[s_])
            for which, (mt, hh) in enumerate(((c_, HS), (s_, HD))):
                ps = g.PS[4 + cnt % 4]
                for sc in range(n):
                    k.do("pe", "matmul", dict(out=(ps, ps[:])), dict(lhsT=(mt, mt[:, sc * 128:(sc + 1) * 128]), rhs=(hh, hh[:, sc, :])),
                         acc=(sc > 0), start=(sc == 0), stop=(sc == n - 1))
                o_ = so[cnt % 4]
                ev(k, cnt, (o_, o_[:]), (ps, ps[:]))
                k.dma("pool", g.SPEC[which, fc], o_[:], reads=[o_], writes=[g.SPEC])
                cnt += 1
    with k.phase():
        cf = [k.sb(f"hy_cf{j}", [128, n * 128], BF16) for j in range(2)]
        sf = [k.sb(f"hy_sf{j}", [128, n * 128], BF16) for j in range(2)]
        UIN = k.sb("hy_uin", [128, n, 512], BF16)
        Y1 = k.sb("hy_y1", [128, n, 512], BF16); Y2 = k.sb("hy_y2", [128, n, 512], BF16)
        pq = [k.sb(f"hy_pq{j}", [128, 2, 512], F32) for j in range(2)]
        vt = [k.sb(f"hy_vt{j}", [128, 512], F32) for j in range(2)]
        xg = [k.sb(f"hy_xg{j}", [128, 512], F32) for j in range(2)]
        ta = k.sb("hy_ta", [128, 512], F32); tb_ = k.sb("hy_tb", [128, 512], F32)
        tc_ = k.sb("hy_tc", [128, 512], F32); td = k.sb("hy_td", [128, 512], F32)
        drep = k.sb("hy_drep", [128, 2, 512], F32)
        k.dma("sp", drep[:], g.hy_drep[li], reads=[g.hy_drep], writes=[drep])
        ssum = k.sb("hy_ss", [128, 4], F32)
        ZT_ = [g.ZTOK[a].rearrange("t b c -> t (b c)") for a in range(4)]
        for lt in range(n):
            v_ = vt[lt % 2]
            k.dma("sp", v_[:], ZT_[0][tok0 + lt * 128: tok0 + (lt + 1) * 128, :], reads=[g.ZTOK], writes=[v_])
            k.do("pool", "tensor_copy", dict(out=(UIN, UIN[:, lt, :])), dict(in_=(v_, v_[:])))
        for o in range(2):
            for fc in range(n):
                c_, s_ = cf[fc % 2], sf[fc % 2]
                k.dma("sp", c_[:], dft[0, fc], reads=[dres], writes=[c_])
                k.dma("act", s_[:], dft[1, fc], reads=[dres], writes=[s_])
                p_ = pq[fc % 2]
                k.dma("pool", p_[:, 0, :], g.SPEC[0, fc], reads=[g.SPEC], writes=[p_])
                k.dma("pool", p_[:, 1, :], g.SPEC[1, fc], reads=[g.SPEC], writes=[p_])
                pa, pb = g.PS[(fc % 2) * 2], g.PS[(fc % 2) * 2 + 1]
                for sc in range(n):
                    k.do("pe", "matmul", dict(out=(pa, pa[:])), dict(lhsT=(c_, c_[:, sc * 128:(sc + 1) * 128]), rhs=(UIN, UIN[:, sc, :])),
                         acc=(sc > 0), start=(sc == 0), stop=(sc == n - 1))
                for sc in range(n):
                    k.do("pe", "matmul", dict(out=(pb, pb[:])), dict(lhsT=(s_, s_[:, sc * 128:(sc + 1) * 128]), rhs=(UIN, UIN[:, sc, :])),
                         acc=(sc > 0), start=(sc == 0), stop=(sc == n - 1))
                P_, Q_ = p_[:, 0, o * 256:(o + 1) * 256], p_[:, 1, o * 256:(o + 1) * 256]
                for b in range(NB):
                    A_, B_ = pa[:, b * 256:(b + 1) * 256], pb[:, b * 256:(b + 1) * 256]
                    bs = slice(b * 256, (b + 1) * 256)
                    k.do("dve", "tensor_tensor", dict(out=(ta, ta[:, bs])), dict(in0=(pa, A_), in1=(p_, P_)), op=ALU.mult)
                    k.do("dve", "tensor_tensor", dict(out=(tb_, tb_[:, bs])), dict(in0=(pb, B_), in1=(p_, Q_)), op=ALU.mult)
                    k.do("dve", "tensor_tensor", dict(out=(tc_, tc_[:, bs])), dict(in0=(pa, A_), in1=(p_, Q_)), op=ALU.mult)
                    k.do("dve", "tensor_tensor", dict(out=(td, td[:, bs])), dict(in0=(pb, B_), in1=(p_, P_)), op=ALU.mult)
                k.do("pool", "tensor_tensor", dict(out=(Y1, Y1[:, fc, :])), dict(in0=(ta, ta[:]), in1=(tb_, tb_[:])), op=ALU.subtract)
                k.do("pool", "tensor_tensor", dict(out=(Y2, Y2[:, fc, :])), dict(in0=(tc_, tc_[:]), in1=(td, td[:])), op=ALU.add)
            for tc in range(n):
                c_, s_ = cf[tc % 2], sf[tc % 2]
                k.dma("sp", c_[:], dft[2, tc], reads=[dres], writes=[c_])
                k.dma("act", s_[:], dft[3, tc], reads=[dres], writes=[s_])
                v_, x_ = vt[tc % 2], xg[tc % 2]
                rows = slice(tok0 + tc * 128, tok0 + (tc + 1) * 128)
                k.dma("pool", v_[:], ZT_[0 if o == 0 else 3][rows, :], reads=[g.ZTOK], writes=[v_])
                k.dma("pool", x_[:], ZT_[1 + o][rows, :], reads=[g.ZTOK], writes=[x_])
                py = g.PS[4 + tc % 2]
                for fc in range(n):
                    k.do("pe", "matmul", dict(out=(py, py[:])), dict(lhsT=(c_, c_[:, fc * 128:(fc + 1) * 128]), rhs=(Y1, Y1[:, fc, :])),
                         acc=(fc > 0), start=(fc == 0), stop=False)
                for fc in range(n):
                    k.do("pe", "matmul", dict(out=(py, py[:])), dict(lhsT=(s_, s_[:, fc * 128:(fc + 1) * 128]), rhs=(Y2, Y2[:, fc, :])),
                         acc=True, start=False, stop=(fc == n - 1))
                k.do("dve", "tensor_tensor", dict(out=(ta, ta[:])), dict(in0=(v_, v_[:]), in1=(drep, drep[:, o, :])), op=ALU.mult)
                k.do("dve", "scalar_tensor_tensor", dict(out=(ta, ta[:])), dict(in0=(py, py[:]), in1=(ta, ta[:])), scalar=1.0 / L, op0=ALU.mult, op1=ALU.add)
                k.do("dve", "tensor_tensor", dict(out=(tb_, tb_[:])), dict(in0=(ta, ta[:]), in1=(x_, x_[:])), op=ALU.mult)
                if o == 0:
                    k.do("pool", "tensor_copy", dict(out=(UIN, UIN[:, tc, :])), dict(in_=(tb_, tb_[:])))
                    k.dma("sp", ZT_[3][rows, :], tb_[:], reads=[tb_], writes=[g.ZTOK])
                else:
                    for b in range(NB):
                        bs = slice(b * 256, (b + 1) * 256)
                        k.do("act", "activation", dict(out=(tc_, tc_[:, bs]), accum_out=(ssum, ssum[:, b:b + 1])), dict(in_=(tb_, tb_[:, bs])), func=AF.Square)
                    k.do("dve", "tensor_scalar", dict(out=(ssum, ssum[:, 2:4])), dict(in0=(ssum, ssum[:, 0:2])), scalar1=1.0 / 256, scalar2=1e-6, op0=ALU.mult, op1=ALU.add)
                    k.do("act", "activation", dict(out=(ssum, ssum[:, 2:4])), dict(in_=(ssum, ssum[:, 2:4])), func=AF.Sqrt)
                    k.do("dve", "reciprocal", dict(out=(ssum, ssum[:, 2:4])), dict(in_=(ssum, ssum[:, 2:4])))
                    for b in range(NB):
                        bs = slice(b * 256, (b + 1) * 256)
                        k.do("dve", "tensor_scalar", dict(out=(td, td[:, bs])), dict(in0=(tb_, tb_[:, bs]), scalar1=(ssum, ssum[:, 2 + b:3 + b])), scalar2=None, op0=ALU.mult)
                    for b in range(NB):
                        for cc in range(2):
                            pt = g.PS[6 + cc]
                            k.do("pe", "transpose", dict(out=(pt, pt[:, 0:128])), dict(in_=(td, td[:, b * 256 + cc * 128: b * 256 + (cc + 1) * 128]), identity=(g.IDENT, g.IDENT[:])))
                            k.do("act", "activation", dict(out=(tc_, tc_[:, (b * 2 + cc) * 128:(b * 2 + cc + 1) * 128])),
                                 dict(in_=(pt, pt[:, 0:128]), scale=(g.PV, g.PV[:, pv["hy_on"] + cc:pv["hy_on"] + cc + 1])), func=AF.Identity)
                            r0 = 768 + cc * 128
                            k.dma("sp", g.YM[r0:r0 + 128, b * TT + tok0 + tc * 128: b * TT + tok0 + (tc + 1) * 128],
                                  tc_[:, (b * 2 + cc) * 128:(b * 2 + cc + 1) * 128], reads=[tc_], writes=[g.YM])


def phase_hyena(g, li):
    phase_hy_prep(g, li)
    phase_hy_main(g, li, LC, 0, g.dftC, g.dftC)
    phase_hy_main(g, li, LL, LC, g.dftL, g.dftL)


NCH = TT // 64


def rwkv_consts():
    th = np.zeros((128, 64, 128), np.float32)
    for b in range(2):
        for r in range(64):
            th[b * 64 + r, r, b * 64:(b + 1) * 64] = 1.0
    mi = np.zeros((128, 128), np.float32); me = np.zeros((128, 128), np.float32); bo = np.zeros((128, 128), np.float32)
    for b in range(2):
        for s in range(64):
            mi[b * 64 + s, b * 64 + s:(b + 1) * 64] = 1.0
            me[b * 64 + s, b * 64 + s + 1:(b + 1) * 64] = 1.0
        bo[b * 64:(b + 1) * 64, b * 64:(b + 1) * 64] = 1.0 / 64
    f32c = np.concatenate([th[:, 63, :], mi, me, bo], 1)
    return th.reshape(128, 64 * 128).astype(ml_dtypes.bfloat16), f32c


def bwd_lo(c):
    return (LC - 64 - 64 * c) if c < LC // 64 else (TT + LC - 64 - 64 * c)


def phase_rwkv(g, li):
    k = g.k
    pv = g.pv
    ZFv = g.ZF.t.rearrange("j p t -> (j p) t")
    with k.phase():
        X = [k.sb(f"r0_x{j}", [128, TT], F32) for j in range(2)]
        Z = [k.sb(f"r0_z{j}", [128, TT], F32) for j in range(2)]
        cm = k.sb("r0_cm", [128, 8], F32)
        k.do("dve", "tensor_tensor", dict(out=(cm, cm[:])), dict(in0=(g.PV, g.PV[:, pv["mu_prev"]:pv["mu_prev"] + 8]), in1=(g.PV, g.PV[:, pv["mu_next"]:pv["mu_next"] + 8])), op=ALU.add)
        k.do("dve", "tensor_scalar", dict(out=(cm, cm[:])), dict(in0=(cm, cm[:])), scalar1=-1.0, scalar2=1.0, op0=ALU.mult, op1=ALU.add)
        n = 0
        for b in range(NB):
            for j in range(8):
                x_, z_ = X[n % 2], Z[n % 2]
                k.dma("sp" if n % 2 == 0 else "act", x_[:], g.PT[PC_R + j, :, b * TT:(b + 1) * TT], reads=[g.PTr[PC_R + j]], writes=[x_])
                k.do("act", "activation", dict(out=(z_, z_[:])), dict(in_=(x_, x_[:]), scale=(cm, cm[:, j:j + 1])), func=AF.Identity)
                for (s0, s1) in SEG1:
                    k.do("dve", "scalar_tensor_tensor", dict(out=(z_, z_[:, s0 + 1:s1])),
                         dict(in0=(x_, x_[:, s0:s1 - 1]), scalar=(g.PV, g.PV[:, pv["mu_prev"] + j:pv["mu_prev"] + j + 1]), in1=(z_, z_[:, s0 + 1:s1])), op0=ALU.mult, op1=ALU.add)
                    k.do("dve", "scalar_tensor_tensor", dict(out=(z_, z_[:, s0:s1 - 1])),
                         dict(in0=(x_, x_[:, s0 + 1:s1]), scalar=(g.PV, g.PV[:, pv["mu_next"] + j:pv["mu_next"] + j + 1]), in1=(z_, z_[:, s0:s1 - 1])), op0=ALU.mult, op1=ALU.add)
                k.dma("sp", g.ZF[j, :, b * TT:(b + 1) * TT], z_[:], reads=[z_], writes=[g.ZF])
                n += 1
    with k.phase():
        CF = k.sb("r1_cf", [128, 512], F32)
        k.dma("sp", CF[:], g.rw_f32c[:], reads=[g.rw_f32c], writes=[CF])
        MI, ME = CF[:, 128:256], CF[:, 256:384]
        REP = k.sb("r1_rep", [128, 6, 256], F32)
        k.dma("sp", REP[:], g.rw_rep[li], reads=[g.rw_rep], writes=[REP])
        LORA = k.sb("r1_lora", [128, 2, 256], F32)
        k.dma("sp", LORA[:], g.rw_lora[li].rearrange("d p n -> p d n"), reads=[g.rw_lora], writes=[LORA])
        ZCs = [k.sb(f"r1_zc{j}", [128, 5, NB, 64], F32) for j in range(2)]
        ZR = k.sb("r1_zr", [128, 5, NB, 64], F32)
        TL = k.sb("r1_tl", [64, 128], F32)
        rs_, ks_ = k.sb("r1_r", [128, 256], F32), k.sb("r1_k", [128, 256], F32)
        kkr, sqk, kk = k.sb("r1_kkr", [128, 256], F32), k.sb("r1_sqk", [128, 256], F32), k.sb("r1_kk", [128, 256], F32)
        ss = k.sb("r1_ss", [128, 8], F32)
        lw, av, kka, tt_, kd = (k.sb(f"r1_{nm}", [128, 256], F32) for nm in ("lw", "a", "kka", "t", "kd"))
        eg, eng, egp = (k.sb(f"r1_{nm}", [128, 256], F32) for nm in ("eg", "eng", "egp"))
        O4 = [k.sb(f"r1_o4{j}", [128, 4, 256], BF16) for j in range(2)]
        ZFb = [g.ZF[j].rearrange("p (b t) -> p b t", b=NB) for j in range(8)]
        it = 0
        for c in range(NCH):
            for d in range(2):
                lo = 64 * c if d == 0 else bwd_lo(c)
                ZC = ZCs[it % 2]
                for jj, j in enumerate((0, 1, 2, 3, 6)):
                    k.dma("sp" if jj % 2 == 0 else "act", ZC[:, jj, :, :], ZFb[j][:, :, lo:lo + 64], reads=[g.ZF], writes=[ZC])
                if d == 1:
                    k.do("pool", "tensor_copy", dict(out=(ZR, ZR[:].rearrange("p j b t -> p (j b) t"))),
                         dict(in_=(ZC, ZC[:].rearrange("p j b t -> p (j b) t")[:, :, ::-1])))
                    ZS = ZR
                else:
                    ZS = ZC
                zs = lambda jj: ZS[:, jj, :, :].rearrange("p b t -> p (b t)")
                pA = g.PS[0]
                for jj in range(4):
                    k.do("pe", "transpose", dict(out=(pA, pA[:, jj * 128:(jj + 1) * 128])), dict(in_=(ZS, zs(jj)), identity=(g.IDENT, g.IDENT[:])), acc=(jj > 0))
                k.do("act", "activation", dict(out=(TL, TL[:])), dict(in_=(ZS, ZS[0:64, 4, :, :].rearrange("p b t -> p (b t)"))), func=AF.Tanh)
                pW, pA2 = g.PS[1], g.PS[2]
                k.do("pe", "matmul", dict(out=(pW, pW[:, 0:256])), dict(lhsT=(TL, TL[0:64, :]), rhs=(LORA, LORA[0:64, d, :])), start=True, stop=True)
                k.do("pe", "matmul", dict(out=(pA2, pA2[:, 0:256])), dict(lhsT=(ZS, ZS[64:128, 4, :, :].rearrange("p b t -> p (b t)")), rhs=(LORA, LORA[64:128, d, :])), start=True, stop=True)
                k.do("act", "copy", dict(out=(rs_, rs_[:])), dict(in_=(pA, pA[:, 0:256])))
                k.do("act", "copy", dict(out=(ks_, ks_[:])), dict(in_=(pA, pA[:, 256:512])))
                k.do("dve", "tensor_tensor", dict(out=(kkr, kkr[:])), dict(in0=(ks_, ks_[:]), in1=(REP, REP[:, 0, :])), op=ALU.mult)
                k.do("pool", "tensor_tensor", dict(out=(sqk, sqk[:])), dict(in0=(kkr, kkr[:]), in1=(kkr, kkr[:])), op=ALU.mult)
                k.do("dve", "tensor_reduce", dict(out=(ss, ss[:, 0:4])), dict(in_=(sqk, sqk[:].rearrange("p (h k) -> p h k", h=4))), op=ALU.add, axis=AX.X)
                k.do("dve", "tensor_scalar", dict(out=(ss, ss[:, 0:4])), dict(in0=(ss, ss[:, 0:4])), scalar1=1e-24, scalar2=None, op0=ALU.max)
                k.do("act", "activation", dict(out=(ss, ss[:, 0:4])), dict(in_=(ss, ss[:, 0:4])), func=AF.Sqrt)
                k.do("dve", "reciprocal", dict(out=(ss, ss[:, 4:8])), dict(in_=(ss, ss[:, 0:4])))
                k.do("dve", "tensor_tensor", dict(out=(kk, kk[:].rearrange("p (h k) -> p h k", h=4))),
                     dict(in0=(kkr, kkr[:].rearrange("p (h k) -> p h k", h=4)), in1=(ss, ss[:, 4:8].unsqueeze(2).to_broadcast([128, 4, 64]))), op=ALU.mult)
                k.do("dve", "tensor_tensor", dict(out=(lw, lw[:])), dict(in0=(pW, pW[:, 0:256]), in1=(REP, REP[:, 2 + d, :])), op=ALU.add)
                k.do("act", "activation", dict(out=(lw, lw[:])), dict(in_=(lw, lw[:])), func=AF.Sigmoid)
                k.do("pool", "tensor_scalar", dict(out=(lw, lw[:])), dict(in0=(lw, lw[:])), scalar1=-math.exp(-0.5), scalar2=None, op0=ALU.mult)
                k.do("dve", "tensor_tensor", dict(out=(av, av[:])), dict(in0=(pA2, pA2[:, 0:256]), in1=(REP, REP[:, 4 + d, :])), op=ALU.add)
                k.do("act", "activation", dict(out=(av, av[:])), dict(in_=(av, av[:])), func=AF.Sigmoid)
                k.do("pool", "tensor_tensor", dict(out=(kka, kka[:])), dict(in0=(kk, kk[:]), in1=(av, av[:])), op=ALU.mult)
                k.do("dve", "scalar_tensor_tensor", dict(out=(tt_, tt_[:])), dict(in0=(av, av[:]), in1=(REP, REP[:, 1, :])), scalar=-1.0, op0=ALU.add, op1=ALU.mult)
                k.do("pool", "tensor_scalar_add", dict(out=(tt_, tt_[:])), dict(in0=(tt_, tt_[:])), scalar1=1.0)
                k.do("dve", "tensor_tensor", dict(out=(kd, kd[:])), dict(in0=(ks_, ks_[:]), in1=(tt_, tt_[:])), op=ALU.mult)
                pG1, pG2 = g.PS[3], g.PS[4]
                k.do("pe", "matmul", dict(out=(pG1, pG1[:, 0:256])), dict(lhsT=(CF, MI), rhs=(lw, lw[:])), start=True, stop=True)
                k.do("pe", "matmul", dict(out=(pG2, pG2[:, 0:256])), dict(lhsT=(CF, ME), rhs=(lw, lw[:])), start=True, stop=True)
                k.do("act", "activation", dict(out=(eg, eg[:])), dict(in_=(pG1, pG1[:, 0:256])), func=AF.Exp)
                k.do("act", "activation", dict(out=(eng, eng[:])), dict(in_=(pG1, pG1[:, 0:256])), func=AF.Exp, scale=-1.0)
                k.do("act", "activation", dict(out=(egp, egp[:])), dict(in_=(pG2, pG2[:, 0:256])), func=AF.Exp)
                o4 = O4[it % 2]
                k.do("dve", "tensor_tensor", dict(out=(o4, o4[:, 0, :])), dict(in0=(kk, kk[:]), in1=(egp, egp[:])), op=ALU.mult)
                k.do("pool", "tensor_tensor", dict(out=(o4, o4[:, 1, :])), dict(in0=(kka, kka[:]), in1=(eng, eng[:])), op=ALU.mult)
                k.do("dve", "tensor_tensor", dict(out=(o4, o4[:, 2, :])), dict(in0=(kd, kd[:]), in1=(eng, eng[:])), op=ALU.mult)
                k.do("pool", "tensor_tensor", dict(out=(o4, o4[:, 3, :])), dict(in0=(rs_, rs_[:]), in1=(eg, eg[:])), op=ALU.mult)
                k.dma("sp", g.OPD[:, c, :, d * 256:(d + 1) * 256].rearrange("o p n -> p o n"), o4[:], reads=[o4], writes=[g.OPD])
                k.dma("act", g.GAM[c, :, d * 256:(d + 1) * 256], eg[:], reads=[eg], writes=[g.GAM])
                it += 1
    with k.phase():
        TH = k.sb("r2_th", [128, 64 * 128], BF16)
        k.dma("sp", TH[:], g.rw_th[:], reads=[g.rw_th], writes=[TH])
        CF = k.sb("r2_cf", [128, 512], F32)
        k.dma("sp", CF[:], g.rw_f32c[:], reads=[g.rw_f32c], writes=[CF])
        N = k.sb("r2_n", [128, 512], F32)
        k.do("pool", "memset", dict(ap=(N, N[:])), {}, constant=0.0)
        OPT = [k.sb(f"r2_opt{j}", [128, 4, 512], BF16) for j in range(2)]
        GT = [k.sb(f"r2_gt{j}", [128, 512], F32) for j in range(2)]
        VT = [k.sb(f"r2_vt{j}", [128, 8, 64], F32) for j in range(2)]
        VRAW = [k.sb(f"r2_vr{j}", [128, 4, 64], F32) for j in range(2)]
        YT = [k.sb(f"r2_yt{j}", [128, 8, 64], F32) for j in range(2)]
        tmp = k.sb("r2_tmp", [128, 512], F32); tmp2 = k.sb("r2_tmp2", [128, 512], F32)
        sa = k.sb("r2_sa", [128, 8], F32)
        v3 = lambda ap: ap.rearrange("p (g k) -> p g k", g=8)
        for c in range(NCH):
            opt, gt, vt, vr, yt = OPT[c % 2], GT[c % 2], VT[c % 2], VRAW[c % 2], YT[c % 2]
            k.dma("sp", opt[:], g.OPD[:, c].rearrange("o p n -> p o n"), reads=[g.OPD], writes=[opt])
            k.dma("act", gt[:], g.GAM[c], reads=[g.GAM], writes=[gt])
            for b in range(NB):
                src = ZFv[512:768, b * TT + 64 * c: b * TT + 64 * c + 64].rearrange("(h v) t -> v h t", v=64)
                k.dma("sp", vt[b * 64:(b + 1) * 64, 0:4, :], src, reads=[g.ZF], writes=[vt])
                lo = bwd_lo(c)
                src = ZFv[512:768, b * TT + lo: b * TT + lo + 64].rearrange("(h v) t -> v h t", v=64)
                k.dma("act", vr[b * 64:(b + 1) * 64, :, :], src, reads=[g.ZF], writes=[vr])
            k.do("pool", "tensor_copy", dict(out=(vt, vt[:, 4:8, :])), dict(in_=(vr, vr[:, :, ::-1])))
            for r in range(64):
                st = c * 64 + r
                pb = [g.PS[(st % 2) * 4 + op] for op in range(4)]
                for op in range(4):
                    k.do("pe", "matmul", dict(out=(pb[op], pb[op][:])), dict(lhsT=(TH, TH[:, r * 128:(r + 1) * 128]), rhs=(opt, opt[:, op, :])), start=True, stop=True)
                k.do("dve", "tensor_tensor", dict(out=(tmp, tmp[:])), dict(in0=(N, N[:]), in1=(pb[0], pb[0][:])), op=ALU.mult)
                k.do("dve", "tensor_reduce", dict(out=(sa, sa[:])), dict(in_=(tmp, v3(tmp[:]))), op=ALU.add, axis=AX.X)
                k.do("dve", "tensor_tensor", dict(out=(tmp, v3(tmp[:]))), dict(in0=(pb[1], v3(pb[1][:])), in1=(sa, sa[:].unsqueeze(2).to_broadcast([128, 8, 64]))), op=ALU.mult)
                k.do("dve", "tensor_tensor", dict(out=(N, N[:])), dict(in0=(N, N[:]), in1=(tmp, tmp[:])), op=ALU.subtract)
                k.do("dve", "tensor_tensor", dict(out=(tmp2, v3(tmp2[:]))), dict(in0=(pb[2], v3(pb[2][:])), in1=(vt, vt[:, :, r:r + 1].to_broadcast([128, 8, 64]))), op=ALU.mult)
                k.do("dve", "tensor_tensor", dict(out=(N, N[:])), dict(in0=(N, N[:]), in1=(tmp2, tmp2[:])), op=ALU.add)
                k.do("dve", "tensor_tensor", dict(out=(tmp, tmp[:])), dict(in0=(N, N[:]), in1=(pb[3], pb[3][:])), op=ALU.mult)
                k.do("dve", "tensor_reduce", dict(out=(yt, yt[:, :, r])), dict(in_=(tmp, v3(tmp[:]))), op=ALU.add, axis=AX.X)
            pg = g.PS[0]
            k.do("pe", "matmul", dict(out=(pg, pg[:])), dict(lhsT=(CF, CF[:, 0:128]), rhs=(gt, gt[:])), start=True, stop=True)
            k.do("dve", "tensor_tensor", dict(out=(N, N[:])), dict(in0=(N, N[:]), in1=(pg, pg[:])), op=ALU.mult)
            for d in range(2):
                for b in range(NB):
                    dst = g.YS[d, b].rearrange("(h v) t -> v h t", v=64)[:, :, 64 * c:64 * c + 64]
                    k.dma("sp" if d == 0 else "act", dst, yt[b * 64:(b + 1) * 64, d * 4:(d + 1) * 4, :], reads=[yt], writes=[g.YS])
    with k.phase():
        CF = k.sb("r3_cf", [128, 512], F32)
        k.dma("sp", CF[:], g.rw_f32c[:], reads=[g.rw_f32c], writes=[CF])
        BO = CF[:, 384:512]
        LORA = k.sb("r3_lora", [128, 2, 256], F32)
        k.dma("sp", LORA[:], g.rw_lora[li].rearrange("d p n -> p d n"), reads=[g.rw_lora], writes=[LORA])
        GUP = k.sb("r3_gup", [64, 256], F32)
        k.dma("sp", GUP[:], g.rw_gup[li], reads=[g.rw_gup], writes=[GUP])
        YF = k.sb("r3_yf", [128, TT], F32); YB = k.sb("r3_yb", [128, TT], F32)
        ZRt = k.sb("r3_zr", [128, TT], F32); ZK = k.sb("r3_zk", [128, TT], F32); ZV = k.sb("r3_zv", [128, TT], F32)
        ZL = k.sb("r3_zl", [128, TT], F32); ZG = k.sb("r3_zg", [64, TT], F32)
        t_ = [k.sb(f"r3_t{j}", [128, 512], F32) for j in range(6)]
        for b in range(NB):
            bs = slice(b * TT, (b + 1) * TT)
            k.dma("sp", ZL[:], g.ZF[6, :, bs], reads=[g.ZF], writes=[ZL])
            k.dma("act", ZG[:], g.ZF[7, 0:64, bs], reads=[g.ZF], writes=[ZG])
            for cc in range(2):
                k.dma("sp", YF[:], g.YS[0, b, cc * 128:(cc + 1) * 128, :], reads=[g.YS], writes=[YF])
                k.dma("act", YB[:], g.YS[1, b, cc * 128:(cc + 1) * 128, :], reads=[g.YS], writes=[YB])
                k.dma("sp", ZRt[:], g.ZF[0 + cc, :, bs], reads=[g.ZF], writes=[ZRt])
                k.dma("act", ZK[:], g.ZF[2 + cc, :, bs], reads=[g.ZF], writes=[ZK])
                k.dma("sp", ZV[:], g.ZF[4 + cc, :, bs], reads=[g.ZF], writes=[ZV])
                k.do("dve", "tensor_tensor", dict(out=(YF, YF[:, 0:LC])), dict(in0=(YF, YF[:, 0:LC]), in1=(YB, YB[:, LC - 1::-1])), op=ALU.add)
                k.do("dve", "tensor_tensor", dict(out=(YF, YF[:, LC:TT])), dict(in0=(YF, YF[:, LC:TT]), in1=(YB, YB[:, TT - 1:LC - 1:-1])), op=ALU.add)
                for n, (t0, t1) in enumerate(BLK):
                    W = t1 - t0
                    sq, mean, var, yn, a0v, a1v = t_
                    pm, pe, pa0, pa1, psb, pgt = (g.PS[i] for i in range(6))
                    k.do("act", "activation", dict(out=(sq, sq[:, :W])), dict(in_=(YF, YF[:, t0:t1])), func=AF.Square)
                    k.do("pe", "matmul", dict(out=(pm, pm[:, :W])), dict(lhsT=(CF, BO), rhs=(YF, YF[:, t0:t1])), start=True, stop=True)
                    k.do("pe", "matmul", dict(out=(pe, pe[:, :W])), dict(lhsT=(CF, BO), rhs=(sq, sq[:, :W])), start=True, stop=True)
                    k.do("act", "copy", dict(out=(mean, mean[:, :W])), dict(in_=(pm, pm[:, :W])))
                    k.do("pool", "tensor_tensor", dict(out=(var, var[:, :W])), dict(in0=(mean, mean[:, :W]), in1=(mean, mean[:, :W])), op=ALU.mult)
                    k.do("dve", "tensor_tensor", dict(out=(var, var[:, :W])), dict(in0=(pe, pe[:, :W]), in1=(var, var[:, :W])), op=ALU.subtract)
                    k.do("dve", "tensor_scalar", dict(out=(var, var[:, :W])), dict(in0=(var, var[:, :W])), scalar1=0.0, scalar2=64e-5, op0=ALU.max, op1=ALU.add)
                    k.do("act", "activation", dict(out=(var, var[:, :W])), dict(in_=(var, var[:, :W])), func=AF.Sqrt)
                    k.do("dve", "reciprocal", dict(out=(var, var[:, :W])), dict(in_=(var, var[:, :W])))
                    k.do("dve", "tensor_tensor", dict(out=(yn, yn[:, :W])), dict(in0=(YF, YF[:, t0:t1]), in1=(mean, mean[:, :W])), op=ALU.subtract)
                    k.do("dve", "tensor_tensor", dict(out=(yn, yn[:, :W])), dict(in0=(yn, yn[:, :W]), in1=(var, var[:, :W])), op=ALU.mult)
                    k.do("act", "activation", dict(out=(yn, yn[:, :W])), dict(in_=(yn, yn[:, :W]), scale=(g.PV, g.PV[:, pv["rw_lng"] + cc:pv["rw_lng"] + cc + 1]), bias=(g.PV, g.PV[:, pv["rw_lnb"] + cc:pv["rw_lnb"] + cc + 1])), func=AF.Identity)
                    for d, (pp, av_) in enumerate(((pa0, a0v), (pa1, a1v))):
                        k.do("pe", "matmul", dict(out=(pp, pp[:, :W])), dict(lhsT=(LORA, LORA[64:128, d, cc * 128:(cc + 1) * 128]), rhs=(ZL, ZL[64:128, t0:t1])), start=True, stop=True)
                        k.do("act", "activation", dict(out=(av_, av_[:, :W])), dict(in_=(pp, pp[:, :W]), bias=(g.PV, g.PV[:, pv["rw_a0"] + d * 2 + cc:pv["rw_a0"] + d * 2 + cc + 1])), func=AF.Sigmoid)
                    k.do("dve", "tensor_tensor", dict(out=(a0v, a0v[:, :W])), dict(in0=(a0v, a0v[:, :W]), in1=(a1v, a1v[:, :W])), op=ALU.add)
                    k.do("dve", "tensor_scalar", dict(out=(a0v, a0v[:, :W])), dict(in0=(a0v, a0v[:, :W]), scalar2=(g.PV, g.PV[:, pv["rw_ka"] + cc:pv["rw_ka"] + cc + 1])), scalar1=-2.0, op0=ALU.add, op1=ALU.mult)
                    k.do("pool", "tensor_scalar_add", dict(out=(a0v, a0v[:, :W])), dict(in0=(a0v, a0v[:, :W])), scalar1=2.0)
                    k.do("dve", "tensor_tensor", dict(out=(a0v, a0v[:, :W])), dict(in0=(a0v, a0v[:, :W]), in1=(ZK, ZK[:, t0:t1])), op=ALU.mult)
                    k.do("dve", "scalar_tensor_tensor", dict(out=(a1v, a1v[:, :W])), dict(in0=(ZRt, ZRt[:, t0:t1]), scalar=(g.PV, g.PV[:, pv["rw_rk"] + cc:pv["rw_rk"] + cc + 1]), in1=(a0v, a0v[:, :W])), op0=ALU.mult, op1=ALU.mult)
                    k.do("pe", "matmul", dict(out=(psb, psb[:, :W])), dict(lhsT=(CF, BO), rhs=(a1v, a1v[:, :W])), start=True, stop=True)
                    k.do("dve", "scalar_tensor_tensor", dict(out=(sq, sq[:, :W])), dict(in0=(psb, psb[:, :W]), in1=(ZV, ZV[:, t0:t1])), scalar=64.0, op0=ALU.mult, op1=ALU.mult)
                    k.do("dve", "tensor_tensor", dict(out=(yn, yn[:, :W])), dict(in0=(yn, yn[:, :W]), in1=(sq, sq[:, :W])), op=ALU.add)
                    k.do("act", "activation", dict(out=(mean, mean[:64, :W])), dict(in_=(ZG, ZG[:, t0:t1])), func=AF.Sigmoid)
                    k.do("pe", "matmul", dict(out=(pgt, pgt[:, :W])), dict(lhsT=(GUP, GUP[:, cc * 128:(cc + 1) * 128]), rhs=(mean, mean[:64, :W])), start=True, stop=True)
                    k.do("dve", "tensor_tensor", dict(out=(yn, yn[:, :W])), dict(in0=(yn, yn[:, :W]), in1=(pgt, pgt[:, :W])), op=ALU.mult)
                    k.dma("sp", g.YM[512 + cc * 128:512 + (cc + 1) * 128, b * TT + t0:b * TT + t1], yn[:, :W], reads=[yn], writes=[g.YM])
```

```python
import math
import numpy as np
import ml_dtypes
from contextlib import ExitStack
import concourse.bass as bass
import concourse.mybir as mybir
from concourse.bass_utils import run_bass_kernel_spmd

F32 = mybir.dt.float32
BF16 = mybir.dt.bfloat16
AF = mybir.ActivationFunctionType
ALU = mybir.AluOpType
AX = mybir.AxisListType

NCORES = 8
D = 1024
DEPTH = 4
LC = 256
LL = 4096
TT = LC + LL
NB = 2
NTOK = NB * TT
GROUP = 256
ALPHA = (2.0 * DEPTH) ** 0.25
FF_DENSE = 2816
FF_EXPERT = 3584
NEXP = 8


class Res:
    __slots__ = ("w", "rd", "name")

    def __init__(self, name=""):
        self.w = None
        self.rd = {}
        self.name = name


class T:
    def __init__(self, t, name=""):
        self.t = t
        self.res = Res(name)

    def __getitem__(self, idx):
        return self.t[idx]


class K:
    ENGS = ("pe", "dve", "act", "pool", "sp")

    def __init__(self, nc, n_dma_sems=16):
        self.nc = nc
        self.es = ExitStack()
        self.ops = {e: [] for e in self.ENGS}
        self.sem = {e: nc.alloc_semaphore(name=f"sem_{e}") for e in ("pe", "dve", "act", "pool")}
        self.cnt = {e: 0 for e in self.sem}
        self.seen = {e: {} for e in self.ENGS}
        self.dsem, self.dcnt, self.dnext = {}, {}, {}
        for q in ("sp", "pool", "act"):
            n = n_dma_sems if q == "sp" else 8
            self.dsem[q] = [nc.alloc_semaphore(name=f"dsem_{q}{i}") for i in range(n)]
            self.dcnt[q] = [0] * n
            self.dnext[q] = 0
        self.n_inst = 0
        self.scopes = []

    def sb(self, name, shape, dt=F32):
        st = self.scopes[-1] if self.scopes else self.es
        self.uid = getattr(self, "uid", 0) + 1
        name = f"{name}_{self.uid}"
        t = st.enter_context(self.nc.sbuf_tensor(name, list(shape), dt))
        return T(t, name)

    def csb(self, name, shape, dt=F32):
        c = self.__dict__.setdefault("_cache", {})
        if name not in c:
            c[name] = self.sb(name, shape, dt)
        return c[name]

    def ps(self, name, shape, dt=F32):
        t = self.es.enter_context(self.nc.psum_tensor(name, list(shape), dt))
        return T(t, name)

    def dram(self, name, shape, dt=F32, kind="Internal"):
        t = self.nc.dram_tensor(name, list(shape), dt, kind=kind)
        return T(t, name)

    def _collect(self, eng, reads, writes, skip_self_w=False):
        waits = {}

        def add(tok, is_w=False):
            if tok is None:
                return
            s, v, e = tok
            if skip_self_w and is_w and e == eng:
                return
            key = id(s)
            if self.seen[eng].get(key, 0) >= v:
                return
            if key not in waits or waits[key][1] < v:
                waits[key] = (s, v)

        for r in reads:
            r = r.res if isinstance(r, T) else r
            add(r.w)
        for w in writes:
            w = w.res if isinstance(w, T) else w
            add(w.w, True)
            for t in w.rd.values():
                add(t)
        out = list(waits.values())
        for s, v in out:
            self.seen[eng][id(s)] = v
        return out

    def _update(self, tok, reads, writes):
        for r in reads:
            r = r.res if isinstance(r, T) else r
            r.rd[id(tok[0])] = tok
        for w in writes:
            w = w.res if isinstance(w, T) else w
            w.w = tok
            w.rd = {}

    def op(self, eng, fn, reads=(), writes=(), acc=False):
        waits = self._collect(eng, reads, writes, skip_self_w=acc)
        self.cnt[eng] += 1
        sem = self.sem[eng]
        tok = (sem, self.cnt[eng], eng)

        def thunk(e):
            for s, v in waits:
                e.wait_ge(s, v)
            fn(e).then_inc(sem, 1)

        self.ops[eng].append(thunk)
        self._update(tok, reads, writes)
        self.n_inst += 1

    def do(self, eng, method, outs, ins, acc=False, **kw):
        aps = {n: v[1] for n, v in outs.items()}
        aps.update({n: v[1] for n, v in ins.items()})
        aps.update(kw)
        self.op(eng, lambda e: getattr(e, method)(**aps), reads=[v[0] for v in ins.values()],
                writes=[v[0] for v in outs.values()], acc=acc)

    def dma(self, q, out, in_, reads=(), writes=(), **kw):
        i = self.dnext[q]
        self.dnext[q] = (i + 1) % len(self.dsem[q])
        sem = self.dsem[q][i]
        waits = self._collect(q, reads, writes)
        prev = self.dcnt[q][i]
        if prev > 0 and self.seen[q].get(id(sem), 0) < prev:
            waits.append((sem, prev))
            self.seen[q][id(sem)] = prev
        self.dcnt[q][i] = prev + 16
        tok = (sem, prev + 16, "dma_" + q)

        def thunk(e):
            for s, v in waits:
                e.wait_ge(s, v)
            e.dma_start(out=out, in_=in_, **kw).then_inc(sem, 16)

        self.ops[q].append(thunk)
        self._update(tok, reads, writes)
        self.n_inst += 1

    def barrier(self):
        targets = [(self.sem[e], self.cnt[e]) for e in self.sem if self.cnt[e] > 0]
        for q in self.dsem:
            for s, c in zip(self.dsem[q], self.dcnt[q]):
                if c > 0:
                    targets.append((s, c))
        for eng in self.ENGS:
            ws = []
            for s, v in targets:
                if self.seen[eng].get(id(s), 0) < v:
                    ws.append((s, v))
                    self.seen[eng][id(s)] = v

            def thunk(e, ws=ws):
                for s, v in ws:
                    e.wait_ge(s, v)

            self.ops[eng].append(thunk)

    def phase(self):
        k = self

        class _P:
            def __enter__(self_):
                st = ExitStack()
                k.scopes.append(st)
                return st

            def __exit__(self_, *a):
                k.barrier()
                k.__dict__["_cache"] = {}
                st = k.scopes.pop()
                st.close()
                return False

        return _P()

    def finish(self):
        self.barrier()
        nc = self.nc
        with nc.Block() as block:
            @block.sync
            def _(e):
                for th in self.ops["sp"]:
                    th(e)

            @block.tensor
            def _(e):
                for th in self.ops["pe"]:
                    th(e)

            @block.vector
            def _(e):
                for th in self.ops["dve"]:
                    th(e)

            @block.scalar
            def _(e):
                for th in self.ops["act"]:
                    th(e)

            @block.gpsimd
            def _(e):
                for th in self.ops["pool"]:
                    th(e)
        self.es.close()


A_CQ, A_CKV, A_KR, B_X, B_GATE, C_OFF, D_OFF, N_IN = 0, 192, 320, 352, 608, 864, 1824, 2592
NPC = 23
PC_CQ0, PC_CQ1, PC_CKV, PC_KR, PC_KRS, PC_LX, PC_LG, PC_R, PC_KK, PC_V, PC_LORA, PC_GD, PC_HY = \
    0, 1, 2, 3, 4, 5, 7, 9, 11, 13, 15, 16, 17


def _pcols():
    cols = -np.ones(NPC * 128, np.int64)

    def put(chunk, off, idx):
        idx = np.asarray(idx)
        cols[chunk * 128 + off: chunk * 128 + off + len(idx)] = idx

    put(0, 0, np.arange(0, 128))
    put(1, 0, np.arange(128, 192))
    put(2, 0, np.arange(192, 320))
    kr = np.arange(320, 352)
    put(3, 64, kr)
    put(4, 64, np.concatenate([kr[16:], kr[:16]]))
    put(5, 0, np.arange(352, 608))
    put(7, 0, np.arange(608, 864))
    put(9, 0, np.arange(864, 1632))
    put(15, 0, np.arange(1632, 1760))
    put(16, 0, np.arange(1760, 1824))
    put(17, 0, np.arange(1824, 2592))
    return cols


def _gather_cols(w, cols):
    out = np.zeros(w.shape[:-1] + (len(cols),), w.dtype)
    m = cols >= 0
    out[..., m] = w[..., cols[m]]
    return out


def _chunks(v, n=None):
    v = np.asarray(v, np.float32).reshape(-1)
    c = (len(v) + 127) // 128
    o = np.zeros(c * 128, np.float32)
    o[:len(v)] = v
    return o.reshape(c, 128).T


SEGS = [(0, LC, 2), (LC, TT, 0), (TT, TT + LC, 2), (TT + LC, 2 * TT, 1)]


def segs_in(t0, t1):
    for a, b, col in SEGS:
        lo, hi = max(a, t0), min(b, t1)
        if lo < hi:
            yield lo - t0, hi - t0, col


class G:
    pass


def ev(k, i, out, in_):
    if i % 2 == 0:
        k.do("act", "copy", dict(out=out), dict(in_=in_))
    else:
        k.do("dve", "tensor_copy", dict(out=out), dict(in_=in_))


def load_w_bf16(k, dst, dst_ap_fn, src_T, src_ap_fn, n, shape, name, qs=("sp",)):
    stg = [k.sb(f"{name}_stg{j}", shape, F32) for j in range(2)]
    for i in range(n):
        s = stg[i % 2]
        k.dma(qs[i % len(qs)], s[:], src_ap_fn(i), reads=[src_T], writes=[s])
        if i % 2 == 0:
            k.do("pool", "tensor_copy", dict(out=(dst, dst_ap_fn(i))), dict(in_=(s, s[:])))
        else:
            k.do("dve", "tensor_copy", dict(out=(dst, dst_ap_fn(i))), dict(in_=(s, s[:])))


def phase_mod(g, li):
    k = g.k
    with k.phase():
        wst = [k.sb(f"adaw{j}", [128, 8, 1024], F32) for j in range(2)]
        for grp in range(6):
            w = wst[grp % 2]
            k.dma("sp" if grp % 2 == 0 else "act", w[:],
                  g.ada_w[li, :, grp * 1024:(grp + 1) * 1024].rearrange("(c p) n -> p c n", p=128),
                  reads=[g.ada_w], writes=[w])
            for jj in range(8):
                j = grp * 8 + jj
                ps = g.PS[j % 4]
                for kc in range(8):
                    k.do("pe", "matmul", dict(out=(ps, ps[:, 0:3])),
                         dict(lhsT=(w, w[:, kc, jj * 128:(jj + 1) * 128]), rhs=(g.sT, g.sT[:, kc, :])),
                         acc=(kc > 0), start=(kc == 0), stop=(kc == 7))
                k.do("act", "activation", dict(out=(g.MOD, g.MOD[:, j, :])),
                     dict(in_=(ps, ps[:, 0:3]), bias=(g.PV, g.PV[:, g.pv["ada_b"] + j:g.pv["ada_b"] + j + 1])),
                     func=AF.Identity, scale=1.0)
        for grp in (1, 4):
            k.do("dve", "tensor_scalar_add", dict(out=(g.MOD1, g.MOD1[:, grp * 8:(grp + 1) * 8, :])),
                 dict(in0=(g.MOD, g.MOD[:, grp * 8:(grp + 1) * 8, :])), scalar1=1.0)


def modulate_block(g, xt, hb, t0, t1, gs, gb):
    k = g.k
    i = 0
    for a, b, col in segs_in(t0, t1):
        for c in range(8):
            sc = (g.MOD1, g.MOD1[:, gs * 8 + c, col:col + 1])
            bi = (g.MOD, g.MOD[:, gb * 8 + c, col:col + 1])
            if i % 2 == 0:
                k.do("act", "activation", dict(out=(hb, hb[:, c, a:b])),
                     dict(in_=(xt, xt[:, c, a:b]), scale=sc, bias=bi), func=AF.Identity)
            else:
                k.do("dve", "tensor_scalar", dict(out=(hb, hb[:, c, a:b])),
                     dict(in0=(xt, xt[:, c, a:b]), scalar1=sc, scalar2=bi), op0=ALU.mult, op1=ALU.add)
            i += 1


def phase_p(g, li):
    k = g.k
    with k.phase():
        win = k.sb("win", [128, 8, NPC * 128], BF16)
        load_w_bf16(k, win, lambda i: win[:, i, :], g.w_in_p,
                    lambda i: g.w_in_p[li, i * 128:(i + 1) * 128, :], 8, [128, NPC * 128], "win", qs=("sp", "act"))
        xts = [k.sb(f"xt{j}", [128, 8, 512], F32) for j in range(2)]
        hbs = [k.sb(f"hb{j}", [128, 8, 512], BF16) for j in range(2)]
        stg = [k.sb(f"pstg{j}", [128, 512], F32) for j in range(4)]
        XTv = g.XT.t.rearrange("(c p) t -> p c t", p=128)
        n = 0
        for tb in range(NTOK // 512):
            t0, t1 = tb * 512, (tb + 1) * 512
            xt, hb = xts[tb % 2], hbs[tb % 2]
            k.dma("sp", xt[:], XTv[:, :, t0:t1], reads=[g.XT], writes=[xt])
            modulate_block(g, xt, hb, t0, t1, 1, 0)
            for pc in range(NPC):
                ps = g.PS[n % 8]
                for kc in range(8):
                    k.do("pe", "matmul", dict(out=(ps, ps[:])),
                         dict(lhsT=(win, win[:, kc, pc * 128:(pc + 1) * 128]), rhs=(hb, hb[:, kc, :])),
                         acc=(kc > 0), start=(kc == 0), stop=(kc == 7))
                s = stg[n % 4]
                ev(k, n, (s, s[:]), (ps, ps[:]))
                k.dma("sp" if n % 2 == 0 else "pool", g.PT[pc, :, t0:t1], s[:], reads=[s], writes=[g.PTr[pc]])
                n += 1


def pv_spec():
    spec = [("ada_b", 48), ("ln1_g", 8), ("ln1_b", 8), ("ln2_g", 8), ("ln2_b", 8),
            ("q_norm", 2), ("kv_norm", 1), ("mla_on", 4),
            ("lru_cw", 8), ("lru_cb", 2), ("lru_br", 4), ("lru_bi", 4), ("lru_lam", 4), ("lru_on", 2),
            ("mu_prev", 8), ("mu_next", 8), ("rw_lng", 2), ("rw_lnb", 2), ("rw_rk", 2), ("rw_ka", 2),
            ("rw_a0", 4), ("rw_w0", 4), ("rw_kk", 2),
            ("hy_cw", 18), ("hy_cb", 6), ("hy_b1", 1), ("hy_b2", 1), ("hy_on", 2), ("hy_d", 4)]
    pv, off = {}, 0
    for n, c in spec:
        pv[n] = off
        off += c
    return pv, off


def make_pv(inp, li):
    pv, n = pv_spec()
    out = np.zeros((128, n), np.float32)

    def put(name, arr):
        arr = np.asarray(arr, np.float32)
        out[:, pv[name]:pv[name] + arr.shape[1]] = arr

    put("ada_b", _chunks(inp["ada_b"][li]))
    for nm in ("ln1_g", "ln1_b", "ln2_g", "ln2_b"):
        put(nm, _chunks(inp[nm][li]))
    put("q_norm", _chunks(inp["mla_q_norm"][li]))
    put("kv_norm", _chunks(inp["mla_kv_norm"][li]))
    on = np.zeros((128, 4), np.float32)
    on[:64, :] = inp["mla_out_norm"][li].reshape(4, 64).T
    put("mla_on", on)
    put("lru_cw", np.concatenate([_chunks(inp["lru_conv_w"][li][j]) for j in range(4)], 1))
    put("lru_cb", _chunks(inp["lru_conv_b"][li]))
    put("lru_br", np.concatenate([_chunks(inp["lru_b_r"][li][d]) for d in range(2)], 1))
    put("lru_bi", np.concatenate([_chunks(inp["lru_b_i"][li][d]) for d in range(2)], 1))
    put("lru_lam", np.concatenate([_chunks(inp["lru_lambda"][li][d]) for d in range(2)], 1))
    put("lru_on", _chunks(inp["lru_out_norm"][li]))
    mp = np.zeros(8 * 128, np.float32); mn = np.zeros(8 * 128, np.float32)
    mp[:960] = inp["rwkv_mu_prev"][li]; mn[:960] = inp["rwkv_mu_next"][li]
    put("mu_prev", mp.reshape(8, 128).T); put("mu_next", mn.reshape(8, 128).T)
    put("rw_lng", _chunks(inp["rwkv_ln_g"][li])); put("rw_lnb", _chunks(inp["rwkv_ln_b"][li]))
    put("rw_rk", _chunks(inp["rwkv_r_k"][li].reshape(-1))); put("rw_ka", _chunks(inp["rwkv_k_a"][li]))
    put("rw_a0", np.concatenate([_chunks(inp["rwkv_a0"][li][d]) for d in range(2)], 1))
    put("rw_w0", np.concatenate([_chunks(inp["rwkv_w0"][li][d]) for d in range(2)], 1))
    put("rw_kk", _chunks(inp["rwkv_k_k"][li]))
    put("hy_cw", np.concatenate([_chunks(inp["hy_conv_w"][li][j]) for j in range(3)], 1))
    put("hy_cb", _chunks(inp["hy_conv_b"][li]))
    b1 = np.zeros((128, 1), np.float32); b1[:64, 0] = inp["hy_f_b1"][li]
    b2 = np.zeros((128, 1), np.float32); b2[:64, 0] = inp["hy_f_b2"][li]
    put("hy_b1", b1); put("hy_b2", b2)
    put("hy_on", _chunks(inp["hy_out_norm"][li]))
    put("hy_d", np.concatenate([_chunks(inp["hy_d"][li][o]) for o in range(2)], 1))
    return out


def build(layers, dbg=(), stages=("mod", "p", "mix", "lru", "mla", "hy", "rwkv", "wout", "ffn"), ym_in=False):
    nc = bass.Bass("TRN2", target_bir_lowering=False)
    k = K(nc)
    g = G()
    g.k, g.nc = k, nc
    g.pv, npv = pv_spec()

    def inp(name, shape, dt=F32):
        return k.dram(name, shape, dt, kind="ExternalInput")

    g.XT0 = inp("xT0", [D, NTOK])
    g.cT = inp("cT", [128, 8, 3])
    g.ada_w = inp("ada_w", [DEPTH, D, 6 * D])
    g.w_in_p = inp("w_in_p", [DEPTH, D, NPC * 128])
    g.PVd = inp("pvd", [DEPTH, 128, npv])
    g.w_out = inp("w_out", [DEPTH, D, D])
    g.ffn_wg = inp("ffn_wg_t", [2, FF_DENSE // 128, 128, D])
    g.ffn_wu = inp("ffn_wu_t", [2, FF_DENSE // 128, 128, D])
    g.ffn_wd = inp("ffn_w_down", [2, FF_DENSE, D])
    g.moe_router = inp("moe_router", [2, D, NEXP])
    g.moe_wg = inp("moe_wg_t", [2, NEXP, FF_EXPERT // 128, 128, D])
    g.moe_wu = inp("moe_wu_t", [2, NEXP, FF_EXPERT // 128, 128, D])
    g.moe_wd = inp("moe_w_down", [2, NEXP, FF_EXPERT, D])
    g.cst = inp("cst", [128, 128 * 3 + NEXP * 128])
    declare_mixer_inputs(g, inp)
    g.OUT = k.dram("outT", [D, NTOK], F32, kind="ExternalOutput")
    g.XT = g.OUT
    g.PT = k.dram("PT", [NPC, 128, NTOK], F32)
    g.PTr = [Res(f"PT{i}") for i in range(NPC)]
    g.YM = k.dram("YM", [D, NTOK], F32)
    g.PS = [k.ps(f"ps{i}", [128, 512], F32) for i in range(8)]
    g.MOD = k.sb("MOD", [128, 48, 3], F32)
    g.MOD1 = k.sb("MOD1", [128, 48, 3], F32)
    g.sT = k.sb("sT", [128, 8, 3], F32)
    g.PV = k.sb("PV", [128, npv], F32)
    g.CST = k.sb("CST", [128, 128 * 3 + NEXP * 128], F32)
    g.ONESD = T(g.CST.t[:, 0:128]); g.ONESD.res = g.CST.res
    g.IDENT = T(g.CST.t[:, 128:256]); g.IDENT.res = g.CST.res
    g.SEL = T(g.CST.t[:, 256:256 + NEXP * 128]); g.SEL.res = g.CST.res
    g.ONES1 = T(g.CST.t[:, 256 + NEXP * 128:384 + NEXP * 128]); g.ONES1.res = g.CST.res

    k.dma("sp", g.CST[:], g.cst[:], reads=[g.cst], writes=[g.CST])
    k.dma("sp", g.XT[:, :], g.XT0[:, :], reads=[g.XT0], writes=[g.XT])
    k.dma("sp", g.sT[:], g.cT[:], reads=[g.cT], writes=[g.sT])
    k.do("act", "activation", dict(out=(g.sT, g.sT[:])), dict(in_=(g.sT, g.sT[:])), func=AF.Silu)
    if ym_in:
        ymi = inp("dbg_YMin", [D, NTOK])
        k.dma("sp", g.YM[:, :], ymi[:, :], reads=[ymi], writes=[g.YM])
    for li in layers:
        k.dma("sp", g.PV[:], g.PVd[li], reads=[g.PVd], writes=[g.PV])
        if "mod" in stages:
            phase_mod(g, li)
        if "p" in stages:
            phase_p(g, li)
        if "mix" in stages:
            phase_mixers(g, li, stages)
        if "wout" in stages:
            phase_wout(g, li)
        if "ffn" in stages:
            phase_ffn(g, li)
    if "PT" in dbg:
        o = k.dram("dbg_PT", [NPC, 128, NTOK], F32, kind="ExternalOutput")
        k.dma("sp", o[:, :, :], g.PT[:, :, :], reads=list(g.PTr), writes=[o])
    if "YM" in dbg:
        o = k.dram("dbg_YM", [D, NTOK], F32, kind="ExternalOutput")
        k.dma("sp", o[:, :], g.YM[:, :], reads=[g.YM], writes=[o])
    for name, (t, shape) in g.dbgd.items():
        if name in dbg:
            o = k.dram("dbg_" + name, shape, F32, kind="ExternalOutput")
            k.dma("sp", o[:], t[:], reads=[t], writes=[o])
    k.finish()
    print("instructions:", k.n_inst)
    return nc


def declare_mixer_inputs(g, inp):
    g.dbgd = {}
    g.lru_w_r = inp("lru_w_r", [DEPTH, 2, 4, 64, 64])
    g.lru_w_i = inp("lru_w_i", [DEPTH, 2, 4, 64, 64])
    g.w_uq = inp("mla_w_uq", [DEPTH, 192, 384])
    g.w_uq_sw = inp("w_uq_sw", [DEPTH, 192, 384])
    g.w_uk_p = inp("w_uk_p", [DEPTH, 128, 384])
    g.w_uv = inp("w_uv", [DEPTH, 128, 256])
    g.rope = inp("rope", [128, 2, TT])
    g.rw_th = inp("rw_th", [128, 64 * 128], BF16)
    g.rw_f32c = inp("rw_f32c", [128, 512])
    g.rw_rep = inp("rw_rep", [DEPTH, 128, 6, 256])
    g.rw_lora = inp("rw_lora", [DEPTH, 2, 128, 256])
    g.rw_gup = inp("rwkv_g_up", [DEPTH, 64, 256])
    g.ZF = g.k.dram("ZF", [8, 128, NTOK], F32)
    g.OPD = g.k.dram("OPD", [4, NCH, 128, 512], BF16)
    g.GAM = g.k.dram("GAM", [NCH, 128, 512], F32)
    g.YS = g.k.dram("YS", [2, NB, 256, TT], F32)
    g.dbgd.update({"ZF": (g.ZF, [8, 128, NTOK]), "YS": (g.YS, [2, NB, 256, TT]), "GAM": (g.GAM, [NCH, 128, 512])})
    g.hy_zT = inp("hy_zT", [33, TT])
    g.hy_win = inp("hy_win", [TT, 256])
    g.dftC = inp("dftC", [4, LC // 128, 128, LC], BF16)
    g.dftL = inp("dftL", [4, LL // 128, 128, LL], BF16)
    g.hy_w1 = inp("hy_f_w1", [DEPTH, 33, 64])
    g.hy_w2 = inp("hy_f_w2", [DEPTH, 64, 64])
    g.hy_w3 = inp("hy_f_w3", [DEPTH, 64, 1024])
    g.hy_drep = inp("hy_drep", [DEPTH, 128, 2, 512])
    g.ZTOK = g.k.dram("ZTOK", [4, TT, NB, 256], F32)
    g.SPEC = g.k.dram("SPEC", [2, LL // 128, 128, 512], F32)
    g.k.HPI = g.k.sb("HPI", [128, 1], F32)
    g.k.do("pool", "memset", dict(ap=(g.k.HPI, g.k.HPI[:])), {}, constant=math.pi / 2)


def phase_mixers(g, li, stages):
    if "lru" in stages:
        phase_lru(g, li)
    if "mla" in stages:
        phase_mla(g, li)
    if "hy" in stages:
        phase_hyena(g, li)
    if "rwkv" in stages:
        phase_rwkv(g, li)


def make_cst():
    c = np.zeros((128, 384 + NEXP * 128), np.float32)
    c[:, 256 + NEXP * 128:] = 1.0
    c[:, 0:128] = 1.0 / D
    c[:, 128:256] = np.eye(128, dtype=np.float32)
    for e in range(NEXP):
        c[e, 256 + e * 128: 256 + (e + 1) * 128] = 1.0
    return c


def host_prep(inp):
    pc = _pcols()
    shared = {
        "ada_w": np.ascontiguousarray(inp["ada_w"], np.float32),
        "w_in_p": _gather_cols(np.asarray(inp["w_in"], np.float32), pc),
        "pvd": np.stack([make_pv(inp, li) for li in range(DEPTH)]),
        "cst": make_cst(),
    }
    wuq = np.asarray(inp["mla_w_uq"], np.float32)
    wukv = np.asarray(inp["mla_w_ukv"], np.float32)
    sw = np.zeros_like(wuq)
    ukp = np.zeros((DEPTH, 128, 384), np.float32)
    uv = np.zeros((DEPTH, 128, 256), np.float32)
    for h in range(4):
        sw[:, :, h * 96 + 64:h * 96 + 80] = wuq[:, :, h * 96 + 80:h * 96 + 96]
        sw[:, :, h * 96 + 80:h * 96 + 96] = wuq[:, :, h * 96 + 64:h * 96 + 80]
        ukp[:, :, h * 96:h * 96 + 64] = wukv[:, :, h * 128:h * 128 + 64]
        uv[:, :, h * 64:(h + 1) * 64] = wukv[:, :, h * 128 + 64:(h + 1) * 128]
    shared.update({"mla_w_uq": wuq, "w_uq_sw": sw, "w_uk_p": ukp, "w_uv": uv, "rope": rope_tables()})
    zT, win, dC, dL = hyena_consts()
    hd = np.asarray(inp["hy_d"], np.float32)
    drep = np.ascontiguousarray(np.broadcast_to(np.tile(hd, (1, 1, 2))[:, None], (DEPTH, 128, 2, 512)))
    shared.update({"hy_zT": zT, "hy_win": win, "dftC": dC, "dftL": dL, "hy_drep": drep})
    for nm in ("hy_f_w1", "hy_f_w2", "hy_f_w3"):
        shared[nm] = np.ascontiguousarray(inp[nm], np.float32)
    th, f32c = rwkv_consts()
    rep = np.zeros((DEPTH, 128, 6, 256), np.float32)
    rep[:, :, 0, :] = np.asarray(inp["rwkv_k_k"], np.float32)[:, None, :]
    rep[:, :, 1, :] = np.asarray(inp["rwkv_k_a"], np.float32)[:, None, :]
    for d_ in range(2):
        rep[:, :, 2 + d_, :] = np.asarray(inp["rwkv_w0"], np.float32)[:, d_, None, :]
        rep[:, :, 4 + d_, :] = np.asarray(inp["rwkv_a0"], np.float32)[:, d_, None, :]
    lora = np.zeros((DEPTH, 2, 128, 256), np.float32)
    for d_ in range(2):
        lora[:, d_, 32 * d_:32 * d_ + 32, :] = np.asarray(inp["rwkv_w_up"], np.float32)[:, d_]
        lora[:, d_, 64 + 32 * d_:96 + 32 * d_, :] = np.asarray(inp["rwkv_a_up"], np.float32)[:, d_]
    shared.update({"rw_th": th, "rw_f32c": f32c, "rw_rep": rep, "rw_lora": np.ascontiguousarray(lora),
                   "rwkv_g_up": np.ascontiguousarray(inp["rwkv_g_up"], np.float32)})
    for nm in ("lru_w_r", "lru_w_i"):
        shared[nm] = np.ascontiguousarray(inp[nm], np.float32)
    for nm in ("w_out", "ffn_w_down", "moe_router", "moe_w_down"):
        shared[nm] = np.ascontiguousarray(inp[nm], np.float32)

    def retile(w):
        w = np.asarray(w, np.float32)
        lead = w.shape[:-2]
        F = w.shape[-1]
        w = w.reshape(lead + (8, 128, F // 128, 128))
        nl = len(lead)
        w = w.transpose(tuple(range(nl)) + (nl + 2, nl + 1, nl + 0, nl + 3))
        return np.ascontiguousarray(w).reshape(lead + (F // 128, 128, D))

    shared["ffn_wg_t"] = retile(inp["ffn_w_gate"]); shared["ffn_wu_t"] = retile(inp["ffn_w_up"])
    shared["moe_wg_t"] = retile(inp["moe_w_gate"]); shared["moe_wu_t"] = retile(inp["moe_w_up"])
    maps = []
    x, ctx, c, c_ctx = (np.asarray(inp[n], np.float32) for n in ("x", "ctx", "c", "c_ctx"))
    for core in range(NCORES):
        m = dict(shared)
        cols = []
        for b in range(NB):
            bb = core * NB + b
            cols.append(ctx[bb].T)
            cols.append(x[bb].T)
        m["xT0"] = np.ascontiguousarray(np.concatenate(cols, 1))
        cc = np.stack([c[core * NB], c[core * NB + 1], c_ctx], 1)
        m["cT"] = np.ascontiguousarray(cc.reshape(8, 128, 3).transpose(1, 0, 2))
        maps.append(m)
    return maps


def kernel(**inputs):
    maps = host_prep(inputs)
    nc = build(list(range(DEPTH)))
    res = run_bass_kernel_spmd(nc, maps, core_ids=list(range(NCORES)))
    out = np.zeros((NCORES * NB, LL, D), np.float32)
    for core in range(NCORES):
        oT = res.results[core]["outT"]
        for b in range(NB):
            out[core * NB + b] = oT[:, b * TT + LC:(b + 1) * TT].T
    return out


def ln_block(g, u, sq, out, gcol, bcol, tmp, W=512):
    k = g.k
    pm, pe = g.PS[0], g.PS[1]
    for c in range(8):
        if c % 2 == 0:
            k.do("act", "activation", dict(out=(sq, sq[:, c, :W])), dict(in_=(u, u[:, c, :W])), func=AF.Square)
        else:
            k.do("pool", "tensor_tensor", dict(out=(sq, sq[:, c, :W])), dict(in0=(u, u[:, c, :W]), in1=(u, u[:, c, :W])), op=ALU.mult)
    for c in range(8):
        k.do("pe", "matmul", dict(out=(pm, pm[:, :W])), dict(lhsT=(g.ONESD, g.ONESD[:]), rhs=(u, u[:, c, :W])),
             acc=(c > 0), start=(c == 0), stop=(c == 7))
    for c in range(8):
        k.do("pe", "matmul", dict(out=(pe, pe[:, :W])), dict(lhsT=(g.ONESD, g.ONESD[:]), rhs=(sq, sq[:, c, :W])),
             acc=(c > 0), start=(c == 0), stop=(c == 7))
    mean, var, rstd = tmp
    k.do("act", "copy", dict(out=(mean, mean[:, :W])), dict(in_=(pm, pm[:, :W])))
    k.do("pool", "tensor_tensor", dict(out=(var, var[:, :W])), dict(in0=(mean, mean[:, :W]), in1=(mean, mean[:, :W])), op=ALU.mult)
    k.do("dve", "tensor_tensor", dict(out=(var, var[:, :W])), dict(in0=(pe, pe[:, :W]), in1=(var, var[:, :W])), op=ALU.subtract)
    k.do("dve", "tensor_scalar", dict(out=(var, var[:, :W])), dict(in0=(var, var[:, :W])), scalar1=0.0, scalar2=1e-5, op0=ALU.max, op1=ALU.add)
    k.do("act", "activation", dict(out=(rstd, rstd[:, :W])), dict(in_=(var, var[:, :W])), func=AF.Sqrt)
    k.do("dve", "reciprocal", dict(out=(rstd, rstd[:, :W])), dict(in_=(rstd, rstd[:, :W])))
    for c in range(8):
        e1 = "dve" if c % 2 == 0 else "pool"
        k.do(e1, "tensor_tensor", dict(out=(sq, sq[:, c, :W])), dict(in0=(u, u[:, c, :W]), in1=(mean, mean[:, :W])), op=ALU.subtract)
        k.do("dve", "tensor_tensor", dict(out=(sq, sq[:, c, :W])), dict(in0=(sq, sq[:, c, :W]), in1=(rstd, rstd[:, :W])), op=ALU.mult)
        k.do("act", "activation", dict(out=(out, out[:, c, :W])),
             dict(in_=(sq, sq[:, c, :W]), scale=(g.PV, g.PV[:, gcol + c:gcol + c + 1]), bias=(g.PV, g.PV[:, bcol + c:bcol + c + 1])),
             func=AF.Identity)


def phase_wout(g, li):
    k = g.k
    with k.phase():
        wo = k.sb("wo", [128, 8, D], BF16)
        load_w_bf16(k, wo, lambda i: wo[:, i, :], g.w_out, lambda i: g.w_out[li, i * 128:(i + 1) * 128, :], 8, [128, D], "wo", qs=("sp", "act"))
        xt = k.sb("xt", [128, 8, 512], F32)
        ym = k.sb("ym", [128, 8, 512], F32)
        yb = k.sb("yb", [128, 8, 512], BF16)
        u = k.sb("u", [128, 8, 512], F32)
        tmp = [k.sb(f"lnt{j}", [128, 512], F32) for j in range(3)]
        XTv = g.XT.t.rearrange("(c p) t -> p c t", p=128)
        YMv = g.YM.t.rearrange("(c p) t -> p c t", p=128)
        for tb in range(NTOK // 512):
            t0, t1 = tb * 512, (tb + 1) * 512
            k.dma("sp", xt[:], XTv[:, :, t0:t1], reads=[g.XT], writes=[xt])
            k.dma("act", ym[:], YMv[:, :, t0:t1], reads=[g.YM], writes=[ym])
            k.do("pool", "tensor_copy", dict(out=(yb, yb[:, 0:4, :])), dict(in_=(ym, ym[:, 0:4, :])))
            k.do("dve", "tensor_copy", dict(out=(yb, yb[:, 4:8, :])), dict(in_=(ym, ym[:, 4:8, :])))
            k.do("act", "mul", dict(out=(xt, xt[:])), dict(in_=(xt, xt[:])), mul=ALPHA)
            for n in range(8):
                ps = g.PS[2 + n % 6]
                for kc in range(8):
                    k.do("pe", "matmul", dict(out=(ps, ps[:])), dict(lhsT=(wo, wo[:, kc, n * 128:(n + 1) * 128]), rhs=(yb, yb[:, kc, :])),
                         acc=(kc > 0), start=(kc == 0), stop=(kc == 7))
                for a, b, col in segs_in(t0, t1):
                    k.do("dve", "scalar_tensor_tensor", dict(out=(u, u[:, n, a:b])),
                         dict(in0=(ps, ps[:, a:b]), scalar=(g.MOD, g.MOD[:, 2 * 8 + n, col:col + 1]), in1=(xt, xt[:, n, a:b])),
                         op0=ALU.mult, op1=ALU.add)
            ln_block(g, u, ym, xt, g.pv["ln1_g"], g.pv["ln1_b"], tmp)
            k.dma("sp", XTv[:, :, t0:t1], xt[:], reads=[xt], writes=[g.XT])


def phase_ffn(g, li):
    k = g.k
    moe = (li % 2 == 1)
    lj = li // 2
    if moe:
        NE, F, wg_d, wu_d, wd_d = NEXP, FF_EXPERT, g.moe_wg, g.moe_wu, g.moe_wd
    else:
        NE, F, wg_d, wu_d, wd_d = 1, FF_DENSE, g.ffn_wg, g.ffn_wu, g.ffn_wd
    NF = F // 128

    def wsl(wt, e):
        return wt[lj, e] if moe else wt[lj]

    with k.phase():
        xt = k.sb("xt", [128, 8, 512], F32)
        hf = k.sb("hf", [128, 8, 512], F32)
        hb = k.sb("hb", [128, 8, 512], BF16)
        acc = k.sb("acc", [128, 8, 512], F32)
        hid = k.sb("hid", [128, NF, 512], BF16)
        NSTG = 4
        wgs = [k.sb(f"wgs{j}", [128, 8, 128], F32) for j in range(NSTG)]
        wus = [k.sb(f"wus{j}", [128, 8, 128], F32) for j in range(NSTG)]
        wgb = [k.sb(f"wgb{j}", [128, 8, 128], BF16) for j in range(NSTG)]
        wub = [k.sb(f"wub{j}", [128, 8, 128], BF16) for j in range(NSTG)]
        wds = [k.sb(f"wds{j}", [128, D], F32) for j in range(NSTG)]
        wdb = [k.sb(f"wdb{j}", [128, D], BF16) for j in range(NSTG)]
        sg = [k.sb(f"sg{j}", [128, 512], F32) for j in range(2)]
        tmp = [k.sb(f"lnt{j}", [128, 512], F32) for j in range(3)]
        if moe:
            rt = k.sb("rt", [128, 8, NEXP], F32)
            k.dma("sp", rt[:], g.moe_router[lj].rearrange("(c p) e -> p c e", p=128), reads=[g.moe_router], writes=[rt])
            lg = k.sb("lg", [128, NEXP], F32)
            eq1 = k.sb("eq1", [128, NEXP], F32)
            eq2 = k.sb("eq2", [128, NEXP], F32)
            l2 = k.sb("l2", [128, NEXP], F32)
            sm = k.sb("sm", [128, 8], F32)
            gts = k.sb("gts", [128, NEXP], F32)
            gT = k.sb("gT", [NEXP, 512], F32)
            GB = k.sb("GB", [128, NEXP, 512], F32)
        XTv = g.XT.t.rearrange("(c p) t -> p c t", p=128)
        cnt = 0
        for tb in range(NTOK // 512):
            t0, t1 = tb * 512, (tb + 1) * 512
            k.dma("sp", xt[:], XTv[:, :, t0:t1], reads=[g.XT], writes=[xt])
            modulate_block(g, xt, hf, t0, t1, 4, 3)
            k.do("pool", "tensor_copy", dict(out=(hb, hb[:, 0:4, :])), dict(in_=(hf, hf[:, 0:4, :])))
            k.do("dve", "tensor_copy", dict(out=(hb, hb[:, 4:8, :])), dict(in_=(hf, hf[:, 4:8, :])))
            if moe:
                for tt_ in range(4):
                    ps = g.PS[tt_]
                    for kc in range(8):
                        k.do("pe", "matmul", dict(out=(ps, ps[:, 0:NEXP])),
                             dict(lhsT=(hf, hf[:, kc, tt_ * 128:(tt_ + 1) * 128]), rhs=(rt, rt[:, kc, :])),
                             acc=(kc > 0), start=(kc == 0), stop=(kc == 7))
                    k.do("act", "copy", dict(out=(lg, lg[:])), dict(in_=(ps, ps[:, 0:NEXP])))
                    k.do("dve", "reduce_max", dict(out=(sm, sm[:, 0:1])), dict(in_=(lg, lg[:])), axis=AX.X)
                    k.do("dve", "tensor_scalar", dict(out=(eq1, eq1[:])), dict(in0=(lg, lg[:]), scalar1=(sm, sm[:, 0:1])), scalar2=None, op0=ALU.is_equal)
                    k.do("dve", "scalar_tensor_tensor", dict(out=(l2, l2[:])), dict(in0=(eq1, eq1[:]), in1=(lg, lg[:])), scalar=-1e30, op0=ALU.mult, op1=ALU.add)
                    k.do("dve", "reduce_max", dict(out=(sm, sm[:, 1:2])), dict(in_=(l2, l2[:])), axis=AX.X)
                    k.do("dve", "tensor_scalar", dict(out=(eq2, eq2[:])), dict(in0=(l2, l2[:]), scalar1=(sm, sm[:, 1:2])), scalar2=None, op0=ALU.is_equal)
                    k.do("dve", "tensor_tensor", dict(out=(sm, sm[:, 2:3])), dict(in0=(sm, sm[:, 1:2]), in1=(sm, sm[:, 0:1])), op=ALU.subtract)
                    k.do("act", "activation", dict(out=(sm, sm[:, 3:4])), dict(in_=(sm, sm[:, 2:3])), func=AF.Exp)
                    k.do("dve", "tensor_scalar_add", dict(out=(sm, sm[:, 4:5])), dict(in0=(sm, sm[:, 3:4])), scalar1=1.0)
                    k.do("dve", "reciprocal", dict(out=(sm, sm[:, 5:6])), dict(in_=(sm, sm[:, 4:5])))
                    k.do("dve", "tensor_tensor", dict(out=(sm, sm[:, 6:7])), dict(in0=(sm, sm[:, 3:4]), in1=(sm, sm[:, 5:6])), op=ALU.mult)
                    k.do("dve", "tensor_scalar", dict(out=(gts, gts[:])), dict(in0=(eq1, eq1[:]), scalar1=(sm, sm[:, 5:6])), scalar2=None, op0=ALU.mult)
                    k.do("dve", "scalar_tensor_tensor", dict(out=(gts, gts[:])), dict(in0=(eq2, eq2[:]), scalar=(sm, sm[:, 6:7]), in1=(gts, gts[:])), op0=ALU.mult, op1=ALU.add)
                    pt = g.PS[4 + tt_ % 2]
                    k.do("pe", "transpose", dict(out=(pt, pt[0:NEXP, 0:128])), dict(in_=(gts, gts[:]), identity=(g.IDENT, g.IDENT[:])))
                    k.do("act", "copy", dict(out=(gT, gT[:, tt_ * 128:(tt_ + 1) * 128])), dict(in_=(pt, pt[0:NEXP, 0:128])))
                for e in range(NEXP):
                    ps = g.PS[e % 4]
                    k.do("pe", "matmul", dict(out=(ps, ps[:])), dict(lhsT=(g.SEL, g.SEL[0:NEXP, e * 128:(e + 1) * 128]), rhs=(gT, gT[:])), start=True, stop=True)
                    ev(k, e, (GB, GB[:, e, :]), (ps, ps[:]))
            for e in range(NE):
                for j in range(NF):
                    a, b = wgs[cnt % NSTG], wus[cnt % NSTG]
                    ab, bb = wgb[cnt % NSTG], wub[cnt % NSTG]
                    k.dma("sp", a[:].rearrange("p c f -> p (c f)"), wsl(wg_d, e)[j], reads=[wg_d], writes=[a])
                    k.dma("sp", b[:].rearrange("p c f -> p (c f)"), wsl(wu_d, e)[j], reads=[wu_d], writes=[b])
                    k.do("pool", "tensor_copy", dict(out=(ab, ab[:])), dict(in_=(a, a[:])))
                    k.do("dve", "tensor_copy", dict(out=(bb, bb[:])), dict(in_=(b, b[:])))
                    pg, pu = g.PS[(cnt % 2) * 2], g.PS[(cnt % 2) * 2 + 1]
                    for kc in range(8):
                        k.do("pe", "matmul", dict(out=(pg, pg[:])), dict(lhsT=(ab, ab[:, kc, :]), rhs=(hb, hb[:, kc, :])),
                             acc=(kc > 0), start=(kc == 0), stop=(kc == 7))
                    for kc in range(8):
                        k.do("pe", "matmul", dict(out=(pu, pu[:])), dict(lhsT=(bb, bb[:, kc, :]), rhs=(hb, hb[:, kc, :])),
                             acc=(kc > 0), start=(kc == 0), stop=(kc == 7))
                    s = sg[cnt % 2]
                    k.do("act", "activation", dict(out=(s, s[:])), dict(in_=(pg, pg[:])), func=AF.Silu)
                    if moe:
                        k.do("dve", "tensor_tensor", dict(out=(s, s[:])), dict(in0=(s, s[:]), in1=(pu, pu[:])), op=ALU.mult)
                        k.do("dve", "tensor_tensor", dict(out=(hid, hid[:, j, :])), dict(in0=(s, s[:]), in1=(GB, GB[:, e, :])), op=ALU.mult)
                    else:
                        k.do("dve", "tensor_tensor", dict(out=(hid, hid[:, j, :])), dict(in0=(s, s[:]), in1=(pu, pu[:])), op=ALU.mult)
                    cnt += 1
                for j in range(NF):
                    ws_, wb_ = wds[j % NSTG], wdb[j % NSTG]
                    k.dma("sp", ws_[:], wsl(wd_d, e)[j * 128:(j + 1) * 128, :], reads=[wd_d], writes=[ws_])
                    k.do("pool", "tensor_copy", dict(out=(wb_, wb_[:])), dict(in_=(ws_, ws_[:])))
                    for n in range(8):
                        ps = g.PS[n]
                        k.do("pe", "matmul", dict(out=(ps, ps[:])), dict(lhsT=(wb_, wb_[:, n * 128:(n + 1) * 128]), rhs=(hid, hid[:, j, :])),
                             acc=(j > 0), start=(j == 0), stop=(j == NF - 1))
                for n in range(8):
                    ps = g.PS[n]
                    if e == 0:
                        ev(k, n, (acc, acc[:, n, :]), (ps, ps[:]))
                    else:
                        k.do("dve", "tensor_tensor", dict(out=(acc, acc[:, n, :])), dict(in0=(acc, acc[:, n, :]), in1=(ps, ps[:])), op=ALU.add)
            k.do("act", "mul", dict(out=(xt, xt[:])), dict(in_=(xt, xt[:])), mul=ALPHA)
            for n in range(8):
                for a_, b_, col in segs_in(t0, t1):
                    k.do("dve", "scalar_tensor_tensor", dict(out=(hf, hf[:, n, a_:b_])),
                         dict(in0=(acc, acc[:, n, a_:b_]), scalar=(g.MOD, g.MOD[:, 5 * 8 + n, col:col + 1]), in1=(xt, xt[:, n, a_:b_])),
                         op0=ALU.mult, op1=ALU.add)
            ln_block(g, hf, acc, xt, g.pv["ln2_g"], g.pv["ln2_b"], tmp)
            k.dma("sp", XTv[:, :, t0:t1], xt[:], reads=[xt], writes=[g.XT])


BLK = [(i * 512, min((i + 1) * 512, TT)) for i in range((TT + 511) // 512)]
SEG1 = [(0, LC), (LC, TT)]


def rms_rows(g, ys, gain_col, dst_row0, b, eps=1e-6, nparts=128):
    k = g.k
    nch = len(ys) * nparts
    sq = k.csb("rms_sq", [128, 512], F32)
    rs = k.csb("rms_rs", [128, 512], F32)
    ot = [k.csb(f"rms_o{j}", [128, 512], F32) for j in range(2)]
    n = 0
    for (t0, t1) in BLK:
        W = t1 - t0
        ps = g.PS[n % 2]
        for ci, y in enumerate(ys):
            k.do("act", "activation", dict(out=(sq, sq[:nparts, :W])), dict(in_=(y, y[:nparts, t0:t1])), func=AF.Square)
            k.do("pe", "matmul", dict(out=(ps, ps[:nparts, :W])), dict(lhsT=(g.ONESD, g.ONESD[:nparts, :nparts]), rhs=(sq, sq[:nparts, :W])),
                 acc=(ci > 0), start=(ci == 0), stop=(ci == len(ys) - 1))
        k.do("dve", "tensor_scalar", dict(out=(rs, rs[:nparts, :W])), dict(in0=(ps, ps[:nparts, :W])), scalar1=float(D) / nch, scalar2=eps, op0=ALU.mult, op1=ALU.add)
        k.do("act", "activation", dict(out=(rs, rs[:nparts, :W])), dict(in_=(rs, rs[:nparts, :W])), func=AF.Sqrt)
        k.do("dve", "reciprocal", dict(out=(rs, rs[:nparts, :W])), dict(in_=(rs, rs[:nparts, :W])))
        for ci, y in enumerate(ys):
            o = ot[n % 2]
            k.do("dve", "scalar_tensor_tensor", dict(out=(o, o[:nparts, :W])),
                 dict(in0=(y, y[:nparts, t0:t1]), scalar=(g.PV, g.PV[:nparts, gain_col + ci:gain_col + ci + 1]), in1=(rs, rs[:nparts, :W])),
                 op0=ALU.mult, op1=ALU.mult)
            r0 = dst_row0 + ci * nparts
            k.dma("sp", g.YM[r0:r0 + nparts, b * TT + t0:b * TT + t1], o[:nparts, :W], reads=[o], writes=[g.YM])
            n += 1


def phase_lru(g, li):
    k = g.k
    pv = g.pv
    with k.phase():
        wst = k.sb("lw_st", [128, 128], F32)
        W = {}
        for gi, wd_ in enumerate((g.lru_w_r, g.lru_w_i)):
            for d in range(2):
                for cc in range(2):
                    k.do("pool", "memset", dict(ap=(wst, wst[:])), {}, constant=0.0)
                    for blk in range(2):
                        k.dma("sp", wst[blk * 64:(blk + 1) * 64, blk * 64:(blk + 1) * 64], wd_[li, d, cc * 2 + blk], reads=[wd_], writes=[wst])
                    wb = k.sb(f"lw_{gi}{d}{cc}", [128, 128], BF16)
                    k.do("dve", "tensor_copy", dict(out=(wb, wb[:])), dict(in_=(wst, wst[:])))
                    W[(gi, d, cc)] = wb
        cn = k.sb("cneg", [128, 4], F32)
        cn2 = k.sb("cneg2", [128, 4], F32)
        k.do("act", "activation", dict(out=(cn, cn[:])), dict(in_=(g.PV, g.PV[:, pv["lru_lam"]:pv["lru_lam"] + 4])), func=AF.Exp, scale=-1.0)
        k.do("act", "activation", dict(out=(cn, cn[:])), dict(in_=(cn, cn[:])), func=AF.Ln, bias=1.0, scale=1.0)
        k.do("dve", "tensor_scalar", dict(out=(cn2, cn2[:])), dict(in0=(cn, cn[:])), scalar1=-16.0, scalar2=None, op0=ALU.mult)
        k.do("dve", "tensor_scalar", dict(out=(cn, cn[:])), dict(in0=(cn, cn[:])), scalar1=-8.0, scalar2=None, op0=ALU.mult)
        XA = k.sb("l_xa", [128, TT], F32)
        U = k.sb("l_u", [128, TT], F32)
        UB = k.sb("l_ub", [128, TT], BF16)
        BV = k.sb("l_bv", [128, TT], F32)
        HF = k.sb("l_hf", [128, TT], F32)
        HB = k.sb("l_hb", [128, TT], F32)
        Y = [k.sb(f"l_y{j}", [128, TT], F32) for j in range(2)]
        tr = [k.sb(f"l_tr{j}", [128, 512], F32) for j in range(2)]
        ti = [k.sb(f"l_ti{j}", [128, 512], F32) for j in range(2)]
        for b in range(NB):
            for cc in range(2):
                k.dma("sp", XA[:], g.PT[PC_LX + cc, :, b * TT:(b + 1) * TT], reads=[g.PTr[PC_LX + cc]], writes=[XA])
                cw = lambda j: (g.PV, g.PV[:, pv["lru_cw"] + j * 2 + cc: pv["lru_cw"] + j * 2 + cc + 1])
                k.do("act", "activation", dict(out=(U, U[:])), dict(in_=(XA, XA[:]), scale=cw(2), bias=(g.PV, g.PV[:, pv["lru_cb"] + cc:pv["lru_cb"] + cc + 1])), func=AF.Identity)
                for j in (0, 1, 3):
                    dlt = j - 2
                    for (s0, s1) in SEG1:
                        lo, hi = max(s0, s0 - dlt), min(s1, s1 - dlt)
                        k.do("dve", "scalar_tensor_tensor", dict(out=(U, U[:, lo:hi])),
                             dict(in0=(XA, XA[:, lo + dlt:hi + dlt]), scalar=cw(j), in1=(U, U[:, lo:hi])), op0=ALU.mult, op1=ALU.add)
                k.do("pool", "tensor_copy", dict(out=(UB, UB[:])), dict(in_=(U, U[:])))
                for d in range(2):
                    H = HF if d == 0 else HB
                    col = d * 2 + cc
                    for n, (t0, t1) in enumerate(BLK):
                        Wd = t1 - t0
                        pr, pi = g.PS[(n % 2) * 2], g.PS[(n % 2) * 2 + 1]
                        k.do("pe", "matmul", dict(out=(pr, pr[:, :Wd])), dict(lhsT=(W[(0, d, cc)], W[(0, d, cc)][:]), rhs=(UB, UB[:, t0:t1])), start=True, stop=True)
                        k.do("pe", "matmul", dict(out=(pi, pi[:, :Wd])), dict(lhsT=(W[(1, d, cc)], W[(1, d, cc)][:]), rhs=(UB, UB[:, t0:t1])), start=True, stop=True)
                        r_, i_ = tr[n % 2], ti[n % 2]
                        k.do("act", "activation", dict(out=(r_, r_[:, :Wd])), dict(in_=(pr, pr[:, :Wd]), bias=(g.PV, g.PV[:, pv["lru_br"] + col:pv["lru_br"] + col + 1])), func=AF.Sigmoid)
                        k.do("act", "activation", dict(out=(i_, i_[:, :Wd])), dict(in_=(pi, pi[:, :Wd]), bias=(g.PV, g.PV[:, pv["lru_bi"] + col:pv["lru_bi"] + col + 1])), func=AF.Sigmoid)
                        k.do("act", "activation", dict(out=(XA, XA[:, t0:t1])), dict(in_=(r_, r_[:, :Wd]), scale=(cn, cn[:, col:col + 1])), func=AF.Exp)
                        k.do("act", "activation", dict(out=(r_, r_[:, :Wd])), dict(in_=(r_, r_[:, :Wd]), scale=(cn2, cn2[:, col:col + 1])), func=AF.Exp)
                        k.do("dve", "tensor_scalar", dict(out=(r_, r_[:, :Wd])), dict(in0=(r_, r_[:, :Wd])), scalar1=0.99999994, scalar2=None, op0=ALU.min)
                        k.do("act", "activation", dict(out=(r_, r_[:, :Wd])), dict(in_=(r_, r_[:, :Wd])), func=AF.Sqrt, scale=-1.0, bias=1.0)
                        k.do("dve", "tensor_tensor", dict(out=(i_, i_[:, :Wd])), dict(in0=(i_, i_[:, :Wd]), in1=(U, U[:, t0:t1])), op=ALU.mult)
                        k.do("dve", "tensor_tensor", dict(out=(BV, BV[:, t0:t1])), dict(in0=(i_, i_[:, :Wd]), in1=(r_, r_[:, :Wd])), op=ALU.mult)
                    if d == 0:
                        k.do("dve", "tensor_tensor_scan", dict(out=(H, H[:, 0:LC])), dict(data0=(XA, XA[:, 0:LC]), data1=(BV, BV[:, 0:LC])), initial=0.0, op0=ALU.mult, op1=ALU.add)
                        k.do("dve", "tensor_tensor_scan", dict(out=(H, H[:, LC:TT])), dict(data0=(XA, XA[:, LC:TT]), data1=(BV, BV[:, LC:TT]), initial=(H, H[:, LC - 1:LC])), op0=ALU.mult, op1=ALU.add)
                    else:
                        k.do("dve", "tensor_tensor_scan", dict(out=(H, H[:, LC - 1::-1])), dict(data0=(XA, XA[:, LC - 1::-1]), data1=(BV, BV[:, LC - 1::-1])), initial=0.0, op0=ALU.mult, op1=ALU.add)
                        k.do("dve", "tensor_tensor_scan", dict(out=(H, H[:, TT - 1:LC - 1:-1])), dict(data0=(XA, XA[:, TT - 1:LC - 1:-1]), data1=(BV, BV[:, TT - 1:LC - 1:-1]), initial=(H, H[:, 0:1])), op0=ALU.mult, op1=ALU.add)
                k.do("pool", "tensor_tensor", dict(out=(HF, HF[:])), dict(in0=(HF, HF[:]), in1=(HB, HB[:])), op=ALU.add)
                k.dma("sp", XA[:], g.PT[PC_LG + cc, :, b * TT:(b + 1) * TT], reads=[g.PTr[PC_LG + cc]], writes=[XA])
                k.do("act", "activation", dict(out=(BV, BV[:])), dict(in_=(XA, XA[:])), func=AF.Square)
                k.do("dve", "tensor_scalar", dict(out=(BV, BV[:])), dict(in0=(BV, BV[:])), scalar1=0.044715, scalar2=1.0, op0=ALU.mult, op1=ALU.add)
                k.do("dve", "tensor_tensor", dict(out=(BV, BV[:])), dict(in0=(BV, BV[:]), in1=(XA, XA[:])), op=ALU.mult)
                k.do("act", "activation", dict(out=(BV, BV[:])), dict(in_=(BV, BV[:])), func=AF.Sigmoid, scale=1.5957691216)
                k.do("dve", "tensor_tensor", dict(out=(BV, BV[:])), dict(in0=(BV, BV[:]), in1=(XA, XA[:])), op=ALU.mult)
                k.do("dve", "tensor_tensor", dict(out=(Y[cc], Y[cc][:])), dict(in0=(HF, HF[:]), in1=(BV, BV[:])), op=ALU.mult)
            rms_rows(g, Y, pv["lru_on"], 256, b)


def rope_tables():
    tab = np.zeros((128, 2, TT), np.float32)
    t = np.arange(LL)
    r, c = (t // 64).astype(np.float32), (t % 64).astype(np.float32)
    half = 16
    inv = (1.0 / (10000.0 ** (np.arange(0, half, 2, dtype=np.float32) / half))).astype(np.float32)
    ang = np.concatenate([r[:, None] * inv, c[:, None] * inv], -1).astype(np.float32)
    cos, sin = np.cos(ang).T, np.sin(ang).T
    tab[64:80, 0, LC:] = cos; tab[80:96, 0, LC:] = cos
    tab[64:80, 1, LC:] = -sin; tab[80:96, 1, LC:] = sin
    tab[64:96, 0, :LC] = 1.0
    return tab


def phase_mla(g, li):
    k = g.k
    pv = g.pv
    scale = 96.0 ** -0.5
    with k.phase():
        wq0 = k.sb("wq0", [128, 384], BF16); wq1 = k.sb("wq1", [64, 384], BF16)
        wqs0 = k.sb("wqs0", [128, 384], BF16); wqs1 = k.sb("wqs1", [64, 384], BF16)
        wuk = k.sb("wuk", [128, 384], BF16); wuv = k.sb("wuv", [128, 256], BF16)
        st = k.sb("mw_st", [128, 384], F32)
        for dst, src, rows, cols in ((wq0, g.w_uq[li, 0:128, :], 128, 384), (wq1, g.w_uq[li, 128:192, :], 64, 384),
                                     (wqs0, g.w_uq_sw[li, 0:128, :], 128, 384), (wqs1, g.w_uq_sw[li, 128:192, :], 64, 384),
                                     (wuk, g.w_uk_p[li], 128, 384), (wuv, g.w_uv[li], 128, 256)):
            k.dma("sp", st[:rows, :cols], src, reads=[g.w_uq, g.w_uq_sw, g.w_uk_p, g.w_uv], writes=[st])
            k.do("dve", "tensor_copy", dict(out=(dst, dst[:rows, :cols])), dict(in_=(st, st[:rows, :cols])))
        TAB = k.sb("ropetab", [128, 2, TT], F32)
        k.dma("act", TAB[:], g.rope[:], reads=[g.rope], writes=[TAB])
        CQN0 = k.sb("cqn0", [128, TT], BF16); CQN1 = k.sb("cqn1", [64, TT], BF16); CKVN = k.sb("ckvn", [128, TT], BF16)
        KR = k.sb("kr", [128, TT], BF16)
        QT = k.sb("qt", [96, TT], BF16); KT = k.sb("kt", [96, TT], BF16)
        VA = k.sb("va", [128, TT // 128, 4, 65], BF16)
        AO = [k.sb(f"ao{h}", [64, TT], BF16) for h in range(4)]
        xin = [k.sb(f"m_x{j}", [128, 512], F32) for j in range(3)]
        sq = k.sb("m_sq", [128, 512], F32); rs = k.sb("m_rs", [128, 512], F32)
        t1 = k.sb("m_t1", [128, 512], F32); t2 = k.sb("m_t2", [128, 512], F32)
        E = [k.sb(f"m_e{j}", [128, 512], BF16) for j in range(3)]
        rd = k.sb("m_rd", [128, 512], F32); rb = k.sb("m_rb", [64, 512], F32)
        k.do("pool", "memset", dict(ap=(VA, VA[:])), {}, constant=1.0)
        for b in range(NB):
            for n, (t0, t1_) in enumerate(BLK):
                W = t1_ - t0
                c0, c1, ckv = xin
                k.dma("sp", c0[:, :W], g.PT[PC_CQ0, :, b * TT + t0:b * TT + t1_], reads=[g.PTr[PC_CQ0]], writes=[c0])
                k.dma("act", c1[:64, :W], g.PT[PC_CQ1, 0:64, b * TT + t0:b * TT + t1_], reads=[g.PTr[PC_CQ1]], writes=[c1])
                k.dma("sp", ckv[:, :W], g.PT[PC_CKV, :, b * TT + t0:b * TT + t1_], reads=[g.PTr[PC_CKV]], writes=[ckv])
                for grp in (0, 1):
                    ps = g.PS[grp]
                    srcs = ((c0, 128), (c1, 64)) if grp == 0 else ((ckv, 128),)
                    nch = 192 if grp == 0 else 128
                    for ci, (s_, np_) in enumerate(srcs):
                        k.do("act", "activation", dict(out=(sq, sq[:np_, :W])), dict(in_=(s_, s_[:np_, :W])), func=AF.Square)
                        k.do("pe", "matmul", dict(out=(ps, ps[:, :W])), dict(lhsT=(g.ONESD, g.ONESD[:np_, :]), rhs=(sq, sq[:np_, :W])),
                             acc=(ci > 0), start=(ci == 0), stop=(ci == len(srcs) - 1))
                    k.do("dve", "tensor_scalar", dict(out=(rs, rs[:, :W])), dict(in0=(ps, ps[:, :W])), scalar1=float(D) / nch, scalar2=1e-6, op0=ALU.mult, op1=ALU.add)
                    k.do("act", "activation", dict(out=(rs, rs[:, :W])), dict(in_=(rs, rs[:, :W])), func=AF.Sqrt)
                    k.do("dve", "reciprocal", dict(out=(rs, rs[:, :W])), dict(in_=(rs, rs[:, :W])))
                    if grp == 0:
                        k.do("dve", "scalar_tensor_tensor", dict(out=(CQN0, CQN0[:, t0:t1_])), dict(in0=(c0, c0[:, :W]), scalar=(g.PV, g.PV[:, pv["q_norm"]:pv["q_norm"] + 1]), in1=(rs, rs[:, :W])), op0=ALU.mult, op1=ALU.mult)
                        k.do("dve", "scalar_tensor_tensor", dict(out=(CQN1, CQN1[:, t0:t1_])), dict(in0=(c1, c1[:64, :W]), scalar=(g.PV, g.PV[:64, pv["q_norm"] + 1:pv["q_norm"] + 2]), in1=(rs, rs[:64, :W])), op0=ALU.mult, op1=ALU.mult)
                    else:
                        k.do("dve", "scalar_tensor_tensor", dict(out=(CKVN, CKVN[:, t0:t1_])), dict(in0=(ckv, ckv[:, :W]), scalar=(g.PV, g.PV[:, pv["kv_norm"]:pv["kv_norm"] + 1]), in1=(rs, rs[:, :W])), op0=ALU.mult, op1=ALU.mult)
                k.dma("sp", c0[64:96, :W], g.PT[PC_KR, 64:96, b * TT + t0:b * TT + t1_], reads=[g.PTr[PC_KR]], writes=[c0])
                k.dma("act", c1[64:96, :W], g.PT[PC_KRS, 64:96, b * TT + t0:b * TT + t1_], reads=[g.PTr[PC_KRS]], writes=[c1])
                k.do("dve", "tensor_tensor", dict(out=(t1, t1[64:96, :W])), dict(in0=(c0, c0[64:96, :W]), in1=(TAB, TAB[64:96, 0, t0:t1_])), op=ALU.mult)
                k.do("pool", "tensor_tensor", dict(out=(t2, t2[64:96, :W])), dict(in0=(c1, c1[64:96, :W]), in1=(TAB, TAB[64:96, 1, t0:t1_])), op=ALU.mult)
                k.do("dve", "tensor_tensor", dict(out=(KR, KR[64:96, t0:t1_])), dict(in0=(t1, t1[64:96, :W]), in1=(t2, t2[64:96, :W])), op=ALU.add)
            for kt in range(TT // 128):
                ps = g.PS[kt % 2]
                k.do("pe", "matmul", dict(out=(ps, ps[:, 0:256])), dict(lhsT=(CKVN, CKVN[:, kt * 128:(kt + 1) * 128]), rhs=(wuv, wuv[:])), start=True, stop=True)
                ev(k, kt, (VA, VA[:, kt, :, 0:64]), (ps, ps[:, 0:256].rearrange("p (h v) -> p h v", h=4)))
            for h in range(4):
                hs = slice(h * 96, (h + 1) * 96)
                for n, (t0, t1_) in enumerate(BLK):
                    W = t1_ - t0
                    pq, pqs, pk = g.PS[0], g.PS[1], g.PS[2]
                    k.do("pe", "matmul", dict(out=(pq, pq[:96, :W])), dict(lhsT=(wq0, wq0[:, hs]), rhs=(CQN0, CQN0[:, t0:t1_])), start=True, stop=False)
                    k.do("pe", "matmul", dict(out=(pq, pq[:96, :W])), dict(lhsT=(wq1, wq1[:, hs]), rhs=(CQN1, CQN1[:, t0:t1_])), acc=True, start=False, stop=True)
                    k.do("pe", "matmul", dict(out=(pqs, pqs[:96, :W])), dict(lhsT=(wqs0, wqs0[:, hs]), rhs=(CQN0, CQN0[:, t0:t1_])), start=True, stop=False)
                    k.do("pe", "matmul", dict(out=(pqs, pqs[:96, :W])), dict(lhsT=(wqs1, wqs1[:, hs]), rhs=(CQN1, CQN1[:, t0:t1_])), acc=True, start=False, stop=True)
                    k.do("pe", "matmul", dict(out=(pk, pk[:96, :W])), dict(lhsT=(wuk, wuk[:, hs]), rhs=(CKVN, CKVN[:, t0:t1_])), start=True, stop=True)
                    k.do("act", "copy", dict(out=(QT, QT[0:64, t0:t1_])), dict(in_=(pq, pq[0:64, :W])))
                    k.do("act", "copy", dict(out=(KT, KT[0:64, t0:t1_])), dict(in_=(pk, pk[0:64, :W])))
                    k.do("dve", "tensor_tensor", dict(out=(t1, t1[64:96, :W])), dict(in0=(pq, pq[64:96, :W]), in1=(TAB, TAB[64:96, 0, t0:t1_])), op=ALU.mult)
                    k.do("dve", "tensor_tensor", dict(out=(t2, t2[64:96, :W])), dict(in0=(pqs, pqs[64:96, :W]), in1=(TAB, TAB[64:96, 1, t0:t1_])), op=ALU.mult)
                    k.do("dve", "tensor_tensor", dict(out=(QT, QT[64:96, t0:t1_])), dict(in0=(t1, t1[64:96, :W]), in1=(t2, t2[64:96, :W])), op=ALU.add)
                k.do("pool", "tensor_copy", dict(out=(KT, KT[64:96, :])), dict(in_=(KR, KR[64:96, :])))
                qblocks = [(0, LC, 2)] + [(LC + i * 512, LC + (i + 1) * 512, TT // 128) for i in range(LL // 512)]
                for qi, (q0, q1, nkt) in enumerate(qblocks):
                    W = q1 - q0
                    po = g.PS[4 + qi % 2]
                    for kt in range(nkt):
                        ps = g.PS[kt % 4]
                        k.do("pe", "matmul", dict(out=(ps, ps[:, :W])), dict(lhsT=(KT, KT[:, kt * 128:(kt + 1) * 128]), rhs=(QT, QT[:, q0:q1])), start=True, stop=True)
                        e_ = E[kt % 3]
                        k.do("act", "activation", dict(out=(e_, e_[:, :W])), dict(in_=(ps, ps[:, :W])), func=AF.Exp, scale=scale)
                        k.do("pe", "matmul", dict(out=(po, po[:65, :W])), dict(lhsT=(VA, VA[:, kt, h, :]), rhs=(e_, e_[:, :W])),
                             acc=(kt > 0), start=(kt == 0), stop=(kt == nkt - 1))
                    k.do("dve", "reciprocal", dict(out=(rd, rd[64:65, :W])), dict(in_=(po, po[64:65, :W])))
                    pb = g.PS[6]
                    k.do("pe", "matmul", dict(out=(pb, pb[:64, :W])), dict(lhsT=(g.ONES1, g.ONES1[64:65, 0:64]), rhs=(rd, rd[64:65, :W])), start=True, stop=True)
                    k.do("act", "copy", dict(out=(rb, rb[:, :W])), dict(in_=(pb, pb[:64, :W])))
                    k.do("dve", "tensor_tensor", dict(out=(AO[h], AO[h][:, q0:q1])), dict(in0=(po, po[0:64, :W]), in1=(rb, rb[:, :W])), op=ALU.mult)
            rms_rows(g, AO, pv["mla_on"], 0, b, nparts=64)


HY_BANDS = 16


def hyena_consts():
    def feats(L):
        t01 = np.linspace(0.0, 1.0, L, dtype=np.float32)[:, None]
        bands = np.linspace(1e-4, HY_BANDS - 1, HY_BANDS, dtype=np.float32)[None, :]
        wpos = ((2.0 * math.pi / L) * np.arange(L, dtype=np.float32)[:, None]).astype(np.float32)
        z = np.concatenate([t01, np.cos(bands * wpos), -np.sin(bands * wpos)], -1).astype(np.float32)
        dmin, dmax = math.log(1e-2) / 1.5, math.log(1e-2) / 0.3
        deltas = np.abs(np.linspace(dmin, dmax, GROUP, dtype=np.float32))
        win = (np.exp(-t01 * deltas) + 0.05).astype(np.float32)
        return z, win

    zc, wc = feats(LC)
    zl, wl = feats(LL)
    zT = np.ascontiguousarray(np.concatenate([zc, zl], 0).T)
    win = np.ascontiguousarray(np.concatenate([wc, wl], 0))

    def dft(L):
        n = L // 128
        s = np.arange(L, dtype=np.float64)[:, None]
        f = np.arange(L, dtype=np.float64)[None, :]
        ang = math.pi * (2 * f + 1) * s / (2 * L)
        out = []
        for M in (np.cos(ang), np.sin(ang)):
            M = M.astype(np.float32)
            F = M.reshape(n, 128, n, 128).transpose(2, 1, 0, 3)
            I = M.reshape(n, 128, n, 128).transpose(0, 3, 2, 1)
            out.append((np.ascontiguousarray(F).reshape(n, 128, n * 128).astype(ml_dtypes.bfloat16),
                        np.ascontiguousarray(I).reshape(n, 128, n * 128).astype(ml_dtypes.bfloat16)))
        return np.stack([out[0][0], out[1][0], out[0][1], out[1][1]])

    return zT, win, dft(LC), dft(LL)


def sin_exact(k, out, x, tmp):
    s1, c1 = tmp
    n = x[1].shape[0]
    W = x[1].shape[1]
    k.do("act", "activation", dict(out=(s1, s1[:n, :W])), dict(in_=x), func=AF.Sin, scale=0.25)
    k.do("act", "activation", dict(out=(c1, c1[:n, :W])), dict(in_=x), func=AF.Abs)
    k.do("act", "activation", dict(out=(c1, c1[:n, :W])), dict(in_=(c1, c1[:n, :W]), bias=(k.HPI, k.HPI[:n, 0:1])), func=AF.Sin, scale=-0.25)
    k.do("dve", "tensor_tensor", dict(out=(c1, c1[:n, :W])), dict(in0=(c1, c1[:n, :W]), in1=(s1, s1[:n, :W])), op=ALU.mult)
    k.do("dve", "tensor_tensor", dict(out=(s1, s1[:n, :W])), dict(in0=(s1, s1[:n, :W]), in1=(s1, s1[:n, :W])), op=ALU.mult)
    k.do("dve", "tensor_scalar", dict(out=(s1, s1[:n, :W])), dict(in0=(s1, s1[:n, :W])), scalar1=-8.0, scalar2=4.0, op0=ALU.mult, op1=ALU.add)
    k.do("dve", "tensor_tensor", dict(out=out), dict(in0=(c1, c1[:n, :W]), in1=(s1, s1[:n, :W])), op=ALU.mult)


def phase_hy_prep(g, li):
    k = g.k
    pv = g.pv
    with k.phase():
        X = k.sb("hp_x", [128, TT], F32)
        Z = k.sb("hp_z", [128, TT], F32)
        ZS = k.sb("hp_zs", [128, TT // 128, 128], F32)
        for b in range(NB):
            for j in range(6):
                k.dma("sp", X[:], g.PT[PC_HY + j, :, b * TT:(b + 1) * TT], reads=[g.PTr[PC_HY + j]], writes=[X])
                cw = lambda tap: (g.PV, g.PV[:, pv["hy_cw"] + tap * 6 + j: pv["hy_cw"] + tap * 6 + j + 1])
                k.do("act", "activation", dict(out=(Z, Z[:])), dict(in_=(X, X[:]), scale=cw(1), bias=(g.PV, g.PV[:, pv["hy_cb"] + j:pv["hy_cb"] + j + 1])), func=AF.Identity)
                for tap in (0, 2):
                    dlt = tap - 1
                    for (s0, s1) in SEG1:
                        lo, hi = max(s0, s0 - dlt), min(s1, s1 - dlt)
                        k.do("dve", "scalar_tensor_tensor", dict(out=(Z, Z[:, lo:hi])),
                             dict(in0=(X, X[:, lo + dlt:hi + dlt]), scalar=cw(tap), in1=(Z, Z[:, lo:hi])), op0=ALU.mult, op1=ALU.add)
                for tt_ in range(TT // 128):
                    ps = g.PS[tt_ % 4]
                    k.do("pe", "transpose", dict(out=(ps, ps[:, 0:128])), dict(in_=(Z, Z[:, tt_ * 128:(tt_ + 1) * 128]), identity=(g.IDENT, g.IDENT[:])))
                    ev(k, tt_, (ZS, ZS[:, tt_, :]), (ps, ps[:, 0:128]))
                dst = g.ZTOK[j // 2].rearrange("(n p) b c -> p n b c", p=128)[:, :, b, (j % 2) * 128:(j % 2 + 1) * 128]
                k.dma("sp", dst, ZS[:], reads=[ZS], writes=[g.ZTOK])


def phase_hy_main(g, li, L, tok0, dft, dres):
    k = g.k
    pv = g.pv
    n = L // 128
    with k.phase():
        zT = k.sb("hy_zT", [33, L], F32)
        k.dma("sp", zT[:], g.hy_zT[:, tok0:tok0 + L], reads=[g.hy_zT], writes=[zT])
        w1 = k.sb("hy_w1", [33, 64], F32); w2 = k.sb("hy_w2", [64, 64], F32); w3 = k.sb("hy_w3", [64, 1024], F32)
        k.dma("sp", w1[:], g.hy_w1[li], reads=[g.hy_w1], writes=[w1])
        k.dma("sp", w2[:], g.hy_w2[li], reads=[g.hy_w2], writes=[w2])
        k.dma("sp", w3[:], g.hy_w3[li], reads=[g.hy_w3], writes=[w3])
        h1 = k.sb("hy_h1", [64, L], F32); h2 = k.sb("hy_h2", [64, L], F32)
        xb = k.sb("hy_xb", [64, 512], F32)
        stmp = [k.sb(f"hy_st{j}", [64, 512], F32) for j in range(2)]
        for layer, (wt, src, dstt, bcol) in enumerate(((w1, zT, h1, pv["hy_b1"]), (w2, h1, h2, pv["hy_b2"]))):
            kk_ = 33 if layer == 0 else 64
            for i in range((L + 511) // 512):
                t0, t1 = i * 512, min((i + 1) * 512, L)
                W = t1 - t0
                ps = g.PS[i % 2]
                k.do("pe", "matmul", dict(out=(ps, ps[:64, :W])), dict(lhsT=(wt, wt[:kk_, :]), rhs=(src, src[:kk_, t0:t1])), start=True, stop=True)
                k.do("act", "activation", dict(out=(xb, xb[:, :W])), dict(in_=(ps, ps[:64, :W]), bias=(g.PV, g.PV[:64, bcol:bcol + 1])), func=AF.Identity)
                sin_exact(k, (dstt, dstt[:, t0:t1]), (xb, xb[:, :W]), stmp)
        HS = k.sb("hy_hs", [128, n, 512], BF16); HD = k.sb("hy_hd", [128, n, 512], BF16)
        hw = k.sb("hy_hw", [128, 1024], F32); wn = k.sb("hy_wn", [128, 256], F32)
        for lt in range(n):
            k.dma("sp", wn[:], g.hy_win[tok0 + lt * 128: tok0 + (lt + 1) * 128, :], reads=[g.hy_win], writes=[wn])
            for half in range(2):
                ps = g.PS[2 + half]
                k.do("pe", "matmul", dict(out=(ps, ps[:])), dict(lhsT=(h2, h2[:, lt * 128:(lt + 1) * 128]), rhs=(w3, w3[:, half * 512:(half + 1) * 512])), start=True, stop=True)
                for q in range(2):
                    k.do("dve", "tensor_tensor", dict(out=(hw, hw[:, half * 512 + q * 256: half * 512 + (q + 1) * 256])),
                         dict(in0=(ps, ps[:, q * 256:(q + 1) * 256]), in1=(wn, wn[:])), op=ALU.mult)
            for o in range(2):
                fw, bw = hw[:, o * 512:o * 512 + 256], hw[:, o * 512 + 256:o * 512 + 512]
                k.do("dve", "tensor_tensor", dict(out=(HD, HD[:, lt, o * 256:(o + 1) * 256])), dict(in0=(hw, fw), in1=(hw, bw)), op=ALU.subtract)
                if lt == 0:
                    k.do("pool", "memset", dict(ap=(hw, hw[0:1, o * 512 + 256:o * 512 + 512])), {}, constant=0.0)
                k.do("dve", "tensor_tensor", dict(out=(HS, HS[:, lt, o * 256:(o + 1) * 256])), dict(in0=(hw, fw), in1=(hw, bw)), op=ALU.add)
        cf = [k.sb(f"hy_cf{j}", [128, n * 128], BF16) for j in range(2)]
        sf = [k.sb(f"hy_sf{j}", [128, n * 128], BF16) for j in range(2)]
        so = [k.sb(f"hy_so{j}", [128, 512], F32) for j in range(4)]
        cnt = 0
        for fc in range(n):
            c_, s_ = cf[fc % 2], sf[fc % 2]
            k.dma("sp", c_[:], dft[0, fc], reads=[dres], writes=[c_])
            k.dma("act", s_[:], dft[1, fc], reads=[dres], writes=[s_])
            for which, (mt, hh) in enumerate(((c_, HS), (s_, HD))):
                ps = g.PS[4 + cnt % 4]
                for sc in range(n):
                    k.do("pe", "matmul", dict(out=(ps, ps[:])), dict(lhsT=(mt, mt[:, sc * 128:(sc + 1) * 128]), rhs=(hh, hh[:, sc, :])),
                         acc=(sc > 0), start=(sc == 0), stop=(sc == n - 1))
                o_ = so[cnt % 4]
                ev(k, cnt, (o_, o_[:]), (ps, ps[:]))
                k.dma("pool", g.SPEC[which, fc], o_[:], reads=[o_], writes=[g.SPEC])
                cnt += 1
    with k.phase():
        cf = [k.sb(f"hy_cf{j}", [128, n * 128], BF16) for j in range(2)]
        sf = [k.sb(f"hy_sf{j}", [128, n * 128], BF16) for j in range(2)]
        UIN = k.sb("hy_uin", [128, n, 512], BF16)
        Y1 = k.sb("hy_y1", [128, n, 512], BF16); Y2 = k.sb("hy_y2", [128, n, 512], BF16)
        pq = [k.sb(f"hy_pq{j}", [128, 2, 512], F32) for j in range(2)]
        vt = [k.sb(f"hy_vt{j}", [128, 512], F32) for j in range(2)]
        xg = [k.sb(f"hy_xg{j}", [128, 512], F32) for j in range(2)]
        ta = k.sb("hy_ta", [128, 512], F32); tb_ = k.sb("hy_tb", [128, 512], F32)
        tc_ = k.sb("hy_tc", [128, 512], F32); td = k.sb("hy_td", [128, 512], F32)
        drep = k.sb("hy_drep", [128, 2, 512], F32)
        k.dma("sp", drep[:], g.hy_drep[li], reads=[g.hy_drep], writes=[drep])
        ssum = k.sb("hy_ss", [128, 4], F32)
        ZT_ = [g.ZTOK[a].rearrange("t b c -> t (b c)") for a in range(4)]
        for lt in range(n):
            v_ = vt[lt % 2]
            k.dma("sp", v_[:], ZT_[0][tok0 + lt * 128: tok0 + (lt + 1) * 128, :], reads=[g.ZTOK], writes=[v_])
            k.do("pool", "tensor_copy", dict(out=(UIN, UIN[:, lt, :])), dict(in_=(v_, v_[:])))
        for o in range(2):
            for fc in range(n):
                c_, s_ = cf[fc % 2], sf[fc % 2]
                k.dma("sp", c_[:], dft[0, fc], reads=[dres], writes=[c_])
                k.dma("act", s_[:], dft[1, fc], reads=[dres], writes=[s_])
                p_ = pq[fc % 2]
                k.dma("pool", p_[:, 0, :], g.SPEC[0, fc], reads=[g.SPEC], writes=[p_])
                k.dma("pool", p_[:, 1, :], g.SPEC[1, fc], reads=[g.SPEC], writes=[p_])
                pa, pb = g.PS[(fc % 2) * 2], g.PS[(fc % 2) * 2 + 1]
                for sc in range(n):
                    k.do("pe", "matmul", dict(out=(pa, pa[:])), dict(lhsT=(c_, c_[:, sc * 128:(sc + 1) * 128]), rhs=(UIN, UIN[:, sc, :])),
                         acc=(sc > 0), start=(sc == 0), stop=(sc == n - 1))
                for sc in range(n):
                    k.do("pe", "matmul", dict(out=(pb, pb[:])), dict(lhsT=(s_, s_[:, sc * 128:(sc + 1) * 128]), rhs=(UIN, UIN[:, sc, :])),
                         acc=(sc > 0), start=(sc == 0), stop=(sc == n - 1))
                P_, Q_ = p_[:, 0, o * 256:(o + 1) * 256], p_[:, 1, o * 256:(o + 1) * 256]
                for b in range(NB):
                    A_, B_ = pa[:, b * 256:(b + 1) * 256], pb[:, b * 256:(b + 1) * 256]
                    bs = slice(b * 256, (b + 1) * 256)
                    k.do("dve", "tensor_tensor", dict(out=(ta, ta[:, bs])), dict(in0=(pa, A_), in1=(p_, P_)), op=ALU.mult)
                    k.do("dve", "tensor_tensor", dict(out=(tb_, tb_[:, bs])), dict(in0=(pb, B_), in1=(p_, Q_)), op=ALU.mult)
                    k.do("dve", "tensor_tensor", dict(out=(tc_, tc_[:, bs])), dict(in0=(pa, A_), in1=(p_, Q_)), op=ALU.mult)
                    k.do("dve", "tensor_tensor", dict(out=(td, td[:, bs])), dict(in0=(pb, B_), in1=(p_, P_)), op=ALU.mult)
                k.do("pool", "tensor_tensor", dict(out=(Y1, Y1[:, fc, :])), dict(in0=(ta, ta[:]), in1=(tb_, tb_[:])), op=ALU.subtract)
                k.do("pool", "tensor_tensor", dict(out=(Y2, Y2[:, fc, :])), dict(in0=(tc_, tc_[:]), in1=(td, td[:])), op=ALU.add)
            for tc in range(n):
                c_, s_ = cf[tc % 2], sf[tc % 2]
                k.dma("sp", c_[:], dft[2, tc], reads=[dres], writes=[c_])
                k.dma("act", s_[:], dft[3, tc], reads=[dres], writes=[s_])
                v_, x_ = vt[tc % 2], xg[tc % 2]
                rows = slice(tok0 + tc * 128, tok0 + (tc + 1) * 128)
                k.dma("pool", v_[:], ZT_[0 if o == 0 else 3][rows, :], reads=[g.ZTOK], writes=[v_])
                k.dma("pool", x_[:], ZT_[1 + o][rows, :], reads=[g.ZTOK], writes=[x_])
                py = g.PS[4 + tc % 2]
                for fc in range(n):
                    k.do("pe", "matmul", dict(out=(py, py[:])), dict(lhsT=(c_, c_[:, fc * 128:(fc + 1) * 128]), rhs=(Y1, Y1[:, fc, :])),
                         acc=(fc > 0), start=(fc == 0), stop=False)
                for fc in range(n):
                    k.do("pe", "matmul", dict(out=(py, py[:])), dict(lhsT=(s_, s_[:, fc * 128:(fc + 1) * 128]), rhs=(Y2, Y2[:, fc, :])),
                         acc=True, start=False, stop=(fc == n - 1))
                k.do("dve", "tensor_tensor", dict(out=(ta, ta[:])), dict(in0=(v_, v_[:]), in1=(drep, drep[:, o, :])), op=ALU.mult)
                k.do("dve", "scalar_tensor_tensor", dict(out=(ta, ta[:])), dict(in0=(py, py[:]), in1=(ta, ta[:])), scalar=1.0 / L, op0=ALU.mult, op1=ALU.add)
                k.do("dve", "tensor_tensor", dict(out=(tb_, tb_[:])), dict(in0=(ta, ta[:]), in1=(x_, x_[:])), op=ALU.mult)
                if o == 0:
                    k.do("pool", "tensor_copy", dict(out=(UIN, UIN[:, tc, :])), dict(in_=(tb_, tb_[:])))
                    k.dma("sp", ZT_[3][rows, :], tb_[:], reads=[tb_], writes=[g.ZTOK])
                else:
                    for b in range(NB):
                        bs = slice(b * 256, (b + 1) * 256)
                        k.do("act", "activation", dict(out=(tc_, tc_[:, bs]), accum_out=(ssum, ssum[:, b:b + 1])), dict(in_=(tb_, tb_[:, bs])), func=AF.Square)
                    k.do("dve", "tensor_scalar", dict(out=(ssum, ssum[:, 2:4])), dict(in0=(ssum, ssum[:, 0:2])), scalar1=1.0 / 256, scalar2=1e-6, op0=ALU.mult, op1=ALU.add)
                    k.do("act", "activation", dict(out=(ssum, ssum[:, 2:4])), dict(in_=(ssum, ssum[:, 2:4])), func=AF.Sqrt)
                    k.do("dve", "reciprocal", dict(out=(ssum, ssum[:, 2:4])), dict(in_=(ssum, ssum[:, 2:4])))
                    for b in range(NB):
                        bs = slice(b * 256, (b + 1) * 256)
                        k.do("dve", "tensor_scalar", dict(out=(td, td[:, bs])), dict(in0=(tb_, tb_[:, bs]), scalar1=(ssum, ssum[:, 2 + b:3 + b])), scalar2=None, op0=ALU.mult)
                    for b in range(NB):
                        for cc in range(2):
                            pt = g.PS[6 + cc]
                            k.do("pe", "transpose", dict(out=(pt, pt[:, 0:128])), dict(in_=(td, td[:, b * 256 + cc * 128: b * 256 + (cc + 1) * 128]), identity=(g.IDENT, g.IDENT[:])))
                            k.do("act", "activation", dict(out=(tc_, tc_[:, (b * 2 + cc) * 128:(b * 2 + cc + 1) * 128])),
                                 dict(in_=(pt, pt[:, 0:128]), scale=(g.PV, g.PV[:, pv["hy_on"] + cc:pv["hy_on"] + cc + 1])), func=AF.Identity)
                            r0 = 768 + cc * 128
                            k.dma("sp", g.YM[r0:r0 + 128, b * TT + tok0 + tc * 128: b * TT + tok0 + (tc + 1) * 128],
                                  tc_[:, (b * 2 + cc) * 128:(b * 2 + cc + 1) * 128], reads=[tc_], writes=[g.YM])


def phase_hyena(g, li):
    phase_hy_prep(g, li)
    phase_hy_main(g, li, LC, 0, g.dftC, g.dftC)
    phase_hy_main(g, li, LL, LC, g.dftL, g.dftL)


NCH = TT // 64


def rwkv_consts():
    th = np.zeros((128, 64, 128), np.float32)
    for b in range(2):
        for r in range(64):
            th[b * 64 + r, r, b * 64:(b + 1) * 64] = 1.0
    mi = np.zeros((128, 128), np.float32); me = np.zeros((128, 128), np.float32); bo = np.zeros((128, 128), np.float32)
    for b in range(2):
        for s in range(64):
            mi[b * 64 + s, b * 64 + s:(b + 1) * 64] = 1.0
            me[b * 64 + s, b * 64 + s + 1:(b + 1) * 64] = 1.0
        bo[b * 64:(b + 1) * 64, b * 64:(b + 1) * 64] = 1.0 / 64
    f32c = np.concatenate([th[:, 63, :], mi, me, bo], 1)
    return th.reshape(128, 64 * 128).astype(ml_dtypes.bfloat16), f32c


def bwd_lo(c):
    return (LC - 64 - 64 * c) if c < LC // 64 else (TT + LC - 64 - 64 * c)


def phase_rwkv(g, li):
    k = g.k
    pv = g.pv
    ZFv = g.ZF.t.rearrange("j p t -> (j p) t")
    with k.phase():
        X = [k.sb(f"r0_x{j}", [128, TT], F32) for j in range(2)]
        Z = [k.sb(f"r0_z{j}", [128, TT], F32) for j in range(2)]
        cm = k.sb("r0_cm", [128, 8], F32)
        k.do("dve", "tensor_tensor", dict(out=(cm, cm[:])), dict(in0=(g.PV, g.PV[:, pv["mu_prev"]:pv["mu_prev"] + 8]), in1=(g.PV, g.PV[:, pv["mu_next"]:pv["mu_next"] + 8])), op=ALU.add)
        k.do("dve", "tensor_scalar", dict(out=(cm, cm[:])), dict(in0=(cm, cm[:])), scalar1=-1.0, scalar2=1.0, op0=ALU.mult, op1=ALU.add)
        n = 0
        for b in range(NB):
            for j in range(8):
                x_, z_ = X[n % 2], Z[n % 2]
                k.dma("sp" if n % 2 == 0 else "act", x_[:], g.PT[PC_R + j, :, b * TT:(b + 1) * TT], reads=[g.PTr[PC_R + j]], writes=[x_])
                k.do("act", "activation", dict(out=(z_, z_[:])), dict(in_=(x_, x_[:]), scale=(cm, cm[:, j:j + 1])), func=AF.Identity)
                for (s0, s1) in SEG1:
                    k.do("dve", "scalar_tensor_tensor", dict(out=(z_, z_[:, s0 + 1:s1])),
                         dict(in0=(x_, x_[:, s0:s1 - 1]), scalar=(g.PV, g.PV[:, pv["mu_prev"] + j:pv["mu_prev"] + j + 1]), in1=(z_, z_[:, s0 + 1:s1])), op0=ALU.mult, op1=ALU.add)
                    k.do("dve", "scalar_tensor_tensor", dict(out=(z_, z_[:, s0:s1 - 1])),
                         dict(in0=(x_, x_[:, s0 + 1:s1]), scalar=(g.PV, g.PV[:, pv["mu_next"] + j:pv["mu_next"] + j + 1]), in1=(z_, z_[:, s0:s1 - 1])), op0=ALU.mult, op1=ALU.add)
                k.dma("sp", g.ZF[j, :, b * TT:(b + 1) * TT], z_[:], reads=[z_], writes=[g.ZF])
                n += 1
    with k.phase():
        CF = k.sb("r1_cf", [128, 512], F32)
        k.dma("sp", CF[:], g.rw_f32c[:], reads=[g.rw_f32c], writes=[CF])
        MI, ME = CF[:, 128:256], CF[:, 256:384]
        REP = k.sb("r1_rep", [128, 6, 256], F32)
        k.dma("sp", REP[:], g.rw_rep[li], reads=[g.rw_rep], writes=[REP])
        LORA = k.sb("r1_lora", [128, 2, 256], F32)
        k.dma("sp", LORA[:], g.rw_lora[li].rearrange("d p n -> p d n"), reads=[g.rw_lora], writes=[LORA])
        ZCs = [k.sb(f"r1_zc{j}", [128, 5, NB, 64], F32) for j in range(2)]
        ZR = k.sb("r1_zr", [128, 5, NB, 64], F32)
        TL = k.sb("r1_tl", [64, 128], F32)
        rs_, ks_ = k.sb("r1_r", [128, 256], F32), k.sb("r1_k", [128, 256], F32)
        kkr, sqk, kk = k.sb("r1_kkr", [128, 256], F32), k.sb("r1_sqk", [128, 256], F32), k.sb("r1_kk", [128, 256], F32)
        ss = k.sb("r1_ss", [128, 8], F32)
        lw, av, kka, tt_, kd = (k.sb(f"r1_{nm}", [128, 256], F32) for nm in ("lw", "a", "kka", "t", "kd"))
        eg, eng, egp = (k.sb(f"r1_{nm}", [128, 256], F32) for nm in ("eg", "eng", "egp"))
        O4 = [k.sb(f"r1_o4{j}", [128, 4, 256], BF16) for j in range(2)]
        ZFb = [g.ZF[j].rearrange("p (b t) -> p b t", b=NB) for j in range(8)]
        it = 0
        for c in range(NCH):
            for d in range(2):
                lo = 64 * c if d == 0 else bwd_lo(c)
                ZC = ZCs[it % 2]
                for jj, j in enumerate((0, 1, 2, 3, 6)):
                    k.dma("sp" if jj % 2 == 0 else "act", ZC[:, jj, :, :], ZFb[j][:, :, lo:lo + 64], reads=[g.ZF], writes=[ZC])
                if d == 1:
                    k.do("pool", "tensor_copy", dict(out=(ZR, ZR[:].rearrange("p j b t -> p (j b) t"))),
                         dict(in_=(ZC, ZC[:].rearrange("p j b t -> p (j b) t")[:, :, ::-1])))
                    ZS = ZR
                else:
                    ZS = ZC
                zs = lambda jj: ZS[:, jj, :, :].rearrange("p b t -> p (b t)")
                pA = g.PS[0]
                for jj in range(4):
                    k.do("pe", "transpose", dict(out=(pA, pA[:, jj * 128:(jj + 1) * 128])), dict(in_=(ZS, zs(jj)), identity=(g.IDENT, g.IDENT[:])), acc=(jj > 0))
                k.do("act", "activation", dict(out=(TL, TL[:])), dict(in_=(ZS, ZS[0:64, 4, :, :].rearrange("p b t -> p (b t)"))), func=AF.Tanh)
                pW, pA2 = g.PS[1], g.PS[2]
                k.do("pe", "matmul", dict(out=(pW, pW[:, 0:256])), dict(lhsT=(TL, TL[0:64, :]), rhs=(LORA, LORA[0:64, d, :])), start=True, stop=True)
                k.do("pe", "matmul", dict(out=(pA2, pA2[:, 0:256])), dict(lhsT=(ZS, ZS[64:128, 4, :, :].rearrange("p b t -> p (b t)")), rhs=(LORA, LORA[64:128, d, :])), start=True, stop=True)
                k.do("act", "copy", dict(out=(rs_, rs_[:])), dict(in_=(pA, pA[:, 0:256])))
                k.do("act", "copy", dict(out=(ks_, ks_[:])), dict(in_=(pA, pA[:, 256:512])))
                k.do("dve", "tensor_tensor", dict(out=(kkr, kkr[:])), dict(in0=(ks_, ks_[:]), in1=(REP, REP[:, 0, :])), op=ALU.mult)
                k.do("pool", "tensor_tensor", dict(out=(sqk, sqk[:])), dict(in0=(kkr, kkr[:]), in1=(kkr, kkr[:])), op=ALU.mult)
                k.do("dve", "tensor_reduce", dict(out=(ss, ss[:, 0:4])), dict(in_=(sqk, sqk[:].rearrange("p (h k) -> p h k", h=4))), op=ALU.add, axis=AX.X)
                k.do("dve", "tensor_scalar", dict(out=(ss, ss[:, 0:4])), dict(in0=(ss, ss[:, 0:4])), scalar1=1e-24, scalar2=None, op0=ALU.max)
                k.do("act", "activation", dict(out=(ss, ss[:, 0:4])), dict(in_=(ss, ss[:, 0:4])), func=AF.Sqrt)
                k.do("dve", "reciprocal", dict(out=(ss, ss[:, 4:8])), dict(in_=(ss, ss[:, 0:4])))
                k.do("dve", "tensor_tensor", dict(out=(kk, kk[:].rearrange("p (h k) -> p h k", h=4))),
                     dict(in0=(kkr, kkr[:].rearrange("p (h k) -> p h k", h=4)), in1=(ss, ss[:, 4:8].unsqueeze(2).to_broadcast([128, 4, 64]))), op=ALU.mult)
                k.do("dve", "tensor_tensor", dict(out=(lw, lw[:])), dict(in0=(pW, pW[:, 0:256]), in1=(REP, REP[:, 2 + d, :])), op=ALU.add)
                k.do("act", "activation", dict(out=(lw, lw[:])), dict(in_=(lw, lw[:])), func=AF.Sigmoid)
                k.do("pool", "tensor_scalar", dict(out=(lw, lw[:])), dict(in0=(lw, lw[:])), scalar1=-math.exp(-0.5), scalar2=None, op0=ALU.mult)
                k.do("dve", "tensor_tensor", dict(out=(av, av[:])), dict(in0=(pA2, pA2[:, 0:256]), in1=(REP, REP[:, 4 + d, :])), op=ALU.add)
                k.do("act", "activation", dict(out=(av, av[:])), dict(in_=(av, av[:])), func=AF.Sigmoid)
                k.do("pool", "tensor_tensor", dict(out=(kka, kka[:])), dict(in0=(kk, kk[:]), in1=(av, av[:])), op=ALU.mult)
                k.do("dve", "scalar_tensor_tensor", dict(out=(tt_, tt_[:])), dict(in0=(av, av[:]), in1=(REP, REP[:, 1, :])), scalar=-1.0, op0=ALU.add, op1=ALU.mult)
                k.do("pool", "tensor_scalar_add", dict(out=(tt_, tt_[:])), dict(in0=(tt_, tt_[:])), scalar1=1.0)
                k.do("dve", "tensor_tensor", dict(out=(kd, kd[:])), dict(in0=(ks_, ks_[:]), in1=(tt_, tt_[:])), op=ALU.mult)
                pG1, pG2 = g.PS[3], g.PS[4]
                k.do("pe", "matmul", dict(out=(pG1, pG1[:, 0:256])), dict(lhsT=(CF, MI), rhs=(lw, lw[:])), start=True, stop=True)
                k.do("pe", "matmul", dict(out=(pG2, pG2[:, 0:256])), dict(lhsT=(CF, ME), rhs=(lw, lw[:])), start=True, stop=True)
                k.do("act", "activation", dict(out=(eg, eg[:])), dict(in_=(pG1, pG1[:, 0:256])), func=AF.Exp)
                k.do("act", "activation", dict(out=(eng, eng[:])), dict(in_=(pG1, pG1[:, 0:256])), func=AF.Exp, scale=-1.0)
                k.do("act", "activation", dict(out=(egp, egp[:])), dict(in_=(pG2, pG2[:, 0:256])), func=AF.Exp)
                o4 = O4[it % 2]
                k.do("dve", "tensor_tensor", dict(out=(o4, o4[:, 0, :])), dict(in0=(kk, kk[:]), in1=(egp, egp[:])), op=ALU.mult)
                k.do("pool", "tensor_tensor", dict(out=(o4, o4[:, 1, :])), dict(in0=(kka, kka[:]), in1=(eng, eng[:])), op=ALU.mult)
                k.do("dve", "tensor_tensor", dict(out=(o4, o4[:, 2, :])), dict(in0=(kd, kd[:]), in1=(eng, eng[:])), op=ALU.mult)
                k.do("pool", "tensor_tensor", dict(out=(o4, o4[:, 3, :])), dict(in0=(rs_, rs_[:]), in1=(eg, eg[:])), op=ALU.mult)
                k.dma("sp", g.OPD[:, c, :, d * 256:(d + 1) * 256].rearrange("o p n -> p o n"), o4[:], reads=[o4], writes=[g.OPD])
                k.dma("act", g.GAM[c, :, d * 256:(d + 1) * 256], eg[:], reads=[eg], writes=[g.GAM])
                it += 1
    with k.phase():
        TH = k.sb("r2_th", [128, 64 * 128], BF16)
        k.dma("sp", TH[:], g.rw_th[:], reads=[g.rw_th], writes=[TH])
        CF = k.sb("r2_cf", [128, 512], F32)
        k.dma("sp", CF[:], g.rw_f32c[:], reads=[g.rw_f32c], writes=[CF])
        N = k.sb("r2_n", [128, 512], F32)
        k.do("pool", "memset", dict(ap=(N, N[:])), {}, constant=0.0)
        OPT = [k.sb(f"r2_opt{j}", [128, 4, 512], BF16) for j in range(2)]
        GT = [k.sb(f"r2_gt{j}", [128, 512], F32) for j in range(2)]
        VT = [k.sb(f"r2_vt{j}", [128, 8, 64], F32) for j in range(2)]
        VRAW = [k.sb(f"r2_vr{j}", [128, 4, 64], F32) for j in range(2)]
        YT = [k.sb(f"r2_yt{j}", [128, 8, 64], F32) for j in range(2)]
        tmp = k.sb("r2_tmp", [128, 512], F32); tmp2 = k.sb("r2_tmp2", [128, 512], F32)
        sa = k.sb("r2_sa", [128, 8], F32)
        v3 = lambda ap: ap.rearrange("p (g k) -> p g k", g=8)
        for c in range(NCH):
            opt, gt, vt, vr, yt = OPT[c % 2], GT[c % 2], VT[c % 2], VRAW[c % 2], YT[c % 2]
            k.dma("sp", opt[:], g.OPD[:, c].rearrange("o p n -> p o n"), reads=[g.OPD], writes=[opt])
            k.dma("act", gt[:], g.GAM[c], reads=[g.GAM], writes=[gt])
            for b in range(NB):
                src = ZFv[512:768, b * TT + 64 * c: b * TT + 64 * c + 64].rearrange("(h v) t -> v h t", v=64)
                k.dma("sp", vt[b * 64:(b + 1) * 64, 0:4, :], src, reads=[g.ZF], writes=[vt])
                lo = bwd_lo(c)
                src = ZFv[512:768, b * TT + lo: b * TT + lo + 64].rearrange("(h v) t -> v h t", v=64)
                k.dma("act", vr[b * 64:(b + 1) * 64, :, :], src, reads=[g.ZF], writes=[vr])
            k.do("pool", "tensor_copy", dict(out=(vt, vt[:, 4:8, :])), dict(in_=(vr, vr[:, :, ::-1])))
            for r in range(64):
                st = c * 64 + r
                pb = [g.PS[(st % 2) * 4 + op] for op in range(4)]
                for op in range(4):
                    k.do("pe", "matmul", dict(out=(pb[op], pb[op][:])), dict(lhsT=(TH, TH[:, r * 128:(r + 1) * 128]), rhs=(opt, opt[:, op, :])), start=True, stop=True)
                k.do("dve", "tensor_tensor", dict(out=(tmp, tmp[:])), dict(in0=(N, N[:]), in1=(pb[0], pb[0][:])), op=ALU.mult)
                k.do("dve", "tensor_reduce", dict(out=(sa, sa[:])), dict(in_=(tmp, v3(tmp[:]))), op=ALU.add, axis=AX.X)
                k.do("dve", "tensor_tensor", dict(out=(tmp, v3(tmp[:]))), dict(in0=(pb[1], v3(pb[1][:])), in1=(sa, sa[:].unsqueeze(2).to_broadcast([128, 8, 64]))), op=ALU.mult)
                k.do("dve", "tensor_tensor", dict(out=(N, N[:])), dict(in0=(N, N[:]), in1=(tmp, tmp[:])), op=ALU.subtract)
                k.do("dve", "tensor_tensor", dict(out=(tmp2, v3(tmp2[:]))), dict(in0=(pb[2], v3(pb[2][:])), in1=(vt, vt[:, :, r:r + 1].to_broadcast([128, 8, 64]))), op=ALU.mult)
                k.do("dve", "tensor_tensor", dict(out=(N, N[:])), dict(in0=(N, N[:]), in1=(tmp2, tmp2[:])), op=ALU.add)
                k.do("dve", "tensor_tensor", dict(out=(tmp, tmp[:])), dict(in0=(N, N[:]), in1=(pb[3], pb[3][:])), op=ALU.mult)
                k.do("dve", "tensor_reduce", dict(out=(yt, yt[:, :, r])), dict(in_=(tmp, v3(tmp[:]))), op=ALU.add, axis=AX.X)
            pg = g.PS[0]
            k.do("pe", "matmul", dict(out=(pg, pg[:])), dict(lhsT=(CF, CF[:, 0:128]), rhs=(gt, gt[:])), start=True, stop=True)
            k.do("dve", "tensor_tensor", dict(out=(N, N[:])), dict(in0=(N, N[:]), in1=(pg, pg[:])), op=ALU.mult)
            for d in range(2):
                for b in range(NB):
                    dst = g.YS[d, b].rearrange("(h v) t -> v h t", v=64)[:, :, 64 * c:64 * c + 64]
                    k.dma("sp" if d == 0 else "act", dst, yt[b * 64:(b + 1) * 64, d * 4:(d + 1) * 4, :], reads=[yt], writes=[g.YS])
    with k.phase():
        CF = k.sb("r3_cf", [128, 512], F32)
        k.dma("sp", CF[:], g.rw_f32c[:], reads=[g.rw_f32c], writes=[CF])
        BO = CF[:, 384:512]
        LORA = k.sb("r3_lora", [128, 2, 256], F32)
        k.dma("sp", LORA[:], g.rw_lora[li].rearrange("d p n -> p d n"), reads=[g.rw_lora], writes=[LORA])
        GUP = k.sb("r3_gup", [64, 256], F32)
        k.dma("sp", GUP[:], g.rw_gup[li], reads=[g.rw_gup], writes=[GUP])
        YF = k.sb("r3_yf", [128, TT], F32); YB = k.sb("r3_yb", [128, TT], F32)
        ZRt = k.sb("r3_zr", [128, TT], F32); ZK = k.sb("r3_zk", [128, TT], F32); ZV = k.sb("r3_zv", [128, TT], F32)
        ZL = k.sb("r3_zl", [128, TT], F32); ZG = k.sb("r3_zg", [64, TT], F32)
        t_ = [k.sb(f"r3_t{j}", [128, 512], F32) for j in range(6)]
        for b in range(NB):
            bs = slice(b * TT, (b + 1) * TT)
            k.dma("sp", ZL[:], g.ZF[6, :, bs], reads=[g.ZF], writes=[ZL])
            k.dma("act", ZG[:], g.ZF[7, 0:64, bs], reads=[g.ZF], writes=[ZG])
            for cc in range(2):
                k.dma("sp", YF[:], g.YS[0, b, cc * 128:(cc + 1) * 128, :], reads=[g.YS], writes=[YF])
                k.dma("act", YB[:], g.YS[1, b, cc * 128:(cc + 1) * 128, :], reads=[g.YS], writes=[YB])
                k.dma("sp", ZRt[:], g.ZF[0 + cc, :, bs], reads=[g.ZF], writes=[ZRt])
                k.dma("act", ZK[:], g.ZF[2 + cc, :, bs], reads=[g.ZF], writes=[ZK])
                k.dma("sp", ZV[:], g.ZF[4 + cc, :, bs], reads=[g.ZF], writes=[ZV])
                k.do("dve", "tensor_tensor", dict(out=(YF, YF[:, 0:LC])), dict(in0=(YF, YF[:, 0:LC]), in1=(YB, YB[:, LC - 1::-1])), op=ALU.add)
                k.do("dve", "tensor_tensor", dict(out=(YF, YF[:, LC:TT])), dict(in0=(YF, YF[:, LC:TT]), in1=(YB, YB[:, TT - 1:LC - 1:-1])), op=ALU.add)
                for n, (t0, t1) in enumerate(BLK):
                    W = t1 - t0
                    sq, mean, var, yn, a0v, a1v = t_
                    pm, pe, pa0, pa1, psb, pgt = (g.PS[i] for i in range(6))
                    k.do("act", "activation", dict(out=(sq, sq[:, :W])), dict(in_=(YF, YF[:, t0:t1])), func=AF.Square)
                    k.do("pe", "matmul", dict(out=(pm, pm[:, :W])), dict(lhsT=(CF, BO), rhs=(YF, YF[:, t0:t1])), start=True, stop=True)
                    k.do("pe", "matmul", dict(out=(pe, pe[:, :W])), dict(lhsT=(CF, BO), rhs=(sq, sq[:, :W])), start=True, stop=True)
                    k.do("act", "copy", dict(out=(mean, mean[:, :W])), dict(in_=(pm, pm[:, :W])))
                    k.do("pool", "tensor_tensor", dict(out=(var, var[:, :W])), dict(in0=(mean, mean[:, :W]), in1=(mean, mean[:, :W])), op=ALU.mult)
                    k.do("dve", "tensor_tensor", dict(out=(var, var[:, :W])), dict(in0=(pe, pe[:, :W]), in1=(var, var[:, :W])), op=ALU.subtract)
                    k.do("dve", "tensor_scalar", dict(out=(var, var[:, :W])), dict(in0=(var, var[:, :W])), scalar1=0.0, scalar2=64e-5, op0=ALU.max, op1=ALU.add)
                    k.do("act", "activation", dict(out=(var, var[:, :W])), dict(in_=(var, var[:, :W])), func=AF.Sqrt)
                    k.do("dve", "reciprocal", dict(out=(var, var[:, :W])), dict(in_=(var, var[:, :W])))
                    k.do("dve", "tensor_tensor", dict(out=(yn, yn[:, :W])), dict(in0=(YF, YF[:, t0:t1]), in1=(mean, mean[:, :W])), op=ALU.subtract)
                    k.do("dve", "tensor_tensor", dict(out=(yn, yn[:, :W])), dict(in0=(yn, yn[:, :W]), in1=(var, var[:, :W])), op=ALU.mult)
                    k.do("act", "activation", dict(out=(yn, yn[:, :W])), dict(in_=(yn, yn[:, :W]), scale=(g.PV, g.PV[:, pv["rw_lng"] + cc:pv["rw_lng"] + cc + 1]), bias=(g.PV, g.PV[:, pv["rw_lnb"] + cc:pv["rw_lnb"] + cc + 1])), func=AF.Identity)
                    for d, (pp, av_) in enumerate(((pa0, a0v), (pa1, a1v))):
                        k.do("pe", "matmul", dict(out=(pp, pp[:, :W])), dict(lhsT=(LORA, LORA[64:128, d, cc * 128:(cc + 1) * 128]), rhs=(ZL, ZL[64:128, t0:t1])), start=True, stop=True)
                        k.do("act", "activation", dict(out=(av_, av_[:, :W])), dict(in_=(pp, pp[:, :W]), bias=(g.PV, g.PV[:, pv["rw_a0"] + d * 2 + cc:pv["rw_a0"] + d * 2 + cc + 1])), func=AF.Sigmoid)
                    k.do("dve", "tensor_tensor", dict(out=(a0v, a0v[:, :W])), dict(in0=(a0v, a0v[:, :W]), in1=(a1v, a1v[:, :W])), op=ALU.add)
                    k.do("dve", "tensor_scalar", dict(out=(a0v, a0v[:, :W])), dict(in0=(a0v, a0v[:, :W]), scalar2=(g.PV, g.PV[:, pv["rw_ka"] + cc:pv["rw_ka"] + cc + 1])), scalar1=-2.0, op0=ALU.add, op1=ALU.mult)
                    k.do("pool", "tensor_scalar_add", dict(out=(a0v, a0v[:, :W])), dict(in0=(a0v, a0v[:, :W])), scalar1=2.0)
                    k.do("dve", "tensor_tensor", dict(out=(a0v, a0v[:, :W])), dict(in0=(a0v, a0v[:, :W]), in1=(ZK, ZK[:, t0:t1])), op=ALU.mult)
                    k.do("dve", "scalar_tensor_tensor", dict(out=(a1v, a1v[:, :W])), dict(in0=(ZRt, ZRt[:, t0:t1]), scalar=(g.PV, g.PV[:, pv["rw_rk"] + cc:pv["rw_rk"] + cc + 1]), in1=(a0v, a0v[:, :W])), op0=ALU.mult, op1=ALU.mult)
                    k.do("pe", "matmul", dict(out=(psb, psb[:, :W])), dict(lhsT=(CF, BO), rhs=(a1v, a1v[:, :W])), start=True, stop=True)
                    k.do("dve", "scalar_tensor_tensor", dict(out=(sq, sq[:, :W])), dict(in0=(psb, psb[:, :W]), in1=(ZV, ZV[:, t0:t1])), scalar=64.0, op0=ALU.mult, op1=ALU.mult)
                    k.do("dve", "tensor_tensor", dict(out=(yn, yn[:, :W])), dict(in0=(yn, yn[:, :W]), in1=(sq, sq[:, :W])), op=ALU.add)
                    k.do("act", "activation", dict(out=(mean, mean[:64, :W])), dict(in_=(ZG, ZG[:, t0:t1])), func=AF.Sigmoid)
                    k.do("pe", "matmul", dict(out=(pgt, pgt[:, :W])), dict(lhsT=(GUP, GUP[:, cc * 128:(cc + 1) * 128]), rhs=(mean, mean[:64, :W])), start=True, stop=True)
                    k.do("dve", "tensor_tensor", dict(out=(yn, yn[:, :W])), dict(in0=(yn, yn[:, :W]), in1=(pgt, pgt[:, :W])), op=ALU.mult)
                    k.dma("sp", g.YM[512 + cc * 128:512 + (cc + 1) * 128, b * TT + t0:b * TT + t1], yn[:, :W], reads=[yn], writes=[g.YM])
```
